# Optimizing a Trainium2 kernel written in Bass

```python
import jax
import jax.numpy as jnp
from jax import lax
import numpy as np

D_MODEL = 1024
BATCH = 8
SEQ = 4096
DEPTH = 2

GLA_HEADS = 4
GLA_DK = 64
GLA_DV = 128
GLA_KW = GLA_HEADS * GLA_DK
GLA_VW = GLA_HEADS * GLA_DV
GLA_DECAY_RANK = 16
GLA_TEMP = 16.0
GLA_CHUNK = 64
LRU_WIDTH = 512
LRU_BLOCKS = 8
LRU_BLOCK_DIM = LRU_WIDTH // LRU_BLOCKS
LRU_CONV = 4
LRU_C = 8.0
RWKV_HEAD = 64
RWKV_WIDTH = 512
RWKV_HEADS = RWKV_WIDTH // RWKV_HEAD
RWKV_DECAY_RANK = 64
RWKV_A_RANK = 64
RWKV_GATE_RANK = 160
RWKV_LNX_EPS = 64e-5
RWKV_IN_SIZES = (RWKV_WIDTH, RWKV_WIDTH, RWKV_WIDTH, RWKV_DECAY_RANK, RWKV_A_RANK, RWKV_GATE_RANK)
RWKV_IN = sum(RWKV_IN_SIZES)
N_BRANCH = 3
IN_SIZES = (GLA_KW, GLA_KW, GLA_VW, GLA_VW, GLA_DECAY_RANK, LRU_WIDTH, LRU_WIDTH, RWKV_IN, N_BRANCH * D_MODEL)
N_IN = sum(IN_SIZES)
FFN_DENSE = 2816
N_EXPERTS = 8
TOP_K = 2
FFN_EXPERT = 3584
MOE_BLOCK = 256
N_DENSE_LAYERS = (DEPTH + 1) // 2
N_MOE_LAYERS = DEPTH // 2
DEEPNORM_ALPHA = (2 * DEPTH) ** 0.25
DEEPNORM_BETA = (8 * DEPTH) ** -0.25
LN_EPS = 1e-5

kernel_name = 'hybrid_gla_rglru_rwkv7_deepnorm_moe'


def split_last(t, sizes):
    points = np.cumsum(np.array(sizes))[:-1].tolist()
    return jnp.split(t, points, axis=-1)


def layer_norm(x, g, b, eps=LN_EPS):
    xf = x.astype(jnp.float32)
    mu = jnp.mean(xf, axis=-1, keepdims=True)
    var = jnp.mean(jnp.square(xf - mu), axis=-1, keepdims=True)
    return ((xf - mu) * lax.rsqrt(var + eps) * g + b).astype(x.dtype)


def head_norm(x, g, b, eps):
    mu = jnp.mean(x, axis=-1, keepdims=True)
    var = jnp.mean(jnp.square(x - mu), axis=-1, keepdims=True)
    y = (x - mu) * lax.rsqrt(var + eps)
    return y.reshape(x.shape[0], x.shape[1], -1) * g + b


def gla_chunked(q, k, v, log_a):
    B, S, H, DK = q.shape
    DV = v.shape[-1]
    C = GLA_CHUNK
    N = S // C

    def chunks(t):
        return t.reshape(B, N, C, H, t.shape[-1]).transpose(0, 3, 1, 2, 4)

    q, k, v, log_a = chunks(q), chunks(k), chunks(v), chunks(log_a)
    b = jnp.cumsum(log_a, axis=3)
    b_end = b[:, :, :, -1:, :]
    q_dec = q * jnp.exp(b)
    k_inv = k * jnp.exp(-b)
    k_end = k * jnp.exp(b_end - b)
    causal = jnp.tril(jnp.ones((C, C), dtype=bool))
    scores = jnp.einsum('bhnid,bhnjd->bhnij', q_dec, k_inv)
    scores = jnp.where(causal, scores, 0.0)
    o_intra = jnp.einsum('bhnij,bhnje->bhnie', scores, v)
    d_state = jnp.einsum('bhnjd,bhnje->bhnde', k_end, v)
    decay_end = jnp.exp(b_end[:, :, :, 0, :])

    def step(state, inp):
        dec, ds = inp
        return state * dec[..., None] + ds, state

    s0 = jnp.zeros((B, H, DK, DV), jnp.float32)
    _, s_before = lax.scan(step, s0, (jnp.moveaxis(decay_end, 2, 0), jnp.moveaxis(d_state, 2, 0)))
    s_before = jnp.moveaxis(s_before, 0, 2)
    o = o_intra + jnp.einsum('bhnid,bhnde->bhnie', q_dec, s_before)
    return o.transpose(0, 2, 3, 1, 4).reshape(B, S, H, DV)


def gla_branch(q, k, v, r, dec_lr, w_decay_up, b_decay, norm_g, norm_b):
    B, S, _ = q.shape
    f32 = jnp.float32
    qh = q.reshape(B, S, GLA_HEADS, GLA_DK).astype(f32) * (GLA_DK ** -0.5)
    kh = k.reshape(B, S, GLA_HEADS, GLA_DK).astype(f32)
    vh = v.reshape(B, S, GLA_HEADS, GLA_DV).astype(f32)
    log_a = jax.nn.log_sigmoid((dec_lr @ w_decay_up + b_decay).astype(f32)) / GLA_TEMP
    o = gla_chunked(qh, kh, vh, log_a.reshape(B, S, GLA_HEADS, GLA_DK))
    o = head_norm(o, norm_g, norm_b, LN_EPS)
    return o.astype(r.dtype) * jax.nn.silu(r)


def rglru_branch(xr, gate, conv_w, conv_b, w_r, b_r, w_i, b_i, lam):
    B, S, W = xr.shape
    xp = jnp.pad(xr, ((0, 0), (LRU_CONV - 1, 0), (0, 0)))
    xc = conv_b + sum(xp[:, j:j + S] * conv_w[j] for j in range(LRU_CONV))
    xb = xc.reshape(B, S, LRU_BLOCKS, LRU_BLOCK_DIM)
    r = jax.nn.sigmoid(jnp.einsum('bsgi,gij->bsgj', xb, w_r).reshape(B, S, W) + b_r)
    i = jax.nn.sigmoid(jnp.einsum('bsgi,gij->bsgj', xb, w_i).reshape(B, S, W) + b_i)
    log_a = (-LRU_C * r * jax.nn.softplus(-lam)).astype(jnp.float32)
    a = jnp.exp(log_a)
    u = jnp.sqrt(-jnp.expm1(2.0 * log_a)) * (i * xc).astype(jnp.float32)

    def combine(c1, c2):
        a1, b1 = c1
        a2, b2 = c2
        return a1 * a2, a2 * b1 + b2

    _, h = lax.associative_scan(combine, (a, u), axis=1)
    return jax.nn.gelu(gate) * h.astype(gate.dtype)


def rwkv7_scan(r, w, k, v, a, b):
    B, S, H, N = r.shape

    def step(state, inp):
        r_t, w_t, k_t, v_t, a_t, b_t = inp
        sa = jnp.einsum('bhij,bhj->bhi', state, a_t)
        state = (state * w_t[:, :, None, :] + sa[..., None] * b_t[:, :, None, :]
                 + v_t[..., None] * k_t[:, :, None, :])
        return state, jnp.einsum('bhij,bhj->bhi', state, r_t)

    xs = tuple(jnp.moveaxis(t, 1, 0) for t in (r, w, k, v, a, b))
    s0 = jnp.zeros((B, H, N, N), jnp.float32)
    _, y = lax.scan(step, s0, xs)
    return jnp.moveaxis(y, 0, 1)


def rwkv7_branch(p, mu, w0, w2, a0, a2, g2, k_k, k_a, r_k, lnx_g, lnx_b):
    B, S, _ = p.shape
    f32 = jnp.float32
    p_prev = jnp.pad(p, ((0, 0), (1, 0), (0, 0)))[:, :-1]
    p = p + (p_prev - p) * mu
    r, k, v, wl, al, gl = split_last(p, RWKV_IN_SIZES)
    w_log = -jax.nn.softplus(-(w0 + jnp.tanh(wl) @ w2)) - 0.5
    decay = jnp.exp(-jnp.exp(w_log.astype(f32)))
    a = jax.nn.sigmoid(a0 + al @ a2)
    g = jax.nn.sigmoid(gl) @ g2

    def heads(t):
        return t.reshape(B, S, RWKV_HEADS, RWKV_HEAD).astype(f32)

    kk = heads(k * k_k)
    kk = kk / jnp.maximum(jnp.linalg.norm(kk, axis=-1, keepdims=True), 1e-12)
    k = k * (1.0 + (a - 1.0) * k_a)
    rh, kh, vh, ah, wh = heads(r), heads(k), heads(v), heads(a), heads(decay)
    y = rwkv7_scan(rh, wh, kh, vh, -kk, kk * ah)
    y = head_norm(y, lnx_g, lnx_b, RWKV_LNX_EPS)
    bonus = jnp.sum(rh * kh * r_k.reshape(RWKV_HEADS, RWKV_HEAD), axis=-1, keepdims=True) * vh
    y = y + bonus.reshape(B, S, RWKV_WIDTH)
    return y.astype(g.dtype) * g


def swiglu(x, w_gate, w_up, w_down):
    return (jax.nn.silu(x @ w_gate) * (x @ w_up)) @ w_down


def moe_swiglu(x, w_router, w_gate, w_up, w_down):
    B, S, D = x.shape
    xt = x.reshape(-1, D)
    T = xt.shape[0]
    TK = T * TOP_K
    logits = (xt @ w_router).astype(jnp.float32)
    top_logit, top_e = lax.top_k(logits, TOP_K)
    top_w = jax.nn.softmax(top_logit, axis=-1)
    flat_e = top_e.reshape(-1)
    flat_tok = jnp.repeat(jnp.arange(T, dtype=jnp.int32), TOP_K)
    order = jnp.argsort(flat_e)
    e_sorted = flat_e[order]
    counts = jnp.bincount(flat_e, length=N_EXPERTS)
    padded = (counts + MOE_BLOCK - 1) // MOE_BLOCK * MOE_BLOCK
    pad_end = jnp.cumsum(padded)
    pad_start = pad_end - padded
    grp_start = jnp.cumsum(counts) - counts
    dest = pad_start[e_sorted] + jnp.arange(TK, dtype=jnp.int32) - grp_start[e_sorted]
    n_blocks = -(-TK // MOE_BLOCK) + N_EXPERTS
    n_buf = n_blocks * MOE_BLOCK
    buf_tok = jnp.zeros((n_buf,), jnp.int32).at[dest].set(flat_tok[order])
    buf_w = jnp.zeros((n_buf,), jnp.float32).at[dest].set(top_w.reshape(-1)[order])
    blk_start = jnp.arange(n_blocks, dtype=jnp.int32) * MOE_BLOCK
    blk_e = jnp.minimum(jnp.sum(blk_start[:, None] >= pad_end[None, :], axis=1), N_EXPERTS - 1)
    xb = xt[buf_tok].reshape(n_blocks, MOE_BLOCK, D)

    def expert_block(args):
        xblk, e = args
        h = jax.nn.silu(xblk @ w_gate[e]) * (xblk @ w_up[e])
        return h @ w_down[e]

    yb = lax.map(expert_block, (xb, blk_e)).reshape(n_buf, D)
    y = jnp.zeros_like(xt).at[buf_tok].add(yb * buf_w[:, None].astype(x.dtype))
    return y.reshape(B, S, D)


def setup_inputs(seed: int = 0) -> dict:
    key = jax.random.key(seed)
    keys = jax.random.split(key, 48)
    it = iter([keys[i] for i in range(48)])
    L, ND, NM, D = DEPTH, N_DENSE_LAYERS, N_MOE_LAYERS, D_MODEL
    f32 = jnp.float32

    def nrm(shape, scale):
        return scale * jax.random.normal(next(it), shape, f32)

    def unif(shape, lo, hi):
        return jax.random.uniform(next(it), shape, f32, lo, hi)

    s = jnp.power(unif((L, LRU_WIDTH), 0.9, 0.999), 1.0 / LRU_C)
    lam = jnp.log(s) - jnp.log1p(-s)
    beta = DEEPNORM_BETA
    return {
        'x': nrm((BATCH, SEQ, D), 1.0),
        'w_in': nrm((L, D, N_IN), D ** -0.5),
        'b_in': nrm((L, N_IN), 0.01),
        'gla_w_decay_up': nrm((L, GLA_DECAY_RANK, GLA_KW), GLA_DECAY_RANK ** -0.5),
        'gla_b_decay': nrm((L, GLA_KW), 0.5),
        'gla_norm_g': 1.0 + nrm((L, GLA_VW), 0.02),
        'gla_norm_b': nrm((L, GLA_VW), 0.02),
        'lru_conv_w': nrm((L, LRU_CONV, LRU_WIDTH), LRU_CONV ** -0.5),
        'lru_conv_b': nrm((L, LRU_WIDTH), 0.01),
        'lru_w_r': nrm((L, LRU_BLOCKS, LRU_BLOCK_DIM, LRU_BLOCK_DIM), LRU_BLOCK_DIM ** -0.5),
        'lru_b_r': nrm((L, LRU_WIDTH), 0.1),
        'lru_w_i': nrm((L, LRU_BLOCKS, LRU_BLOCK_DIM, LRU_BLOCK_DIM), LRU_BLOCK_DIM ** -0.5),
        'lru_b_i': nrm((L, LRU_WIDTH), 0.1),
        'lru_lambda': lam,
        'rwkv_mu': unif((L, RWKV_IN), 0.0, 1.0),
        'rwkv_w0': unif((L, RWKV_WIDTH), -6.0, -1.0),
        'rwkv_w2': nrm((L, RWKV_DECAY_RANK, RWKV_WIDTH), 0.5 * RWKV_DECAY_RANK ** -0.5),
        'rwkv_a0': nrm((L, RWKV_WIDTH), 0.1),
        'rwkv_a2': nrm((L, RWKV_A_RANK, RWKV_WIDTH), 0.5 * RWKV_A_RANK ** -0.5),
        'rwkv_g2': nrm((L, RWKV_GATE_RANK, RWKV_WIDTH), RWKV_GATE_RANK ** -0.5),
        'rwkv_k_k': 0.85 + nrm((L, RWKV_WIDTH), 0.05),
        'rwkv_k_a': 1.0 + nrm((L, RWKV_WIDTH), 0.05),
        'rwkv_r_k': nrm((L, RWKV_WIDTH), 0.1),
        'rwkv_lnx_g': 1.0 + nrm((L, RWKV_WIDTH), 0.02),
        'rwkv_lnx_b': nrm((L, RWKV_WIDTH), 0.02),
        'p_gla': nrm((L, GLA_VW, D), beta * GLA_VW ** -0.5),
        'p_lru': nrm((L, LRU_WIDTH, D), beta * LRU_WIDTH ** -0.5),
        'p_rwkv': nrm((L, RWKV_WIDTH, D), beta * RWKV_WIDTH ** -0.5),
        'w_out': nrm((L, D, D), beta * D ** -0.5),
        'ln_mix_g': 1.0 + nrm((L, D), 0.02),
        'ln_mix_b': nrm((L, D), 0.02),
        'ffn_w_gate': nrm((ND, D, FFN_DENSE), D ** -0.5),
        'ffn_w_up': nrm((ND, D, FFN_DENSE), D ** -0.5),
        'ffn_w_down': nrm((ND, FFN_DENSE, D), beta * FFN_DENSE ** -0.5),
        'moe_w_router': nrm((NM, D, N_EXPERTS), D ** -0.5),
        'moe_w_gate': nrm((NM, N_EXPERTS, D, FFN_EXPERT), D ** -0.5),
        'moe_w_up': nrm((NM, N_EXPERTS, D, FFN_EXPERT), D ** -0.5),
        'moe_w_down': nrm((NM, N_EXPERTS, FFN_EXPERT, D), beta * FFN_EXPERT ** -0.5),
        'ln_ffn_g': 1.0 + nrm((L, D), 0.02),
        'ln_ffn_b': nrm((L, D), 0.02),
    }


def reference(x, w_in, b_in, gla_w_decay_up, gla_b_decay, gla_norm_g, gla_norm_b,
              lru_conv_w, lru_conv_b, lru_w_r, lru_b_r, lru_w_i, lru_b_i, lru_lambda,
              rwkv_mu, rwkv_w0, rwkv_w2, rwkv_a0, rwkv_a2, rwkv_g2, rwkv_k_k, rwkv_k_a, rwkv_r_k,
              rwkv_lnx_g, rwkv_lnx_b, p_gla, p_lru, p_rwkv, w_out, ln_mix_g, ln_mix_b,
              ffn_w_gate, ffn_w_up, ffn_w_down, moe_w_router, moe_w_gate, moe_w_up, moe_w_down,
              ln_ffn_g, ln_ffn_b):
    B, S, D = x.shape
    for l in range(DEPTH):
        h = x @ w_in[l] + b_in[l]
        q, k, v, r, dec_lr, lru_x, lru_g, rw_in, gate_logits = split_last(h, IN_SIZES)
        o_gla = gla_branch(q, k, v, r, dec_lr, gla_w_decay_up[l], gla_b_decay[l],
                           gla_norm_g[l], gla_norm_b[l])
        o_lru = rglru_branch(lru_x, lru_g, lru_conv_w[l], lru_conv_b[l], lru_w_r[l], lru_b_r[l],
                             lru_w_i[l], lru_b_i[l], lru_lambda[l])
        o_rwkv = rwkv7_branch(rw_in, rwkv_mu[l], rwkv_w0[l], rwkv_w2[l], rwkv_a0[l], rwkv_a2[l],
                              rwkv_g2[l], rwkv_k_k[l], rwkv_k_a[l], rwkv_r_k[l],
                              rwkv_lnx_g[l], rwkv_lnx_b[l])
        gates = jax.nn.sigmoid(gate_logits).reshape(B, S, N_BRANCH, D)
        merged = (gates[:, :, 0] * (o_gla @ p_gla[l])
                  + gates[:, :, 1] * (o_lru @ p_lru[l])
                  + gates[:, :, 2] * (o_rwkv @ p_rwkv[l]))
        x = layer_norm(DEEPNORM_ALPHA * x + merged @ w_out[l], ln_mix_g[l], ln_mix_b[l])
        i = l // 2
        if l % 2 == 0:
            f = swiglu(x, ffn_w_gate[i], ffn_w_up[i], ffn_w_down[i])
        else:
            f = moe_swiglu(x, moe_w_router[i], moe_w_gate[i], moe_w_up[i], moe_w_down[i])
        x = layer_norm(DEEPNORM_ALPHA * x + f, ln_ffn_g[l], ln_ffn_b[l])
    return x
```

```python
import numpy as np
from contextlib import ExitStack, contextmanager
import concourse.bass as bass
import concourse.mybir as mybir
from concourse.bass_utils import run_bass_kernel_spmd

F32 = mybir.dt.float32
BF16 = mybir.dt.bfloat16
I32 = mybir.dt.int32
AF = mybir.ActivationFunctionType
ALU = mybir.AluOpType

T = 4096
TT = 512
NT = T // TT
CH = 64
NCH = TT // CH
DM = 1024
NIN = 7472
NMIX = 4400
NMIXP = 4480
ALPHA = 4.0 ** 0.25
FD = 2816
FE = 3584
NE = 8
C0 = float(np.exp(-0.5))

Q0, K0, V0, R0, DEC0, LX0, LG0, RW0 = 0, 256, 512, 1024, 1536, 1552, 2064, 2576
RWR, RWK, RWV, RWW, RWA, RWG = RW0, RW0 + 512, RW0 + 1024, RW0 + 1536, RW0 + 1600, RW0 + 1664

C128 = {}
_o = 0
for _n, _c in [('b_in', 35), ('b_gate', 24), ('gla_ng', 4), ('gla_nb', 4), ('conv_w', 16), ('conv_b', 4),
               ('b_r', 4), ('b_i', 4), ('lam', 4), ('ln_mix_g', 8), ('ln_mix_b', 8), ('ln_ffn_g', 8),
               ('ln_ffn_b', 8), ('mu_g', 2)]:
    C128[_n] = _o
    _o += _c
N128 = _o
C64 = {}
_o = 0
for _n, _c in [('gla_bd', 4), ('mu_r', 8), ('mu_k', 8), ('mu_v', 8), ('mu_w', 1), ('mu_a', 1), ('w0', 8), ('a0', 8),
               ('k_k', 8), ('k_a', 8), ('r_k', 8), ('lnx_g', 8), ('lnx_b', 8)]:
    C64[_n] = _o
    _o += _c
N64 = _o


def _cols(v, P):
    v = np.asarray(v, np.float32).reshape(-1)
    n = -(-v.size // P) * P
    pad = np.zeros(n, np.float32)
    pad[:v.size] = v
    return pad.reshape(n // P, P).T


def pack_cols(inp, l):
    c128 = np.zeros((128, N128), np.float32)
    c64 = np.zeros((64, N64), np.float32)

    def put(dst, table, name, arr, P):
        a = _cols(arr, P)
        dst[:, table[name]:table[name] + a.shape[1]] = a
    put(c128, C128, 'b_in', inp['b_in'][l][:NMIX], 128)
    put(c128, C128, 'b_gate', inp['b_in'][l][NMIX:], 128)
    put(c128, C128, 'gla_ng', inp['gla_norm_g'][l], 128)
    put(c128, C128, 'gla_nb', inp['gla_norm_b'][l], 128)
    put(c128, C128, 'conv_w', inp['lru_conv_w'][l], 128)
    put(c128, C128, 'conv_b', inp['lru_conv_b'][l], 128)
    put(c128, C128, 'b_r', inp['lru_b_r'][l], 128)
    put(c128, C128, 'b_i', inp['lru_b_i'][l], 128)
    put(c128, C128, 'lam', inp['lru_lambda'][l], 128)
    put(c128, C128, 'ln_mix_g', inp['ln_mix_g'][l], 128)
    put(c128, C128, 'ln_mix_b', inp['ln_mix_b'][l], 128)
    put(c128, C128, 'ln_ffn_g', inp['ln_ffn_g'][l], 128)
    put(c128, C128, 'ln_ffn_b', inp['ln_ffn_b'][l], 128)
    mu = inp['rwkv_mu'][l]
    put(c128, C128, 'mu_g', mu[1664:1824], 128)
    put(c64, C64, 'gla_bd', inp['gla_b_decay'][l], 64)
    put(c64, C64, 'mu_r', mu[0:512], 64)
    put(c64, C64, 'mu_k', mu[512:1024], 64)
    put(c64, C64, 'mu_v', mu[1024:1536], 64)
    put(c64, C64, 'mu_w', mu[1536:1600], 64)
    put(c64, C64, 'mu_a', mu[1600:1664], 64)
    put(c64, C64, 'w0', inp['rwkv_w0'][l], 64)
    put(c64, C64, 'a0', inp['rwkv_a0'][l], 64)
    put(c64, C64, 'k_k', inp['rwkv_k_k'][l], 64)
    put(c64, C64, 'k_a', inp['rwkv_k_a'][l], 64)
    put(c64, C64, 'r_k', inp['rwkv_r_k'][l], 64)
    put(c64, C64, 'lnx_g', inp['rwkv_lnx_g'][l], 64)
    put(c64, C64, 'lnx_b', inp['rwkv_lnx_b'][l], 64)
    return c128, c64


class Tl:
    def __init__(self, t, key):
        self.t = t
        self.key = key

    def __getitem__(self, idx):
        return self.t[idx]


def _keys(xs):
    out = []
    for x in xs:
        if isinstance(x, Tl):
            out.append(x.key)
        elif isinstance(x, list):
            out.extend(_keys(x))
        else:
            out.append(x)
    return out


class Prog:
    NDMA = 4

    def __init__(self, nc, es):
        self.nc = nc
        self.es = es
        self.engs = {'pe': nc.tensor, 'act': nc.scalar, 'dve': nc.vector, 'pool': nc.gpsimd, 'sp': nc.sync}
        self.sem = {}
        self.cnt = {}
        for n in self.engs:
            self.sem[n] = es.enter_context(nc.semaphore('s_' + n))
            self.cnt[n] = 0
        self.dq = {}
        for q in ('sp', 'pool', 'act'):
            sems = []
            for i in range(self.NDMA):
                nm = 'd_%s%d' % (q, i)
                self.sem[nm] = es.enter_context(nc.semaphore(nm))
                self.cnt[nm] = 0
                sems.append(nm)
            self.dq[q] = [sems, 0]
        self.seen = {}
        self.rec = None
        self.lw = {}
        self.rd = {}
        self.nwait = 0
        self.ninst = 0
        self.uid = 0
        self.ps = [Tl(es.enter_context(nc.psum_tensor('ps%d' % i, [128, 512], F32)), 'ps%d' % i) for i in range(8)]
        self.ps_rots = {}
        self.ps_pool = list(range(8))

    def _wait(self, eng, deps):
        e = self.engs[eng]
        for s, v in deps:
            if v <= 0 or self.seen.get((eng, s), 0) >= v:
                continue
            e.wait_ge(self.sem[s], v)
            self.nwait += 1
            self.seen[(eng, s)] = v

    def _deps(self, reads, writes):
        deps = {}
        for k in reads:
            if k in self.lw:
                s, v = self.lw[k]
                deps[s] = max(deps.get(s, 0), v)
        for k in writes:
            if k in self.lw:
                s, v = self.lw[k]
                deps[s] = max(deps.get(s, 0), v)
            for s, v in self.rd.get(k, {}).items():
                deps[s] = max(deps.get(s, 0), v)
        return list(deps.items())

    def _mark(self, s, v, reads, writes):
        for k in writes:
            self.lw[k] = (s, v)
            self.rd[k] = {}
        for k in reads:
            d = self.rd.setdefault(k, {})
            d[s] = max(d.get(s, 0), v)

    def op(self, eng, fn, r=(), w=()):
        if self.rec is not None:
            self.rec.append(('op', eng, fn, r, w, None))
            return
        reads, writes = _keys(r), _keys(w)
        self._wait(eng, self._deps(reads, writes))
        inst = fn()
        self.cnt[eng] += 1
        inst.then_inc(self.sem[eng], 1)
        self.ninst += 1
        self._mark(eng, self.cnt[eng], reads, writes)

    def dma(self, q, out, in_, r=(), w=(), **kw):
        if self.rec is not None:
            self.rec.append(('dma', q, (out, in_), r, w, kw))
            return
        reads, writes = _keys(r), _keys(w)
        sems, i = self.dq[q]
        s = sems[i % self.NDMA]
        self.dq[q][1] = i + 1
        deps = self._deps(reads, writes)
        deps.append((s, self.cnt[s]))
        self._wait(q, deps)
        inst = self.engs[q].dma_start(out=out, in_=in_, **kw)
        self.cnt[s] += 16
        inst.then_inc(self.sem[s], 16)
        self.ninst += 1
        self._mark(s, self.cnt[s], reads, writes)

    def record(self, fn):
        self.rec = []
        fn()
        L = self.rec
        self.rec = None
        return L

    def play(self, lists):
        lists = [L for L in lists if L]
        items = []
        for L in lists:
            n = float(len(L))
            items.extend(((i + 0.5) / n, j, i, it) for j, L2 in enumerate([L]) for i, it in enumerate(L))
        tagged = []
        for li, L in enumerate(lists):
            n = float(len(L))
            for i, it in enumerate(L):
                tagged.append(((i + 0.5) / n, li, i, it))
        tagged.sort(key=lambda t: (t[0], t[1], t[2]))
        for _, _, _, it in tagged:
            kind, a, b, r, w, kw = it
            if kind == 'op':
                self.op(a, b, r, w)
            else:
                self.dma(a, b[0], b[1], r, w, **kw)

    def barrier(self):
        for e in self.engs:
            self._wait(e, [(s, v) for s, v in self.cnt.items()])

    def sb(self, stack, name, shape, dt=F32):
        self.uid += 1
        t = stack.enter_context(self.nc.sbuf_tensor('%s_%d' % (name, self.uid), shape, dt))
        return Tl(t, '%s_%d' % (name, self.uid))

    @contextmanager
    def scope(self):
        with ExitStack() as st:
            yield st
            self.barrier()

    def psum(self):
        key = tuple(self.ps_pool)
        rot = self.ps_rots.get(key, 0)
        self.ps_rots[key] = rot + 1
        return self.ps[self.ps_pool[rot % len(self.ps_pool)]]

    def act(self, out, in_, func, bias=0.0, scale=1.0, r=(), w=()):
        nc = self.nc
        self.op('act', lambda: nc.scalar.activation(out=out, in_=in_, func=func, bias=bias, scale=scale), r, w)

    def tt(self, out, in0, in1, op, r=(), w=(), eng='dve'):
        e = self.engs[eng]
        self.op(eng, lambda: e.tensor_tensor(out=out, in0=in0, in1=in1, op=op), r, w)

    def ts(self, out, in0, s1, s2, op0, op1=None, r=(), w=(), eng='dve'):
        e = self.engs[eng]
        if op1 is None:
            self.op(eng, lambda: e.tensor_scalar(out=out, in0=in0, scalar1=s1, scalar2=None, op0=op0), r, w)
        else:
            self.op(eng, lambda: e.tensor_scalar(out=out, in0=in0, scalar1=s1, scalar2=s2, op0=op0, op1=op1), r, w)

    def stt(self, out, in0, scalar, in1, op0, op1, r=(), w=()):
        nc = self.nc
        self.op('dve', lambda: nc.vector.scalar_tensor_tensor(out=out, in0=in0, scalar=scalar, in1=in1, op0=op0, op1=op1), r, w)

    def copy(self, out, in_, r=(), w=(), eng='dve'):
        nc = self.nc
        if eng == 'act':
            self.op('act', lambda: nc.scalar.copy(out=out, in_=in_), r, w)
        else:
            e = self.engs[eng]
            self.op(eng, lambda: e.tensor_copy(out=out, in_=in_), r, w)

    def mm(self, out, pairs, r=(), w=()):
        nc = self.nc
        n = len(pairs)

        def f():
            inst = None
            for i, (lt, rh) in enumerate(pairs):
                inst = nc.tensor.matmul(out, lt, rh, start=(i == 0), stop=(i == n - 1))
            return inst
        self.op('pe', f, r, w)

    def mms(self, groups, r=(), w=()):
        nc = self.nc

        def f():
            inst = None
            for out, pairs in groups:
                n = len(pairs)
                for i, (lt, rh) in enumerate(pairs):
                    inst = nc.tensor.matmul(out, lt, rh, start=(i == 0), stop=(i == n - 1))
            return inst
        self.op('pe', f, r, w)

    def transposes(self, items, ident, r=(), w=()):
        nc = self.nc

        def f():
            inst = None
            for out, in_ in items:
                inst = nc.tensor.transpose(out, in_, ident)
            return inst
        self.op('pe', f, r, w)


def hk(name, r0, nrows, tt, halo=False):
    ks = []
    for c in range(r0 // 128, (r0 + nrows - 1) // 128 + 1):
        ks.append((name, c, tt))
        if halo and tt > 0:
            ks.append((name, c, tt - 1))
    return ks


USED = []


def build(stop_after=None, dbg=(), tlen=4096, layers=(0, 1)):
    global T, NT
    T = tlen
    NT = T // TT
    del USED[:]
    nc = bass.Bass("TRN2", target_bir_lowering=False)
    SHAPES = {'xT': [DM, T], 'w_in': [2, DM, NIN], 'c128': [2, 128, N128], 'c64': [2, 64, N64], 'b_v': [2, 512],
              'gla_wup': [2, 16, 256], 'lru_w_r': [2, 8, 64, 64], 'lru_w_i': [2, 8, 64, 64], 'rwkv_w2': [2, 64, 512],
              'rwkv_a2': [2, 64, 512], 'rwkv_g2': [2, 160, 512], 'p_gla': [2, 512, DM], 'p_lru': [2, 512, DM],
              'p_rwkv': [2, 512, DM], 'w_out': [2, DM, DM], 'ffn_w_gate': [DM, FD], 'ffn_w_up': [DM, FD],
              'ffn_w_down': [FD, DM], 'moe_w_router': [DM, NE], 'moe_w_gate': [NE, DM, FE], 'moe_w_up': [NE, DM, FE],
              'moe_w_down': [NE, FE, DM]}

    class Lazy(dict):
        def __missing__(self, name):
            ap = nc.dram_tensor(name, SHAPES[name], F32, kind="ExternalInput").ap()
            self[name] = ap
            USED.append(name)
            return ap
    IN = Lazy()
    outT = nc.dram_tensor('outT', [DM, T], F32, kind="ExternalOutput").ap()

    SC = {}

    def dsc(name, shape, dt=F32):
        kind = "ExternalOutput" if name in dbg else "Internal"
        SC[name] = nc.dram_tensor(name, shape, dt, kind=kind).ap()
    dsc('hT', [NMIXP, T])
    dsc('vtok', [T, 512])
    dsc('ogT', [512, T], BF16)
    dsc('olT', [512, T], BF16)
    dsc('orT', [512, T], BF16)
    dsc('x1T', [DM, T])
    dsc('x2T', [DM, T])
    dsc('wb_in', [2, DM, NIN], BF16)
    dsc('wb_pg', [2, 512, DM], BF16)
    dsc('wb_pl', [2, 512, DM], BF16)
    dsc('wb_pr', [2, 512, DM], BF16)
    dsc('wb_out', [2, DM, DM], BF16)
    dsc('wb_fg', [DM, FD], BF16)
    dsc('wb_fu', [DM, FD], BF16)
    dsc('wb_fd', [FD, DM], BF16)
    dsc('wb_mg', [NE, DM, FE], BF16)
    dsc('wb_mu', [NE, DM, FE], BF16)
    dsc('wb_md', [NE, FE, DM], BF16)

    with ExitStack() as es:
        p = Prog(nc, es)
        _build_body(nc, p, IN, SC, outT, stop_after, layers)
        p.barrier()
        print('program: ninst', p.ninst, 'nwait', p.nwait)
    return nc


def flat128(ap2d_elems, ap):
    return ap


def conv_weights(nc, p, pairs):
    CW = 7168
    with p.scope() as st:
        fb = [p.sb(st, 'cvf', [128, CW], F32) for _ in range(2)]
        bb = [p.sb(st, 'cvb', [128, CW], BF16) for _ in range(2)]
        rnd = 0
        engs = ['pool', 'act', 'dve']
        for key, src, dst, nel in pairs:
            M = nel // 128
            s2 = src.rearrange("(p m) -> p m", p=128)
            d2 = dst.rearrange("(p m) -> p m", p=128)
            c0 = 0
            while c0 < M:
                cw = min(CW, M - c0)
                f, b = fb[rnd % 2], bb[rnd % 2]
                p.dma('sp', f[:, 0:cw], s2[:, c0:c0 + cw], w=[f])
                p.copy(b[:, 0:cw], f[:, 0:cw], r=[f], w=[b], eng=engs[rnd % 3])
                p.dma('act', d2[:, c0:c0 + cw], b[:, 0:cw], r=[b], w=[('cv', rnd)])
                c0 += cw
                rnd += 1


class ConvStream:
    CW = 1792

    def __init__(self, nc, p, st, pairs):
        self.p = p
        CW = self.CW
        fb = [p.sb(st, 'cvf2', [128, CW]) for _ in range(2)]
        bb = [p.sb(st, 'cvb2', [128, CW], BF16) for _ in range(2)]

        def rec():
            rnd = 0
            for key, src, dst, nel in pairs:
                M = nel // 128
                assert M % CW == 0
                s2 = src.rearrange("(p m) -> p m", p=128)
                d2 = dst.rearrange("(p m) -> p m", p=128)
                for c0 in range(0, M, CW):
                    f, b = fb[rnd % 2], bb[rnd % 2]
                    p.dma('sp', f[:, :], s2[:, c0:c0 + CW], w=[f])
                    p.copy(b[:, :], f[:, :], r=[f], w=[b], eng='act')
                    p.dma('sp', d2[:, c0:c0 + CW], b[:, :], r=[b], w=[('cv2', rnd)])
                    rnd += 1
        self.ops = p.record(rec)
        self.pos = 0

    def take(self, steps_left):
        left = len(self.ops) - self.pos
        if left <= 0:
            return []
        rounds = -(-(left // 3) // max(1, steps_left))
        n = min(left, rounds * 3)
        out = self.ops[self.pos:self.pos + n]
        self.pos += n
        return out

    def flush(self):
        rest = self.ops[self.pos:]
        self.pos = len(self.ops)
        if rest:
            self.p.play([rest])


def _build_body(nc, p, IN, SC, outT, stop_after, layers):
    def fl(ap):
        nd = len(ap.shape)
        if nd == 2:
            return ap.rearrange("a b -> (a b)")
        if nd == 3:
            return ap.rearrange("a b c -> (a b c)")
        return ap

    pairs = []
    for l in layers:
        pairs.append((('wb_in', l), fl(IN['w_in'][l]), fl(SC['wb_in'][l]), DM * NIN))
        pairs.append((('wb_pg', l), fl(IN['p_gla'][l]), fl(SC['wb_pg'][l]), 512 * DM))
        pairs.append((('wb_pl', l), fl(IN['p_lru'][l]), fl(SC['wb_pl'][l]), 512 * DM))
        pairs.append((('wb_pr', l), fl(IN['p_rwkv'][l]), fl(SC['wb_pr'][l]), 512 * DM))
        pairs.append((('wb_out', l), fl(IN['w_out'][l]), fl(SC['wb_out'][l]), DM * DM))
    if 0 in layers:
        pairs.append((('wb_fg', 0), fl(IN['ffn_w_gate']), fl(SC['wb_fg']), DM * FD))
        pairs.append((('wb_fu', 0), fl(IN['ffn_w_up']), fl(SC['wb_fu']), DM * FD))
        pairs.append((('wb_fd', 0), fl(IN['ffn_w_down']), fl(SC['wb_fd']), DM * FD))
    conv_weights(nc, p, pairs)
    es_conv = ExitStack()
    cstream = None
    if 1 in layers:
        mpairs = [(('wb_mg', 0), fl(IN['moe_w_gate']), fl(SC['wb_mg']), NE * DM * FE),
                  (('wb_mu', 0), fl(IN['moe_w_up']), fl(SC['wb_mu']), NE * DM * FE),
                  (('wb_md', 0), fl(IN['moe_w_down']), fl(SC['wb_md']), NE * DM * FE)]
        cstream = ConvStream(nc, p, es_conv, mpairs)
    if stop_after == ('conv', 0):
        return
    for l in layers:
        xin = IN['xT'] if l == 0 else SC['x2T']
        phase1(nc, p, IN, SC, l, xin)
        if stop_after == ('p1', l):
            return
        if mixers(nc, p, IN, SC, l, stop_after, cstream if l == 0 else None):
            return
        if l == 0 and cstream is not None:
            cstream.flush()
            p.barrier()
            es_conv.close()
        last = (l == layers[-1]) and l == 1
        phase_tail(nc, p, IN, SC, l, xin, outT if last else SC['x2T'], 'outT' if last else 'x2T')
        if stop_after == ('tail', l):
            return


def phase1(nc, p, IN, SC, l, xin):
    with p.scope() as st:
        xb = p.sb(st, 'xb', [128, 8, T], BF16)
        xf = [p.sb(st, 'xf', [128, 8, TT], F32) for _ in range(2)]
        c128 = p.sb(st, 'c128', [128, N128])
        p.dma('sp', c128[:, :], IN['c128'][l], w=[c128])
        xin3 = xin.rearrange("(kc p) t -> p kc t", p=128)
        for tt in range(NT):
            f = xf[tt % 2]
            rkeys = [('x2T', c, tt) for c in range(8)] if l > 0 else []
            p.dma('sp', f[:, :, :], xin3[:, :, tt * TT:(tt + 1) * TT], r=rkeys, w=[f])
            p.copy(xb[:, :, tt * TT:(tt + 1) * TT], f[:, :, :], r=[f], w=[('xb', tt)], eng=['act', 'dve'][tt % 2])
        wg = [p.sb(st, 'wg', [128, 8, 512], BF16) for _ in range(2)]
        stg = [p.sb(st, 'stg', [128, TT], F32) for _ in range(4)]
        w3 = SC['wb_in'][l].rearrange("(kc p) n -> p kc n", p=128)
        si = 0
        ngroups = (NMIX + 511) // 512
        for g in range(ngroups):
            n0 = g * 512
            if n0 == V0:
                continue
            gw = min(512, NMIX - n0)
            W = wg[g % 2]
            p.dma('sp', W[:, :, 0:gw], w3[:, :, n0:n0 + gw], w=[W])
            for tt in range(NT):
                for j in range((gw + 127) // 128):
                    cw = min(128, gw - j * 128)
                    ps = p.psum()
                    p.mm(ps[0:cw, :], [(W[:, kc, j * 128:j * 128 + cw], xb[:, kc, tt * TT:(tt + 1) * TT]) for kc in range(8)],
                         r=[W, ('xb', tt)], w=[ps])
                    s = stg[si % 4]
                    si += 1
                    row = n0 + j * 128
                    col = C128['b_in'] + row // 128
                    p.act(s[0:cw, :], ps[0:cw, :], AF.Identity, bias=c128[0:cw, col:col + 1], r=[ps, c128], w=[s])
                    p.dma('sp', SC['hT'][row:row + cw, tt * TT:(tt + 1) * TT], s[0:cw, :], r=[s], w=[('hT', row // 128, tt)])
        bv = p.sb(st, 'bv', [128, 512])
        p.dma('sp', bv[:, :], IN['b_v'][l].partition_broadcast(128), w=[bv])
        W = wg[ngroups % 2]
        p.dma('sp', W[:, :, :], w3[:, :, V0:V0 + 512], w=[W])
        for tb in range(T // 128):
            ps = p.psum()
            p.mm(ps[:, :], [(xb[:, kc, tb * 128:(tb + 1) * 128], W[:, kc, :]) for kc in range(8)], r=[W, ('xb', tb // 4)], w=[ps])
            s = stg[si % 4]
            si += 1
            p.tt(s[:, :], ps[:, :], bv[:, :], ALU.add, r=[ps, bv], w=[s])
            p.dma('sp', SC['vtok'][tb * 128:(tb + 1) * 128, :], s[:, :], r=[s], w=[('vtok', tb)])


def make_in_maps(inputs):
    inp = {k: np.asarray(v) for k, v in inputs.items()}
    cc = [pack_cols(inp, l) for l in range(2)]
    shared = {
        'w_in': inp['w_in'],
        'c128': np.stack([cc[0][0], cc[1][0]]),
        'c64': np.stack([cc[0][1], cc[1][1]]),
        'b_v': np.ascontiguousarray(inp['b_in'][:, V0:V0 + 512]),
        'gla_wup': inp['gla_w_decay_up'],
        'lru_w_r': inp['lru_w_r'], 'lru_w_i': inp['lru_w_i'],
        'rwkv_w2': inp['rwkv_w2'], 'rwkv_a2': inp['rwkv_a2'], 'rwkv_g2': inp['rwkv_g2'],
        'p_gla': inp['p_gla'], 'p_lru': inp['p_lru'], 'p_rwkv': inp['p_rwkv'], 'w_out': inp['w_out'],
        'ffn_w_gate': inp['ffn_w_gate'][0], 'ffn_w_up': inp['ffn_w_up'][0], 'ffn_w_down': inp['ffn_w_down'][0],
        'moe_w_router': inp['moe_w_router'][0], 'moe_w_gate': inp['moe_w_gate'][0], 'moe_w_up': inp['moe_w_up'][0],
        'moe_w_down': inp['moe_w_down'][0],
    }
    shared = {k: np.ascontiguousarray(v, dtype=np.float32) for k, v in shared.items()}
    maps = []
    for b in range(8):
        m = {k: v for k, v in shared.items() if k in USED}
        m['xT'] = np.ascontiguousarray(inp['x'][b, :T].T)
        maps.append(m)
    return maps


def kernel(**inputs):
    nc = build()
    maps = make_in_maps(inputs)
    res = run_bass_kernel_spmd(nc, maps, core_ids=list(range(8)))
    out = np.stack([np.ascontiguousarray(res.results[b]['outT'].T) for b in range(8)])
    return out.astype(np.float32)


class Consts:
    pass


def make_consts(nc, p, st):
    c = Consts()
    c.ident = p.sb(st, 'ident', [64, 64])
    c.ones64 = p.sb(st, 'ones64', [64, 64])
    c.onesm128 = p.sb(st, 'onesm128', [128, 128])
    c.onesm64 = p.sb(st, 'onesm64', [64, 64])
    c.m01 = p.sb(st, 'm01', [64, TT])
    c.mge = p.sb(st, 'mge', [64, NCH, CH])
    c.mgt = p.sb(st, 'mgt', [64, NCH, CH])
    c.mlt = p.sb(st, 'mlt', [64, NCH, CH])
    c.id8 = p.sb(st, 'id8', [64, NCH, CH])
    ones8 = p.sb(st, 'ones8', [64, NCH, CH])
    p.op('pool', lambda: nc.gpsimd.memset(c.ones64[:, :], 1.0), w=[c.ones64])
    p.op('pool', lambda: nc.gpsimd.memset(c.onesm128[:, :], 1.0 / 128.0), w=[c.onesm128])
    p.op('pool', lambda: nc.gpsimd.memset(c.onesm64[:, :], 1.0 / 64.0), w=[c.onesm64])
    p.op('pool', lambda: nc.gpsimd.memset(ones8[:, :, :], 1.0), w=[ones8])
    p.op('pool', lambda: nc.gpsimd.memset(c.m01[:, :], 1.0), w=[c.m01])
    m3 = c.m01[:, :].rearrange("p (c t) -> p c t", t=CH)
    p.op('pool', lambda: nc.gpsimd.memset(m3[:, :, 0:1], 0.0), r=[c.m01], w=[c.m01])
    pat = [[0, NCH], [1, CH]]
    p.op('pool', lambda: nc.gpsimd.affine_select(out=c.mge[:, :, :], in_=ones8[:, :, :], pattern=pat, compare_op=ALU.is_ge,
                                                 fill=0.0, base=0, channel_multiplier=-1), r=[ones8], w=[c.mge])
    p.op('pool', lambda: nc.gpsimd.affine_select(out=c.mgt[:, :, :], in_=ones8[:, :, :], pattern=pat, compare_op=ALU.is_gt,
                                                 fill=0.0, base=0, channel_multiplier=-1), r=[ones8], w=[c.mgt])
    pat2 = [[0, NCH], [-1, CH]]
    p.op('pool', lambda: nc.gpsimd.affine_select(out=c.mlt[:, :, :], in_=ones8[:, :, :], pattern=pat2, compare_op=ALU.is_gt,
                                                 fill=0.0, base=0, channel_multiplier=1), r=[ones8], w=[c.mlt])
    p.tt(c.id8[:, :, :], c.mge[:, :, :], c.mgt[:, :, :], ALU.subtract, r=[c.mge, c.mgt], w=[c.id8])
    p.copy(c.ident[:, :], c.id8[:, 0, :], r=[c.id8], w=[c.ident])
    return c


def cview(t):
    return t[:, :].rearrange("p (c t) -> p c t", t=CH)


def fm_norm(nc, p, st, cst, ps, P, eps, gcol, bcol, tag, tiles=None):
    ones = cst.onesm128 if P == 128 else cst.onesm64
    if tiles is not None:
        o, osq, mean, msq = tiles
    else:
        o = p.sb(st, tag + 'o', [P, TT])
        osq = p.sb(st, tag + 'osq', [P, TT])
    p.copy(o[:, :], ps[0:P, :], r=[ps], w=[o], eng='act')
    p.act(osq[:, :], ps[0:P, :], AF.Square, r=[ps], w=[osq])
    psM = p.psum()
    p.mm(psM[0:P, :], [(ones[:, :], o[:, :])], r=[ones, o], w=[psM])
    psQ = p.psum()
    p.mm(psQ[0:P, :], [(ones[:, :], osq[:, :])], r=[ones, osq], w=[psQ])
    if tiles is None:
        mean = p.sb(st, tag + 'mean', [P, TT])
        msq = p.sb(st, tag + 'msq', [P, TT])
    p.copy(mean[:, :], psM[0:P, :], r=[psM], w=[mean], eng='act')
    p.act(msq[:, :], psM[0:P, :], AF.Square, r=[psM], w=[msq])
    var = osq
    p.tt(var[:, :], psQ[0:P, :], msq[:, :], ALU.subtract, r=[psQ, msq], w=[var])
    p.ts(var[:, :], var[:, :], 0.0, eps, ALU.max, ALU.add, r=[var], w=[var])
    p.act(msq[:, :], var[:, :], AF.Sqrt, r=[var], w=[msq])
    p.op('dve', lambda: nc.vector.reciprocal(out=var[:, :], in_=msq[:, :]), r=[msq], w=[var])
    p.tt(o[:, :], o[:, :], mean[:, :], ALU.subtract, r=[o, mean], w=[o])
    p.tt(o[:, :], o[:, :], var[:, :], ALU.mult, r=[o, var], w=[o])
    p.ts(o[:, :], o[:, :], gcol, bcol, ALU.mult, ALU.add, r=[o], w=[o])
    return o


def phase_gla(nc, p, IN, SC, l, cst, c128, c64, st0):
    p.ps_pool = [0, 1, 2, 3, 4]
    if True:
        wup = p.sb(st0, 'wup', [16, 256])
        p.dma('sp', wup[:, :], IN['gla_wup'][l], w=[wup])
        negbd = p.sb(st0, 'negbd', [64, 4])
        p.ts(negbd[:, :], c64[:, C64['gla_bd']:C64['gla_bd'] + 4], -1.0, None, ALU.mult, r=[c64], w=[negbd])
        S = [p.sb(st0, 'S%d' % h, [64, NCH + 1, 128]) for h in range(4)]
        for h in range(4):
            p.op('pool', lambda h=h: nc.gpsimd.memset(S[h][:, 0, :], 0.0), w=[S[h]])
        G = {}
        for nm, shp, dt in [('dec', [16, TT], F32), ('q', [64, TT], F32), ('k', [64, TT], F32), ('v', [64, NCH, 128], F32),
                            ('r', [128, TT], F32), ('l1', [64, TT], F32), ('cum', [64, TT], F32), ('E', [64, TT], F32),
                            ('qd', [64, TT], F32), ('ki', [64, TT], F32), ('ke', [64, TT], F32), ('D', [64, NCH, CH], F32),
                            ('dend', [64, NCH, 1], F32), ('keT', [64, TT], F32), ('sc', [64, TT], F32), ('dst', [64, NCH, 128], F32),
                            ('no', [128, TT], F32), ('nosq', [128, TT], F32), ('nmean', [128, TT], F32), ('nmsq', [128, TT], F32),
                            ('og', [128, TT], BF16)]:
            G[nm] = p.sb(st0, 'g_' + nm, shp, dt)
        for tt in range(NT):
            tsl = slice(tt * TT, (tt + 1) * TT)
            for h in range(4):
                if True:
                    st = None
                    dec = G['dec']
                    p.dma('sp', dec[:, :], SC['hT'][DEC0:DEC0 + 16, tsl], r=hk('hT', DEC0, 16, tt), w=[dec])
                    q = G['q']
                    k = G['k']
                    v = G['v']
                    r = G['r']
                    p.dma('sp', q[:, :], SC['hT'][Q0 + h * 64:Q0 + (h + 1) * 64, tsl], r=hk('hT', Q0 + h * 64, 64, tt), w=[q])
                    p.dma('sp', k[:, :], SC['hT'][K0 + h * 64:K0 + (h + 1) * 64, tsl], r=hk('hT', K0 + h * 64, 64, tt), w=[k])
                    p.dma('sp', v[:, :, :], SC['vtok'][tsl, h * 128:(h + 1) * 128].rearrange("(c t) e -> t c e", t=CH),
                          r=[('vtok', tb) for tb in range(tt * 4, tt * 4 + 4)], w=[v])
                    p.dma('sp', r[:, :], SC['hT'][R0 + h * 128:R0 + (h + 1) * 128, tsl], r=hk('hT', R0 + h * 128, 128, tt), w=[r])
                    ps = p.psum()
                    p.mm(ps[0:64, :], [(wup[:, h * 64:(h + 1) * 64], dec[:, :])], r=[wup, dec], w=[ps])
                    l1 = G['l1']
                    p.act(l1[:, :], ps[0:64, :], AF.Exp, bias=negbd[:, h:h + 1], scale=-1.0, r=[ps, negbd], w=[l1])
                    p.act(l1[:, :], l1[:, :], AF.Ln, bias=1.0, scale=1.0, r=[l1], w=[l1])
                    cum = G['cum']
                    p.op('dve', lambda: nc.vector.tensor_tensor_scan(out=cum[:, :], data0=cst.m01[:, :], data1=l1[:, :], initial=0.0,
                                                                     op0=ALU.mult, op1=ALU.add), r=[cst.m01, l1], w=[cum])
                    E = G['E']
                    qd = G['qd']
                    ki = G['ki']
                    ke = G['ke']
                    p.act(E[:, :], cum[:, :], AF.Exp, scale=-1.0 / 16.0, r=[cum], w=[E])
                    p.stt(qd[:, :], q[:, :], 0.125, E[:, :], ALU.mult, ALU.mult, r=[q, E], w=[qd])
                    p.act(E[:, :], cum[:, :], AF.Exp, scale=1.0 / 16.0, r=[cum], w=[E])
                    p.tt(ki[:, :], k[:, :], E[:, :], ALU.mult, r=[k, E], w=[ki])
                    c3 = cview(cum)
                    D = G['D']
                    p.tt(D[:, :, :], c3[:, :, CH - 1:CH].to_broadcast([64, NCH, CH]), c3, ALU.subtract, r=[cum], w=[D])
                    p.act(D[:, :, :], D[:, :, :], AF.Exp, scale=-1.0 / 16.0, r=[D], w=[D])
                    p.tt(ke[:, :], k[:, :], D[:, :, :].rearrange("p c t -> p (c t)"), ALU.mult, r=[k, D], w=[ke])
                    dend = G['dend']
                    p.act(dend[:, :, :], c3[:, :, CH - 1:CH], AF.Exp, scale=-1.0 / 16.0, r=[cum], w=[dend])
                    psT = p.psum()
                    p.transposes([(psT[0:64, c * 64:(c + 1) * 64], ke[:, c * 64:(c + 1) * 64]) for c in range(NCH)], cst.ident[:, :],
                                 r=[ke, cst.ident], w=[psT])
                    keT = G['keT']
                    p.copy(keT[:, :], psT[0:64, :], r=[psT], w=[keT], eng='act')
                    psS = p.psum()
                    p.mms([(psS[0:64, c * 64:(c + 1) * 64], [(ki[:, c * 64:(c + 1) * 64], qd[:, c * 64:(c + 1) * 64])]) for c in range(NCH)],
                          r=[ki, qd], w=[psS])
                    sc = G['sc']
                    p.tt(sc[:, :], psS[0:64, :], cst.mge[:, :, :].rearrange("p c t -> p (c t)"), ALU.mult, r=[psS, cst.mge], w=[sc])
                    dst = G['dst']
                    for half in range(2):
                        psD = p.psum()
                        p.mms([(psD[0:64, cc * 128:(cc + 1) * 128], [(keT[:, (half * 4 + cc) * 64:(half * 4 + cc + 1) * 64], v[:, half * 4 + cc, :])])
                               for cc in range(4)], r=[keT, v], w=[psD])
                        p.copy(dst[:, half * 4:half * 4 + 4, :].rearrange("p c e -> p (c e)"), psD[0:64, :], r=[psD], w=[dst],
                               eng=['act', 'dve'][half])
                    for c in range(NCH):
                        p.stt(S[h][:, c + 1, :], S[h][:, c, :], dend[:, c, :], dst[:, c, :], ALU.mult, ALU.add, r=[S[h], dend, dst], w=[S[h]])
                    psO = p.psum()
                    p.mms([(psO[:, c * 64:(c + 1) * 64], [(v[:, c, :], sc[:, c * 64:(c + 1) * 64]), (S[h][:, c, :], qd[:, c * 64:(c + 1) * 64])])
                           for c in range(NCH)], r=[v, sc, S[h], qd], w=[psO])
                    p.copy(S[h][:, 0, :], S[h][:, NCH, :], r=[S[h]], w=[S[h]], eng='pool')
                    y = fm_norm(nc, p, st, cst, psO, 128, 1e-5, c128[:, C128['gla_ng'] + h:C128['gla_ng'] + h + 1],
                                c128[:, C128['gla_nb'] + h:C128['gla_nb'] + h + 1], 'gn', tiles=(G['no'], G['nosq'], G['nmean'], G['nmsq']))
                    p.act(r[:, :], r[:, :], AF.Silu, r=[r], w=[r])
                    og = G['og']
                    p.tt(og[:, :], y[:, :], r[:, :], ALU.mult, r=[y, r], w=[og])
                    p.dma('sp', SC['ogT'][h * 128:(h + 1) * 128, tsl], og[:, :], r=[og], w=[('ogT', h, tt)])


def phase_lru(nc, p, IN, SC, l, cst, c128, c64, st0):
    p.ps_pool = [5, 6, 7]
    if True:
        wr = p.sb(st0, 'wr', [128, 4, 128])
        wi = p.sb(st0, 'wi', [128, 4, 128])
        for wt, nm in ((wr, 'lru_w_r'), (wi, 'lru_w_i')):
            p.op('pool', lambda wt=wt: nc.gpsimd.memset(wt[:, :, :], 0.0), w=[wt])
            for g in range(8):
                o = (g % 2) * 64
                p.dma('sp', wt[o:o + 64, g // 2, o:o + 64], IN[nm][l, g], r=[wt], w=[wt])
        cex = p.sb(st0, 'cex', [128, 4])
        lam = c128[:, C128['lam']:C128['lam'] + 4]
        p.act(cex[:, :], lam, AF.Exp, scale=-1.0, r=[c128], w=[cex])
        p.act(cex[:, :], cex[:, :], AF.Ln, bias=1.0, r=[cex], w=[cex])
        p.ts(cex[:, :], cex[:, :], -8.0, None, ALU.mult, r=[cex], w=[cex])
        hprev = p.sb(st0, 'hprev', [128, 4])
        p.op('pool', lambda: nc.gpsimd.memset(hprev[:, :], 0.0), w=[hprev])
        LT = {}
        for nm, shp, dt in [('xh', [128, TT + 3], F32), ('gate', [128, TT], F32), ('xc', [128, TT], F32), ('a', [128, TT], F32),
                            ('ii', [128, TT], F32), ('ml', [128, TT], F32), ('hh', [128, TT], F32), ('ol', [128, TT], BF16)]:
            LT[nm] = p.sb(st0, 'l_' + nm, shp, dt)
        for tt in range(NT):
            t0 = tt * TT
            tsl = slice(t0, t0 + TT)
            for g in range(4):
                if True:
                    xh = LT['xh']
                    rows = slice(LX0 + g * 128, LX0 + (g + 1) * 128)
                    if tt == 0:
                        p.op('pool', lambda: nc.gpsimd.memset(xh[:, 0:3], 0.0), w=[xh])
                        p.dma('sp', xh[:, 3:], SC['hT'][rows, tsl], r=hk('hT', LX0 + g * 128, 128, tt), w=[xh])
                    else:
                        p.dma('sp', xh[:, :], SC['hT'][rows, t0 - 3:t0 + TT], r=hk('hT', LX0 + g * 128, 128, tt, True), w=[xh])
                    gate = LT['gate']
                    p.dma('sp', gate[:, :], SC['hT'][LG0 + g * 128:LG0 + (g + 1) * 128, tsl], r=hk('hT', LG0 + g * 128, 128, tt), w=[gate])
                    xc = LT['xc']
                    cw = C128['conv_w']
                    p.ts(xc[:, :], xh[:, 0:TT], c128[:, cw + g:cw + g + 1], c128[:, C128['conv_b'] + g:C128['conv_b'] + g + 1],
                         ALU.mult, ALU.add, r=[xh, c128], w=[xc])
                    for j in range(1, 4):
                        p.stt(xc[:, :], xh[:, j:j + TT], c128[:, cw + j * 4 + g:cw + j * 4 + g + 1], xc[:, :], ALU.mult, ALU.add,
                              r=[xh, xc, c128], w=[xc])
                    psr = p.psum()
                    p.mm(psr[:, :], [(wr[:, g, :], xc[:, :])], r=[wr, xc], w=[psr])
                    psi = p.psum()
                    p.mm(psi[:, :], [(wi[:, g, :], xc[:, :])], r=[wi, xc], w=[psi])
                    a = LT['a']
                    ii = LT['ii']
                    ml = LT['ml']
                    p.act(a[:, :], psr[:, :], AF.Sigmoid, bias=c128[:, C128['b_r'] + g:C128['b_r'] + g + 1], r=[psr, c128], w=[a])
                    p.act(ii[:, :], psi[:, :], AF.Sigmoid, bias=c128[:, C128['b_i'] + g:C128['b_i'] + g + 1], r=[psi, c128], w=[ii])
                    p.act(a[:, :], a[:, :], AF.Exp, scale=cex[:, g:g + 1], r=[a, cex], w=[a])
                    p.act(ml[:, :], a[:, :], AF.Square, r=[a], w=[ml])
                    p.act(ml[:, :], ml[:, :], AF.Sqrt, bias=1.0, scale=-1.0, r=[ml], w=[ml])
                    p.tt(ii[:, :], ii[:, :], xc[:, :], ALU.mult, r=[ii, xc], w=[ii])
                    p.tt(ii[:, :], ii[:, :], ml[:, :], ALU.mult, r=[ii, ml], w=[ii])
                    hh = LT['hh']
                    p.op('dve', lambda g=g: nc.vector.tensor_tensor_scan(out=hh[:, :], data0=a[:, :], data1=ii[:, :], initial=hprev[:, g:g + 1],
                                                                     op0=ALU.mult, op1=ALU.add), r=[a, ii, hprev], w=[hh])
                    p.copy(hprev[:, g:g + 1], hh[:, TT - 1:TT], r=[hh], w=[hprev], eng='dve')
                    p.act(ml[:, :], gate[:, :], AF.Square, r=[gate], w=[ml])
                    p.ts(ml[:, :], ml[:, :], 0.044715, 1.0, ALU.mult, ALU.add, r=[ml], w=[ml])
                    p.tt(ml[:, :], ml[:, :], gate[:, :], ALU.mult, r=[ml, gate], w=[ml])
                    p.act(ml[:, :], ml[:, :], AF.Sigmoid, scale=1.5957691216057308, r=[ml], w=[ml])
                    p.tt(ml[:, :], ml[:, :], gate[:, :], ALU.mult, r=[ml, gate], w=[ml])
                    ol = LT['ol']
                    p.tt(ol[:, :], ml[:, :], hh[:, :], ALU.mult, r=[ml, hh], w=[ol])
                    p.dma('sp', SC['olT'][g * 128:(g + 1) * 128, tsl], ol[:, :], r=[ol], w=[('olT', g, tt)])


def phase_rwkv(nc, p, IN, SC, l, cst, c128, c64):
    YB = 7
    p.ps_pool = [0, 1, 2, 3, 4, 5, 6]
    with p.scope() as st0:
        w2 = p.sb(st0, 'w2', [64, 512])
        a2 = p.sb(st0, 'a2', [64, 512])
        g2a = p.sb(st0, 'g2a', [128, 512])
        g2b = p.sb(st0, 'g2b', [32, 512])
        p.dma('sp', w2[:, :], IN['rwkv_w2'][l], w=[w2])
        p.dma('sp', a2[:, :], IN['rwkv_a2'][l], w=[a2])
        p.dma('sp', g2a[:, :], IN['rwkv_g2'][l, 0:128, :], w=[g2a])
        p.dma('sp', g2b[:, :], IN['rwkv_g2'][l, 128:160, :], w=[g2b])
        nmu = C64['mu_a'] + 1 - C64['mu_r']
        omu = p.sb(st0, 'omu', [64, N64])
        p.ts(omu[:, :], c64[:, :], -1.0, 1.0, ALU.mult, ALU.add, r=[c64], w=[omu])
        omug = p.sb(st0, 'omug', [128, 2])
        p.ts(omug[:, :], c128[:, C128['mu_g']:C128['mu_g'] + 2], -1.0, 1.0, ALU.mult, ALU.add, r=[c128], w=[omug])
        Tst = [p.sb(st0, 'Tst%d' % h, [64, 64]) for h in range(8)]
        for h in range(8):
            p.op('pool', lambda: nc.gpsimd.memset(Tst[h][:, :], 0.0), w=[Tst[h]])
        psY = p.ps[YB]

        def load_shift(st, tt, row0, nrows, mu_ap, omu_ap, rd, tag):
            t0 = tt * TT
            raw = p.sb(st, tag + 'raw', [nrows, TT + 1])
            if tt == 0:
                p.op('pool', lambda: nc.gpsimd.memset(raw[:, 0:1], 0.0), w=[raw])
                p.dma('sp', raw[:, 1:], SC['hT'][row0:row0 + nrows, 0:TT], r=hk('hT', row0, nrows, tt), w=[raw])
            else:
                p.dma('sp', raw[:, :], SC['hT'][row0:row0 + nrows, t0 - 1:t0 + TT], r=hk('hT', row0, nrows, tt, True), w=[raw])
            out = p.sb(st, tag, [nrows, TT])
            p.ts(out[:, :], raw[:, 1:TT + 1], omu_ap, None, ALU.mult, r=[raw] + rd, w=[out])
            p.stt(out[:, :], raw[:, 0:TT], mu_ap, out[:, :], ALU.mult, ALU.add, r=[raw, out] + rd, w=[out])
            return out

        def col(name, h=0):
            return c64[:, C64[name] + h:C64[name] + h + 1]

        def ocol(name, h=0):
            return omu[:, C64[name] + h:C64[name] + h + 1]

        for tt in range(NT):
            tsl = slice(tt * TT, (tt + 1) * TT)
            with p.scope() as stt_:
                wl = load_shift(stt_, tt, RWW, 64, col('mu_w'), ocol('mu_w'), [c64, omu], 'wl')
                al = load_shift(stt_, tt, RWA, 64, col('mu_a'), ocol('mu_a'), [c64, omu], 'al')
                mg = C128['mu_g']
                gl1 = load_shift(stt_, tt, RWG, 128, c128[:, mg:mg + 1], omug[:, 0:1], [c128, omug], 'gl1')
                gl2 = load_shift(stt_, tt, RWG + 128, 32, c128[0:32, mg + 1:mg + 2], omug[0:32, 1:2], [c128, omug], 'gl2')
                p.act(wl[:, :], wl[:, :], AF.Tanh, r=[wl], w=[wl])
                p.act(gl1[:, :], gl1[:, :], AF.Sigmoid, r=[gl1], w=[gl1])
                p.act(gl2[:, :], gl2[:, :], AF.Sigmoid, r=[gl2], w=[gl2])
                for h in range(8):
                    hs = slice(h * 64, (h + 1) * 64)
                    with p.scope() as st:
                        r = load_shift(st, tt, RWR + h * 64, 64, col('mu_r', h), ocol('mu_r', h), [c64, omu], 'r')
                        k = load_shift(st, tt, RWK + h * 64, 64, col('mu_k', h), ocol('mu_k', h), [c64, omu], 'k')
                        v = load_shift(st, tt, RWV + h * 64, 64, col('mu_v', h), ocol('mu_v', h), [c64, omu], 'v')

                        def new(tag, shape=None, dt=F32):
                            return p.sb(st, tag, shape or [64, TT], dt)
                        ps = p.psum()
                        p.mm(ps[0:64, :], [(w2[:, hs], wl[:, :])], r=[w2, wl], w=[ps])
                        sgm = new('sgm')
                        p.act(sgm[:, :], ps[0:64, :], AF.Sigmoid, bias=col('w0', h), r=[ps, c64], w=[sgm])
                        cum = new('cum')
                        p.op('dve', lambda: nc.vector.tensor_tensor_scan(out=cum[:, :], data0=cst.m01[:, :], data1=sgm[:, :], initial=0.0,
                                                                         op0=ALU.mult, op1=ALU.add), r=[cst.m01, sgm], w=[cum])
                        ps = p.psum()
                        p.mm(ps[0:64, :], [(a2[:, hs], al[:, :])], r=[a2, al], w=[ps])
                        ag = new('ag')
                        p.act(ag[:, :], ps[0:64, :], AF.Sigmoid, bias=col('a0', h), r=[ps, c64], w=[ag])
                        ps = p.psum()
                        p.mm(ps[0:64, :], [(g2a[:, hs], gl1[:, :]), (g2b[:, hs], gl2[:, :])], r=[g2a, g2b, gl1, gl2], w=[ps])
                        gh = new('gh')
                        p.copy(gh[:, :], ps[0:64, :], r=[ps], w=[gh], eng='act')
                        kk = new('kk')
                        tmp = new('tmp')
                        p.ts(kk[:, :], k[:, :], col('k_k', h), None, ALU.mult, r=[k, c64], w=[kk])
                        p.act(tmp[:, :], kk[:, :], AF.Square, r=[kk], w=[tmp])
                        ps = p.psum()
                        p.mm(ps[0:64, :], [(cst.ones64[:, :], tmp[:, :])], r=[cst.ones64, tmp], w=[ps])
                        p.act(tmp[:, :], ps[0:64, :], AF.Sqrt, r=[ps], w=[tmp])
                        p.ts(tmp[:, :], tmp[:, :], 1e-12, None, ALU.max, r=[tmp], w=[tmp])
                        p.op('dve', lambda: nc.vector.reciprocal(out=tmp[:, :], in_=tmp[:, :]), r=[tmp], w=[tmp])
                        p.tt(kk[:, :], kk[:, :], tmp[:, :], ALU.mult, r=[kk, tmp], w=[kk])
                        p.ts(tmp[:, :], ag[:, :], col('k_a', h), ocol('k_a', h), ALU.mult, ALU.add, r=[ag, c64, omu], w=[tmp])
                        k2 = new('k2')
                        p.tt(k2[:, :], k[:, :], tmp[:, :], ALU.mult, r=[k, tmp], w=[k2])
                        bv = new('bv')
                        p.tt(bv[:, :], kk[:, :], ag[:, :], ALU.mult, r=[kk, ag], w=[bv])
                        E = new('E')
                        rt, kt, bt, at, kh, bh = new('rt'), new('kt'), new('bt'), new('at'), new('kh'), new('bh')
                        p.act(E[:, :], cum[:, :], AF.Exp, scale=-C0, r=[cum], w=[E])
                        p.tt(rt[:, :], r[:, :], E[:, :], ALU.mult, r=[r, E], w=[rt])
                        p.act(E[:, :], cum[:, :], AF.Exp, scale=C0, r=[cum], w=[E])
                        p.tt(kt[:, :], k2[:, :], E[:, :], ALU.mult, r=[k2, E], w=[kt])
                        p.tt(bt[:, :], bv[:, :], E[:, :], ALU.mult, r=[bv, E], w=[bt])
                        p.tt(tmp[:, :], cum[:, :], sgm[:, :], ALU.subtract, r=[cum, sgm], w=[tmp])
                        p.act(E[:, :], tmp[:, :], AF.Exp, scale=-C0, r=[tmp], w=[E])
                        p.stt(at[:, :], kk[:, :], -1.0, E[:, :], ALU.mult, ALU.mult, r=[kk, E], w=[at])
                        c3 = cview(cum)
                        D = new('D', [64, NCH, CH])
                        p.tt(D[:, :, :], c3[:, :, CH - 1:CH].to_broadcast([64, NCH, CH]), c3, ALU.subtract, r=[cum], w=[D])
                        p.act(D[:, :, :], D[:, :, :], AF.Exp, scale=-C0, r=[D], w=[D])
                        Df = D[:, :, :].rearrange("p c t -> p (c t)")
                        p.tt(kh[:, :], k2[:, :], Df, ALU.mult, r=[k2, D], w=[kh])
                        p.tt(bh[:, :], bv[:, :], Df, ALU.mult, r=[bv, D], w=[bh])
                        WC = new('WC', [64, NCH, 1])
                        p.act(WC[:, :, :], c3[:, :, CH - 1:CH], AF.Exp, scale=-C0, r=[cum], w=[WC])
                        p.stt(tmp[:, :], r[:, :], col('r_k', h), k2[:, :], ALU.mult, ALU.mult, r=[r, k2, c64], w=[tmp])
                        ps = p.psum()
                        p.mm(ps[0:64, :], [(cst.ones64[:, :], tmp[:, :])], r=[cst.ones64, tmp], w=[ps])
                        bonus = new('bonus')
                        p.tt(bonus[:, :], ps[0:64, :], v[:, :], ALU.mult, r=[ps, v], w=[bonus])
                        toks = []
                        for src, tag in ((v, 'vT'), (kh, 'khT'), (bh, 'bhT')):
                            psT = p.psum()
                            p.transposes([(psT[0:64, c * 64:(c + 1) * 64], src[:, c * 64:(c + 1) * 64]) for c in range(NCH)], cst.ident[:, :],
                                         r=[src, cst.ident], w=[psT])
                            d = new(tag)
                            p.copy(d[:, :], psT[0:64, :], r=[psT], w=[d], eng='act')
                            toks.append(d)
                        vT, khT, bhT = toks

                        def amat(lt, rh, mask, tag):
                            psA = p.psum()
                            p.mms([(psA[0:64, c * 64:(c + 1) * 64], [(lt[:, c * 64:(c + 1) * 64], rh[:, c * 64:(c + 1) * 64])]) for c in range(NCH)],
                                  r=[lt, rh], w=[psA])
                            d = new(tag)
                            p.tt(d[:, :], psA[0:64, :], mask[:, :, :].rearrange("p c t -> p (c t)"), ALU.mult, r=[psA, mask], w=[d])
                            return d
                        AabT = amat(bt, at, cst.mgt, 'AabT')
                        ArbT = amat(bt, rt, cst.mge, 'ArbT')
                        AakT = amat(kt, at, cst.mgt, 'AakT')
                        ArkT = amat(kt, rt, cst.mge, 'ArkT')
                        Aab = amat(at, bt, cst.mlt, 'Aab')
                        P_, Q_ = Aab, AabT
                        N_ = new('N0')
                        p.tt(N_[:, :], Q_[:, :], cst.id8[:, :, :].rearrange("p c t -> p (c t)"), ALU.add, r=[Q_, cst.id8], w=[N_])
                        for lev in range(5):
                            psP = p.psum()
                            p.mms([(psP[0:64, c * 64:(c + 1) * 64], [(Q_[:, c * 64:(c + 1) * 64], P_[:, c * 64:(c + 1) * 64])]) for c in range(NCH)],
                                  r=[P_, Q_], w=[psP])
                            if lev < 4:
                                psQ = p.psum()
                                p.mms([(psQ[0:64, c * 64:(c + 1) * 64], [(P_[:, c * 64:(c + 1) * 64], Q_[:, c * 64:(c + 1) * 64])]) for c in range(NCH)],
                                      r=[P_, Q_], w=[psQ])
                            P2 = new('P%d' % lev)
                            p.copy(P2[:, :], psP[0:64, :], r=[psP], w=[P2], eng='act')
                            if lev < 4:
                                Q2 = new('Q%d' % lev)
                                p.copy(Q2[:, :], psQ[0:64, :], r=[psQ], w=[Q2], eng='dve')
                            psN = p.psum()
                            p.mms([(psN[0:64, c * 64:(c + 1) * 64], [(P2[:, c * 64:(c + 1) * 64], N_[:, c * 64:(c + 1) * 64])]) for c in range(NCH)],
                                  r=[P2, N_], w=[psN])
                            N2 = new('N%d' % (lev + 1))
                            p.tt(N2[:, :], N_[:, :], psN[0:64, :], ALU.add, r=[N_, psN], w=[N2])
                            P_, N_ = P2, N2
                            if lev < 4:
                                Q_ = Q2
                        X = new('X', [64, 64])
                        U = new('U', [64, 64])
                        for c in range(NCH):
                            cs = slice(c * 64, (c + 1) * 64)
                            psX = p.psum()
                            p.mm(psX[0:64, 0:64], [(at[:, cs], Tst[h][:, :]), (AakT[:, cs], vT[:, cs])], r=[at, Tst[h], AakT, vT], w=[psX])
                            p.copy(X[:, :], psX[0:64, 0:64], r=[psX], w=[X], eng='act')
                            psU = p.psum()
                            p.mm(psU[0:64, 0:64], [(N_[:, cs], X[:, :])], r=[N_, X], w=[psU])
                            p.copy(U[:, :], psU[0:64, 0:64], r=[psU], w=[U], eng='dve')
                            p.mm(psY[0:64, cs], [(Tst[h][:, :], rt[:, cs]), (U[:, :], ArbT[:, cs]), (vT[:, cs], ArkT[:, cs])],
                                 r=[Tst[h], rt, U, ArbT, vT, ArkT], w=[psY])
                            psT2 = p.psum()
                            p.mm(psT2[0:64, 0:64], [(bhT[:, cs], U[:, :]), (khT[:, cs], vT[:, cs])], r=[bhT, U, khT, vT], w=[psT2])
                            p.stt(Tst[h][:, :], Tst[h][:, :], WC[:, c, :], psT2[0:64, 0:64], ALU.mult, ALU.add, r=[Tst[h], WC, psT2], w=[Tst[h]])
                        y = fm_norm(nc, p, st, cst, psY, 64, 64e-5, col('lnx_g', h), col('lnx_b', h), 'rn')
                        p.tt(y[:, :], y[:, :], bonus[:, :], ALU.add, r=[y, bonus], w=[y])
                        orw = new('orw', [64, TT], BF16)
                        p.tt(orw[:, :], y[:, :], gh[:, :], ALU.mult, r=[y, gh], w=[orw])
                        p.dma('sp', SC['orT'][h * 64:(h + 1) * 64, tsl], orw[:, :], r=[orw], w=[('orT', h, tt)])
    p.ps_pool = list(range(8))


def mixers(nc, p, IN, SC, l, stop_after, cstream=None):
    with p.scope() as st:
        cst = make_consts(nc, p, st)
        c128 = p.sb(st, 'c128m', [128, N128])
        c64 = p.sb(st, 'c64m', [64, N64])
        p.dma('sp', c128[:, :], IN['c128'][l], w=[c128])
        p.dma('sp', c64[:, :], IN['c64'][l], w=[c64])
        with p.scope() as stg:
            Lg = p.record(lambda: phase_gla(nc, p, IN, SC, l, cst, c128, c64, stg))
            Ll = p.record(lambda: phase_lru(nc, p, IN, SC, l, cst, c128, c64, stg))
            p.ps_pool = list(range(8))
            p.play([Lg, Ll])
        if stop_after == ('lru', l):
            return True
        phase_rwkv3(nc, p, IN, SC, l, cst, c128, c64, cstream)
        if stop_after == ('rwkv', l):
            return True
    return False


def ln_fm(nc, p, st, onesD, z, out, c128, gname, bname, tag):
    zsq = p.sb(st, tag + 'zsq', [128, 8, TT])
    p.act(zsq[:, :, :], z[:, :, :], AF.Square, r=[z], w=[zsq])
    psM = p.psum()
    p.mm(psM[:, :], [(onesD[:, :], z[:, kc, :]) for kc in range(8)], r=[onesD, z], w=[psM])
    psQ = p.psum()
    p.mm(psQ[:, :], [(onesD[:, :], zsq[:, kc, :]) for kc in range(8)], r=[onesD, zsq], w=[psQ])
    mean = p.sb(st, tag + 'mean', [128, TT])
    rstd = p.sb(st, tag + 'rstd', [128, TT])
    tmp = p.sb(st, tag + 'tmp', [128, TT])
    p.copy(mean[:, :], psM[:, :], r=[psM], w=[mean], eng='act')
    p.act(tmp[:, :], psM[:, :], AF.Square, r=[psM], w=[tmp])
    p.tt(rstd[:, :], psQ[:, :], tmp[:, :], ALU.subtract, r=[psQ, tmp], w=[rstd])
    p.ts(rstd[:, :], rstd[:, :], 0.0, 1e-5, ALU.max, ALU.add, r=[rstd], w=[rstd])
    p.act(tmp[:, :], rstd[:, :], AF.Sqrt, r=[rstd], w=[tmp])
    p.op('dve', lambda: nc.vector.reciprocal(out=rstd[:, :], in_=tmp[:, :]), r=[tmp], w=[rstd])
    tks = [p.sb(st, tag + 'tk', [128, TT]) for _ in range(2)]
    for kc in range(8):
        eng = 'dve' if kc % 2 == 0 else 'pool'
        tk = tks[kc % 2]
        p.tt(tk[:, :], z[:, kc, :], mean[:, :], ALU.subtract, r=[z, mean], w=[tk], eng=eng)
        p.tt(tk[:, :], tk[:, :], rstd[:, :], ALU.mult, r=[tk, rstd], w=[tk], eng=eng)
        p.ts(out[:, kc, :], tk[:, :], c128[:, C128[gname] + kc:C128[gname] + kc + 1], c128[:, C128[bname] + kc:C128[bname] + kc + 1],
             ALU.mult, ALU.add, r=[tk, c128], w=[(out.key, kc)], eng='dve')
    p._mark('dve', p.cnt['dve'], [], [out.key])


def swiglu_bufs(p, st, F, nwd=1):
    nfc = F // 128
    return dict(hmid=p.sb(st, 'hmid', [128, nfc, TT], BF16),
                wgs=[p.sb(st, 'wgs', [128, 8, 512], BF16) for _ in range(2)],
                wus=[p.sb(st, 'wus', [128, 8, 512], BF16) for _ in range(2)],
                sgt=[p.sb(st, 'sgt', [128, TT]) for _ in range(2)],
                wd=[p.sb(st, 'wd', [128, nfc, 512], BF16) for _ in range(nwd)], cnt=[0, 0])


def swiglu_fm(nc, p, st, xb, wg_d, wu_d, wd_d, F, sink, bufs=None):
    nfc = F // 128
    if bufs is None:
        bufs = swiglu_bufs(p, st, F)
    hmid, wgs, wus, sgt = bufs['hmid'], bufs['wgs'], bufs['wus'], bufs['sgt']
    wg3 = wg_d.rearrange("(kc p) f -> p kc f", p=128)
    wu3 = wu_d.rearrange("(kc p) f -> p kc f", p=128)
    for f0 in range(0, F, 512):
        fw = min(512, F - f0)
        gi = bufs['cnt'][0]
        bufs['cnt'][0] += 1
        wg, wu = wgs[gi % 2], wus[gi % 2]
        p.dma('sp', wg[:, :, 0:fw], wg3[:, :, f0:f0 + fw], w=[wg])
        p.dma('sp', wu[:, :, 0:fw], wu3[:, :, f0:f0 + fw], w=[wu])
        for j in range(fw // 128):
            fc = f0 // 128 + j
            psG = p.psum()
            p.mm(psG[:, :], [(wg[:, kc, j * 128:(j + 1) * 128], xb[:, kc, :]) for kc in range(8)], r=[wg, xb], w=[psG])
            psU = p.psum()
            p.mm(psU[:, :], [(wu[:, kc, j * 128:(j + 1) * 128], xb[:, kc, :]) for kc in range(8)], r=[wu, xb], w=[psU])
            sg = sgt[fc % 2]
            p.act(sg[:, :], psG[:, :], AF.Silu, r=[psG], w=[sg])
            p.tt(hmid[:, fc, :], sg[:, :], psU[:, :], ALU.mult, r=[sg, psU], w=[hmid])
    wd3 = wd_d.rearrange("(fc p) m -> p fc m", p=128)
    for half in range(2):
        wi = bufs['cnt'][1]
        bufs['cnt'][1] += 1
        wd = bufs['wd'][wi % len(bufs['wd'])]
        for f0 in range(0, nfc, 7):
            f1 = min(nfc, f0 + 7)
            p.dma('sp', wd[:, f0:f1, :], wd3[:, f0:f1, half * 512:(half + 1) * 512], w=[(wd.key, f0)])
        for mm_ in range(4):
            mo = half * 4 + mm_
            ps = p.psum()
            p.mm(ps[:, :], [(wd[:, fc, mm_ * 128:(mm_ + 1) * 128], hmid[:, fc, :]) for fc in range(nfc)],
                 r=[(wd.key, f0) for f0 in range(0, nfc, 7)] + [hmid], w=[ps])
            sink(mo, ps)


def phase_tail(nc, p, IN, SC, l, xin, outd, outkey):
    with p.scope() as st0:
        c128 = p.sb(st0, 'c128t', [128, N128])
        p.dma('sp', c128[:, :], IN['c128'][l], w=[c128])
        onesD = p.sb(st0, 'onesD', [128, 128])
        p.op('pool', lambda: nc.gpsimd.memset(onesD[:, :], 1.0 / 1024.0), w=[onesD])
        if l == 1:
            wrt = p.sb(st0, 'wrt', [128, 8, NE])
            p.dma('sp', wrt[:, :, :], IN['moe_w_router'].rearrange("(kc p) e -> p kc e", p=128), w=[wrt])
            id128 = p.sb(st0, 'id128', [128, 128])
            ones_ = p.sb(st0, 'ones_', [128, 128])
            p.op('pool', lambda: nc.gpsimd.memset(ones_[:, :], 1.0), w=[ones_])
            p.op('pool', lambda: nc.gpsimd.affine_select(out=id128[:, :], in_=ones_[:, :], pattern=[[1, 128]], compare_op=ALU.is_equal,
                                                         fill=0.0, base=0, channel_multiplier=-1), r=[ones_], w=[id128])
            sel = p.sb(st0, 'sel', [8, NE, 128])
            ones3 = p.sb(st0, 'ones3', [8, NE, 128])
            p.op('pool', lambda: nc.gpsimd.memset(ones3[:, :, :], 1.0), w=[ones3])
            p.op('pool', lambda: nc.gpsimd.affine_select(out=sel[:, :, :], in_=ones3[:, :, :], pattern=[[-1, NE], [0, 128]],
                                                         compare_op=ALU.is_equal, fill=0.0, base=0, channel_multiplier=1), r=[ones3], w=[sel])
        xin3 = xin.rearrange("(kc p) t -> p kc t", p=128)
        out3 = outd.rearrange("(kc p) t -> p kc t", p=128)
        wbp = [SC['wb_pg'][l], SC['wb_pl'][l], SC['wb_pr'][l]]
        obd = [SC['ogT'], SC['olT'], SC['orT']]
        for tt in range(NT):
            tsl = slice(tt * TT, (tt + 1) * TT)
            with p.scope() as stx:
                x1 = p.sb(stx, 'x1', [128, 8, TT])
                x1b = p.sb(stx, 'x1b', [128, 8, TT], BF16)
                with p.scope() as st:
                    xf = p.sb(st, 'xf', [128, 8, TT])
                    xb = p.sb(st, 'xb', [128, 8, TT], BF16)
                    rk = [('x2T', c, tt) for c in range(8)] if l > 0 else []
                    p.dma('sp', xf[:, :, :], xin3[:, :, tsl], r=rk, w=[xf])
                    p.copy(xb[:, :, :], xf[:, :, :], r=[xf], w=[xb], eng='act')
                    acc = p.sb(st, 'acc', [128, 8, TT])
                    mb = p.sb(st, 'mb', [128, 8, TT], BF16)
                    wgh = [p.sb(st, 'wgh', [128, 8, 512], BF16) for _ in range(2)]
                    wph = [p.sb(st, 'wph', [128, 4, 512], BF16) for _ in range(2)]
                    ob = p.sb(st, 'ob', [128, 4, TT], BF16)
                    sig = [p.sb(st, 'sig', [128, TT]) for _ in range(2)]
                    for b in range(3):
                        for hf in range(2):
                            g0 = NMIX + b * 1024 + hf * 512
                            p.dma('sp', wgh[hf][:, :, :], SC['wb_in'][l].rearrange("(kc p) n -> p kc n", p=128)[:, :, g0:g0 + 512], w=[wgh[hf]])
                            p.dma('sp', wph[hf][:, :, :], wbp[b].rearrange("(fc p) m -> p fc m", p=128)[:, :, hf * 512:(hf + 1) * 512], w=[wph[hf]])
                        nrow = 128 if b < 2 else 64
                        okeys = [(['ogT', 'olT', 'orT'][b], i, tt) for i in range(512 // nrow)]
                        p.dma('sp', ob[:, :, :], obd[b].rearrange("(fc p) t -> p fc t", p=128)[:, :, tsl], r=okeys, w=[ob])
                        for mc in range(8):
                            ps1 = p.psum()
                            wgt, wpt, mq = wgh[mc // 4], wph[mc // 4], mc % 4
                            p.mm(ps1[:, :], [(wgt[:, kc, mq * 128:(mq + 1) * 128], xb[:, kc, :]) for kc in range(8)], r=[wgt, xb], w=[ps1])
                            ps2 = p.psum()
                            p.mm(ps2[:, :], [(wpt[:, fc, mq * 128:(mq + 1) * 128], ob[:, fc, :]) for fc in range(4)], r=[wpt, ob], w=[ps2])
                            sg = sig[mc % 2]
                            col = C128['b_gate'] + b * 8 + mc
                            p.act(sg[:, :], ps1[:, :], AF.Sigmoid, bias=c128[:, col:col + 1], r=[ps1, c128], w=[sg])
                            ak = (acc.key, mc)
                            if b == 0:
                                p.tt(acc[:, mc, :], sg[:, :], ps2[:, :], ALU.mult, r=[sg, ps2], w=[ak])
                            else:
                                p.tt(sg[:, :], sg[:, :], ps2[:, :], ALU.mult, r=[sg, ps2], w=[sg])
                                if b == 1:
                                    p.tt(acc[:, mc, :], acc[:, mc, :], sg[:, :], ALU.add, r=[ak, sg], w=[ak], eng='pool')
                                else:
                                    p.tt(mb[:, mc, :], acc[:, mc, :], sg[:, :], ALU.add, r=[ak, sg], w=[(mb.key, mc)], eng='pool')
                    p._mark('pool', p.cnt['pool'], [], [mb.key])
                    for hf in range(2):
                        p.dma('sp', wgh[hf][:, :, :], SC['wb_out'][l].rearrange("(kc p) n -> p kc n", p=128)[:, :, hf * 512:(hf + 1) * 512], w=[wgh[hf]])
                    z = acc
                    for mo in range(8):
                        wo, mq = wgh[mo // 4], mo % 4
                        ps = p.psum()
                        p.mm(ps[:, :], [(wo[:, kc, mq * 128:(mq + 1) * 128], mb[:, kc, :]) for kc in range(8)], r=[wo, mb], w=[ps])
                        p.stt(z[:, mo, :], xf[:, mo, :], ALPHA, ps[:, :], ALU.mult, ALU.add, r=[xf, ps, (acc.key, mo)], w=[(acc.key, mo)])
                    p._mark('dve', p.cnt['dve'], [], [z.key])
                    ln_fm(nc, p, st, onesD, z, x1, c128, 'ln_mix_g', 'ln_mix_b', 'l1')
                    p.copy(x1b[:, :, :], x1[:, :, :], r=[x1], w=[x1b], eng='act')
                with p.scope() as st:
                    z2 = p.sb(st, 'z2', [128, 8, TT])
                    if l == 0:
                        def sink(mo, ps):
                            p.stt(z2[:, mo, :], x1[:, mo, :], ALPHA, ps[:, :], ALU.mult, ALU.add, r=[x1, ps], w=[(z2.key, mo)])
                        swiglu_fm(nc, p, st, x1b, SC['wb_fg'], SC['wb_fu'], SC['wb_fd'], FD, sink, bufs=swiglu_bufs(p, st, FD, nwd=2))
                        p._mark('dve', p.cnt['dve'], [], [z2.key])
                    else:
                        wts = p.sb(st, 'wts', [128, 4, NE])
                        for tb in range(4):
                            psl = p.psum()
                            p.mm(psl[:, 0:NE], [(x1[:, kc, tb * 128:(tb + 1) * 128], wrt[:, kc, :]) for kc in range(8)], r=[x1, wrt], w=[psl])
                            lg = p.sb(st, 'lg', [128, NE])
                            p.copy(lg[:, :], psl[:, 0:NE], r=[psl], w=[lg], eng='act')
                            m1 = p.sb(st, 'm1', [128, 1])
                            m2 = p.sb(st, 'm2', [128, 1])
                            t8 = p.sb(st, 't8', [128, NE])
                            p.op('dve', lambda: nc.vector.tensor_reduce(out=m1[:, :], in_=lg[:, :], axis=mybir.AxisListType.X, op=ALU.max), r=[lg], w=[m1])
                            p.ts(t8[:, :], lg[:, :], m1[:, 0:1], -1e30, ALU.is_equal, ALU.mult, r=[lg, m1], w=[t8])
                            p.tt(t8[:, :], t8[:, :], lg[:, :], ALU.add, r=[t8, lg], w=[t8])
                            p.op('dve', lambda: nc.vector.tensor_reduce(out=m2[:, :], in_=t8[:, :], axis=mybir.AxisListType.X, op=ALU.max), r=[t8], w=[m2])
                            p.ts(t8[:, :], lg[:, :], m2[:, 0:1], None, ALU.is_ge, r=[lg, m2], w=[t8])
                            p.ts(m1[:, :], m1[:, :], -1.0, None, ALU.mult, r=[m1], w=[m1])
                            p.act(lg[:, :], lg[:, :], AF.Exp, bias=m1[:, 0:1], r=[lg, m1], w=[lg])
                            p.tt(lg[:, :], lg[:, :], t8[:, :], ALU.mult, r=[lg, t8], w=[lg])
                            p.op('dve', lambda: nc.vector.tensor_reduce(out=m2[:, :], in_=lg[:, :], axis=mybir.AxisListType.X, op=ALU.add), r=[lg], w=[m2])
                            p.op('dve', lambda: nc.vector.reciprocal(out=m2[:, :], in_=m2[:, :]), r=[m2], w=[m2])
                            p.ts(wts[:, tb, :], lg[:, :], m2[:, 0:1], None, ALU.mult, r=[lg, m2], w=[wts])
                        psw = p.psum()
                        p.transposes([(psw[0:NE, tb * 128:(tb + 1) * 128], wts[:, tb, :]) for tb in range(4)], id128[:, :], r=[wts, id128], w=[psw])
                        wT = p.sb(st, 'wT', [NE, TT])
                        p.copy(wT[:, :], psw[0:NE, :], r=[psw], w=[wT], eng='act')
                        accm = z2
                        mb_ = swiglu_bufs(p, st, FE, nwd=2)
                        wbes = [p.sb(st, 'wbe', [128, TT]) for _ in range(2)]
                        tmpm = [p.sb(st, 'tmpm', [128, TT]) for _ in range(2)]
                        for e in range(NE):
                            psb = p.psum()
                            p.mm(psb[:, :], [(sel[:, e, :], wT[:, :])], r=[sel, wT], w=[psb])
                            wbe = wbes[e % 2]
                            p.copy(wbe[:, :], psb[:, :], r=[psb], w=[wbe], eng='act')

                            def sink(mo, ps, e=e, wbe=wbe, tmpm=tmpm):
                                ak = (accm.key, mo)
                                if e == 0:
                                    p.tt(accm[:, mo, :], ps[:, :], wbe[:, :], ALU.mult, r=[ps, wbe], w=[ak])
                                else:
                                    tm = tmpm[mo % 2]
                                    p.tt(tm[:, :], ps[:, :], wbe[:, :], ALU.mult, r=[ps, wbe], w=[tm])
                                    p.tt(accm[:, mo, :], accm[:, mo, :], tm[:, :], ALU.add, r=[ak, tm], w=[ak], eng='pool')
                            swiglu_fm(nc, p, st, x1b, SC['wb_mg'][e], SC['wb_mu'][e], SC['wb_md'][e], FE, sink, bufs=mb_)
                        p._mark('pool', p.cnt['pool'], [], [accm.key])
                        for mo in range(8):
                            p.stt(z2[:, mo, :], x1[:, mo, :], ALPHA, accm[:, mo, :], ALU.mult, ALU.add, r=[x1, accm], w=[(z2.key, mo)])
                        p._mark('dve', p.cnt['dve'], [], [z2.key])
                    x2 = x1
                    ln_fm(nc, p, st, onesD, z2, x2, c128, 'ln_ffn_g', 'ln_ffn_b', 'l2')
                    p.dma('sp', out3[:, :, tsl], x2[:, :, :], r=[x2], w=[(outkey, c, tt) for c in range(8)])


def phase_rwkv2(nc, p, IN, SC, l, cst, c128, c64, cstream=None):
    YB = 7
    POOL1 = [0, 1, 2, 3]
    POOL2 = [4, 5, 6]
    with p.scope() as st0:
        w2 = p.sb(st0, 'w2', [64, 512])
        a2 = p.sb(st0, 'a2', [64, 512])
        g2a = p.sb(st0, 'g2a', [128, 512])
        g2b = p.sb(st0, 'g2b', [32, 512])
        p.dma('sp', w2[:, :], IN['rwkv_w2'][l], w=[w2])
        p.dma('sp', a2[:, :], IN['rwkv_a2'][l], w=[a2])
        p.dma('sp', g2a[:, :], IN['rwkv_g2'][l, 0:128, :], w=[g2a])
        p.dma('sp', g2b[:, :], IN['rwkv_g2'][l, 128:160, :], w=[g2b])
        omu = p.sb(st0, 'omu', [64, N64])
        p.ts(omu[:, :], c64[:, :], -1.0, 1.0, ALU.mult, ALU.add, r=[c64], w=[omu])
        omug = p.sb(st0, 'omug', [128, 2])
        p.ts(omug[:, :], c128[:, C128['mu_g']:C128['mu_g'] + 2], -1.0, 1.0, ALU.mult, ALU.add, r=[c128], w=[omug])
        Tst = [p.sb(st0, 'Tst%d' % h, [64, 64]) for h in range(8)]
        for h in range(8):
            p.op('pool', lambda: nc.gpsimd.memset(Tst[h][:, :], 0.0), w=[Tst[h]])
        psY = p.ps[YB]

        def T(tag, shape=None, dt=F32):
            return p.sb(st0, tag, shape or [64, TT], dt)
        raw64 = T('raw64', [64, TT + 1])
        raw128 = T('raw128', [128, TT + 1])
        wl, al = T('wl'), T('al')
        gl1 = T('gl1', [128, TT])
        gl2 = T('gl2', [32, TT])
        r, k, v = T('r'), T('k'), T('v')
        sgm, cum, ag, kk, tmp, k2, bv, E = T('sgm'), T('cum'), T('ag'), T('kk'), T('tmp'), T('k2'), T('bv'), T('E')
        kt, bt, kh, bh = T('kt', dt=BF16), T('bt', dt=BF16), T('kh'), T('bh')
        at16, rt16 = T('at16', dt=BF16), T('rt16', dt=BF16)
        D = T('D', [64, NCH, CH])
        Aab = T('Aab', dt=BF16)
        Pp = [T('Pa', dt=BF16), T('Pb', dt=BF16)]
        Qp = [T('Qa', dt=BF16), T('Qb', dt=BF16)]
        Nx = T('Nx', dt=BF16)
        Ny = T('Ny', dt=BF16)
        HB = []
        for i in range(2):
            HB.append(dict(at=T('at'), AakT=T('AakT'), vT=T('vT'), N=T('N'), rt=T('rt'), ArbT=T('ArbT'), ArkT=T('ArkT'),
                           bhT=T('bhT'), khT=T('khT'), WC=T('WC', [64, NCH, 1]), bonus=T('bonus'), gh=T('gh')))
        X = T('X', [64, 64])
        U = T('U', [64, 64])
        no, nosq, nmean, nmsq = T('no'), T('nosq'), T('nmean'), T('nmsq')
        orw = T('orw', [64, TT], BF16)

        def col(name, h=0):
            return c64[:, C64[name] + h:C64[name] + h + 1]

        def ocol(name, h=0):
            return omu[:, C64[name] + h:C64[name] + h + 1]

        def load_shift(out, raw, tt, row0, nrows, mu_ap, omu_ap, rd):
            t0 = tt * TT
            if tt == 0:
                p.op('pool', lambda: nc.gpsimd.memset(raw[0:nrows, 0:1], 0.0), w=[raw])
                p.dma('sp', raw[0:nrows, 1:], SC['hT'][row0:row0 + nrows, 0:TT], r=hk('hT', row0, nrows, tt), w=[raw])
            else:
                p.dma('sp', raw[0:nrows, :], SC['hT'][row0:row0 + nrows, t0 - 1:t0 + TT], r=hk('hT', row0, nrows, tt, True), w=[raw])
            p.ts(out[:, :], raw[0:nrows, 1:TT + 1], omu_ap, None, ALU.mult, r=[raw] + rd, w=[out])
            p.stt(out[:, :], raw[0:nrows, 0:TT], mu_ap, out[:, :], ALU.mult, ALU.add, r=[raw, out] + rd, w=[out])

        def tile_prep(tt):
            load_shift(wl, raw64, tt, RWW, 64, col('mu_w'), ocol('mu_w'), [c64, omu])
            load_shift(al, raw64, tt, RWA, 64, col('mu_a'), ocol('mu_a'), [c64, omu])
            mg = C128['mu_g']
            load_shift(gl1, raw128, tt, RWG, 128, c128[:, mg:mg + 1], omug[:, 0:1], [c128, omug])
            load_shift(gl2, raw128, tt, RWG + 128, 32, c128[0:32, mg + 1:mg + 2], omug[0:32, 1:2], [c128, omug])
            p.act(wl[:, :], wl[:, :], AF.Tanh, r=[wl], w=[wl])
            p.act(gl1[:, :], gl1[:, :], AF.Sigmoid, r=[gl1], w=[gl1])
            p.act(gl2[:, :], gl2[:, :], AF.Sigmoid, r=[gl2], w=[gl2])

        def cslices(t):
            return [t[:, c * 64:(c + 1) * 64] for c in range(NCH)]

        def stage1(tt, h, B):
            p.ps_pool = POOL1
            if h == 0:
                tile_prep(tt)
            hs = slice(h * 64, (h + 1) * 64)
            load_shift(r, raw64, tt, RWR + h * 64, 64, col('mu_r', h), ocol('mu_r', h), [c64, omu])
            load_shift(k, raw64, tt, RWK + h * 64, 64, col('mu_k', h), ocol('mu_k', h), [c64, omu])
            load_shift(v, raw64, tt, RWV + h * 64, 64, col('mu_v', h), ocol('mu_v', h), [c64, omu])
            ps = p.psum()
            p.mm(ps[0:64, :], [(w2[:, hs], wl[:, :])], r=[w2, wl], w=[ps])
            p.act(sgm[:, :], ps[0:64, :], AF.Sigmoid, bias=col('w0', h), r=[ps, c64], w=[sgm])
            p.op('dve', lambda: nc.vector.tensor_tensor_scan(out=cum[:, :], data0=cst.m01[:, :], data1=sgm[:, :], initial=0.0,
                                                             op0=ALU.mult, op1=ALU.add), r=[cst.m01, sgm], w=[cum])
            ps = p.psum()
            p.mm(ps[0:64, :], [(a2[:, hs], al[:, :])], r=[a2, al], w=[ps])
            p.act(ag[:, :], ps[0:64, :], AF.Sigmoid, bias=col('a0', h), r=[ps, c64], w=[ag])
            ps = p.psum()
            p.mm(ps[0:64, :], [(g2a[:, hs], gl1[:, :]), (g2b[:, hs], gl2[:, :])], r=[g2a, g2b, gl1, gl2], w=[ps])
            p.copy(B['gh'][:, :], ps[0:64, :], r=[ps], w=[B['gh']], eng='act')
            p.ts(kk[:, :], k[:, :], col('k_k', h), None, ALU.mult, r=[k, c64], w=[kk])
            p.act(tmp[:, :], kk[:, :], AF.Square, r=[kk], w=[tmp])
            ps = p.psum()
            p.mm(ps[0:64, :], [(cst.ones64[:, :], tmp[:, :])], r=[cst.ones64, tmp], w=[ps])
            p.act(tmp[:, :], ps[0:64, :], AF.Sqrt, r=[ps], w=[tmp])
            p.ts(tmp[:, :], tmp[:, :], 1e-12, None, ALU.max, r=[tmp], w=[tmp])
            p.op('dve', lambda: nc.vector.reciprocal(out=tmp[:, :], in_=tmp[:, :]), r=[tmp], w=[tmp])
            p.tt(kk[:, :], kk[:, :], tmp[:, :], ALU.mult, r=[kk, tmp], w=[kk])
            p.ts(tmp[:, :], ag[:, :], col('k_a', h), ocol('k_a', h), ALU.mult, ALU.add, r=[ag, c64, omu], w=[tmp])
            p.tt(k2[:, :], k[:, :], tmp[:, :], ALU.mult, r=[k, tmp], w=[k2])
            p.tt(bv[:, :], kk[:, :], ag[:, :], ALU.mult, r=[kk, ag], w=[bv])
            rt, at = B['rt'], B['at']
            p.act(E[:, :], cum[:, :], AF.Exp, scale=-C0, r=[cum], w=[E])
            p.tt(rt[:, :], r[:, :], E[:, :], ALU.mult, r=[r, E], w=[rt])
            p.copy(rt16[:, :], rt[:, :], r=[rt], w=[rt16], eng='act')
            p.act(E[:, :], cum[:, :], AF.Exp, scale=C0, r=[cum], w=[E])
            p.tt(kt[:, :], k2[:, :], E[:, :], ALU.mult, r=[k2, E], w=[kt])
            p.tt(bt[:, :], bv[:, :], E[:, :], ALU.mult, r=[bv, E], w=[bt])
            p.tt(tmp[:, :], cum[:, :], sgm[:, :], ALU.subtract, r=[cum, sgm], w=[tmp])
            p.act(E[:, :], tmp[:, :], AF.Exp, scale=-C0, r=[tmp], w=[E])
            p.stt(at[:, :], kk[:, :], -1.0, E[:, :], ALU.mult, ALU.mult, r=[kk, E], w=[at])
            p.copy(at16[:, :], at[:, :], r=[at], w=[at16], eng='act')
            c3 = cview(cum)
            p.tt(D[:, :, :], c3[:, :, CH - 1:CH].to_broadcast([64, NCH, CH]), c3, ALU.subtract, r=[cum], w=[D])
            p.act(D[:, :, :], D[:, :, :], AF.Exp, scale=-C0, r=[D], w=[D])
            Df = D[:, :, :].rearrange("p c t -> p (c t)")
            p.tt(kh[:, :], k2[:, :], Df, ALU.mult, r=[k2, D], w=[kh])
            p.tt(bh[:, :], bv[:, :], Df, ALU.mult, r=[bv, D], w=[bh])
            p.act(B['WC'][:, :, :], c3[:, :, CH - 1:CH], AF.Exp, scale=-C0, r=[cum], w=[B['WC']])
            p.stt(tmp[:, :], r[:, :], col('r_k', h), k2[:, :], ALU.mult, ALU.mult, r=[r, k2, c64], w=[tmp])
            ps = p.psum()
            p.mm(ps[0:64, :], [(cst.ones64[:, :], tmp[:, :])], r=[cst.ones64, tmp], w=[ps])
            p.tt(B['bonus'][:, :], ps[0:64, :], v[:, :], ALU.mult, r=[ps, v], w=[B['bonus']])
            for src, dn in ((v, 'vT'), (kh, 'khT'), (bh, 'bhT')):
                psT = p.psum()
                p.transposes(list(zip([psT[0:64, c * 64:(c + 1) * 64] for c in range(NCH)], cslices(src))),
                             cst.ident[:, :], r=[src, cst.ident], w=[psT])
                p.copy(B[dn][:, :], psT[0:64, :], r=[psT], w=[B[dn]], eng='act')

            def amat(lt, rh, mask, d):
                psA = p.psum()
                p.mms([(psA[0:64, c * 64:(c + 1) * 64], [(lt[:, c * 64:(c + 1) * 64], rh[:, c * 64:(c + 1) * 64])]) for c in range(NCH)],
                      r=[lt, rh], w=[psA])
                p.tt(d[:, :], psA[0:64, :], mask[:, :, :].rearrange("p c t -> p (c t)"), ALU.mult, r=[psA, mask], w=[d])
            amat(bt, at16, cst.mgt, Qp[0])
            amat(bt, rt16, cst.mge, B['ArbT'])
            amat(kt, at16, cst.mgt, B['AakT'])
            amat(kt, rt16, cst.mge, B['ArkT'])
            amat(at16, bt, cst.mlt, Aab)
            P_, Q_ = Aab, Qp[0]
            Ns = [Nx, Ny]
            N_ = Ns[0]
            p.tt(N_[:, :], Q_[:, :], cst.id8[:, :, :].rearrange("p c t -> p (c t)"), ALU.add, r=[Q_, cst.id8], w=[N_])
            for lev in range(5):
                psP = p.psum()
                p.mms([(psP[0:64, c * 64:(c + 1) * 64], [(Q_[:, c * 64:(c + 1) * 64], P_[:, c * 64:(c + 1) * 64])]) for c in range(NCH)],
                      r=[P_, Q_], w=[psP])
                if lev < 4:
                    psQ = p.psum()
                    p.mms([(psQ[0:64, c * 64:(c + 1) * 64], [(P_[:, c * 64:(c + 1) * 64], Q_[:, c * 64:(c + 1) * 64])]) for c in range(NCH)],
                          r=[P_, Q_], w=[psQ])
                P2 = Pp[lev % 2]
                p.copy(P2[:, :], psP[0:64, :], r=[psP], w=[P2], eng='act')
                if lev < 4:
                    Q2 = Qp[(lev + 1) % 2]
                    p.copy(Q2[:, :], psQ[0:64, :], r=[psQ], w=[Q2], eng='dve')
                psN = p.psum()
                p.mms([(psN[0:64, c * 64:(c + 1) * 64], [(P2[:, c * 64:(c + 1) * 64], N_[:, c * 64:(c + 1) * 64])]) for c in range(NCH)],
                      r=[P2, N_], w=[psN])
                N2 = Ns[(lev + 1) % 2] if lev < 4 else B['N']
                p.tt(N2[:, :], N_[:, :], psN[0:64, :], ALU.add, r=[N_, psN], w=[N2])
                P_, N_ = P2, N2
                if lev < 4:
                    Q_ = Q2
            assert N_ is B['N']

        def stage2(tt, h, B):
            p.ps_pool = POOL2
            tsl = slice(tt * TT, (tt + 1) * TT)
            at, AakT, vT, N_, rt, ArbT, ArkT, bhT, khT, WC = (B[x] for x in ('at', 'AakT', 'vT', 'N', 'rt', 'ArbT', 'ArkT', 'bhT', 'khT', 'WC'))
            for c in range(NCH):
                cs = slice(c * 64, (c + 1) * 64)
                psX = p.psum()
                p.mm(psX[0:64, 0:64], [(at[:, cs], Tst[h][:, :]), (AakT[:, cs], vT[:, cs])], r=[at, Tst[h], AakT, vT], w=[psX])
                p.copy(X[:, :], psX[0:64, 0:64], r=[psX], w=[X], eng='act')
                psU = p.psum()
                p.mm(psU[0:64, 0:64], [(N_[:, cs], X[:, :])], r=[N_, X], w=[psU])
                p.copy(U[:, :], psU[0:64, 0:64], r=[psU], w=[U], eng='dve')
                p.mm(psY[0:64, cs], [(Tst[h][:, :], rt[:, cs]), (U[:, :], ArbT[:, cs]), (vT[:, cs], ArkT[:, cs])],
                     r=[Tst[h], rt, U, ArbT, vT, ArkT], w=[psY])
                psT2 = p.psum()
                p.mm(psT2[0:64, 0:64], [(bhT[:, cs], U[:, :]), (khT[:, cs], vT[:, cs])], r=[bhT, U, khT, vT], w=[psT2])
                p.stt(Tst[h][:, :], Tst[h][:, :], WC[:, c, :], psT2[0:64, 0:64], ALU.mult, ALU.add, r=[Tst[h], WC, psT2], w=[Tst[h]])
            ones = cst.onesm64
            p.copy(no[:, :], psY[0:64, :], r=[psY], w=[no], eng='act')
            p.act(nosq[:, :], psY[0:64, :], AF.Square, r=[psY], w=[nosq])
            psM = p.psum()
            p.mm(psM[0:64, :], [(ones[:, :], no[:, :])], r=[ones, no], w=[psM])
            psQ2 = p.psum()
            p.mm(psQ2[0:64, :], [(ones[:, :], nosq[:, :])], r=[ones, nosq], w=[psQ2])
            p.copy(nmean[:, :], psM[0:64, :], r=[psM], w=[nmean], eng='act')
            p.act(nmsq[:, :], psM[0:64, :], AF.Square, r=[psM], w=[nmsq])
            p.tt(nosq[:, :], psQ2[0:64, :], nmsq[:, :], ALU.subtract, r=[psQ2, nmsq], w=[nosq])
            p.ts(nosq[:, :], nosq[:, :], 0.0, 64e-5, ALU.max, ALU.add, r=[nosq], w=[nosq])
            p.act(nmsq[:, :], nosq[:, :], AF.Sqrt, r=[nosq], w=[nmsq])
            p.op('dve', lambda: nc.vector.reciprocal(out=nosq[:, :], in_=nmsq[:, :]), r=[nmsq], w=[nosq])
            p.tt(no[:, :], no[:, :], nmean[:, :], ALU.subtract, r=[no, nmean], w=[no])
            p.tt(no[:, :], no[:, :], nosq[:, :], ALU.mult, r=[no, nosq], w=[no])
            p.ts(no[:, :], no[:, :], col('lnx_g', h), col('lnx_b', h), ALU.mult, ALU.add, r=[no, c64], w=[no])
            p.tt(no[:, :], no[:, :], B['bonus'][:, :], ALU.add, r=[no, B['bonus']], w=[no])
            p.tt(orw[:, :], no[:, :], B['gh'][:, :], ALU.mult, r=[no, B['gh']], w=[orw])
            p.dma('sp', SC['orT'][h * 64:(h + 1) * 64, tsl], orw[:, :], r=[orw], w=[('orT', h, tt)])

        seq = [(tt, h) for tt in range(NT) for h in range(8)]
        L1 = p.record(lambda: stage1(seq[0][0], seq[0][1], HB[0]))
        p.play([L1])
        for n, (tt, h) in enumerate(seq):
            L2 = p.record(lambda: stage2(tt, h, HB[n % 2]))
            lists = [L2]
            if n + 1 < len(seq):
                tn, hn = seq[n + 1]
                lists.append(p.record(lambda: stage1(tn, hn, HB[(n + 1) % 2])))
            if cstream is not None:
                lists.append(cstream.take(len(seq) - n))
            p.play(lists)
    p.ps_pool = list(range(8))


def phase_rwkv3(nc, p, IN, SC, l, cst, c128, c64, cstream=None):
    POOLA = [0, 1]
    POOLB = [2, 3]
    POOLC = [4, 5]
    with p.scope() as st0:
        w2 = p.sb(st0, 'w2', [64, 512])
        a2 = p.sb(st0, 'a2', [64, 512])
        g2a = p.sb(st0, 'g2a', [128, 512])
        g2b = p.sb(st0, 'g2b', [32, 512])
        p.dma('sp', w2[:, :], IN['rwkv_w2'][l], w=[w2])
        p.dma('sp', a2[:, :], IN['rwkv_a2'][l], w=[a2])
        p.dma('sp', g2a[:, :], IN['rwkv_g2'][l, 0:128, :], w=[g2a])
        p.dma('sp', g2b[:, :], IN['rwkv_g2'][l, 128:160, :], w=[g2b])
        omu = p.sb(st0, 'omu', [64, N64])
        p.ts(omu[:, :], c64[:, :], -1.0, 1.0, ALU.mult, ALU.add, r=[c64], w=[omu])
        omug = p.sb(st0, 'omug', [128, 2])
        p.ts(omug[:, :], c128[:, C128['mu_g']:C128['mu_g'] + 2], -1.0, 1.0, ALU.mult, ALU.add, r=[c128], w=[omug])
        Tst = [p.sb(st0, 'Tst%d' % h, [64, 64]) for h in range(8)]
        for h in range(8):
            p.op('pool', lambda: nc.gpsimd.memset(Tst[h][:, :], 0.0), w=[Tst[h]])
        psYs = [p.ps[6], p.ps[7]]

        def T(tag, shape=None, dt=F32):
            return p.sb(st0, tag, shape or [64, TT], dt)
        raw64 = T('raw64', [64, TT + 1])
        raw128 = T('raw128', [128, TT + 1])
        wl, al = T('wl'), T('al')
        gl1 = T('gl1', [128, TT])
        gl2 = T('gl2', [32, TT])
        r, k, v = T('r'), T('k'), T('v')
        sgm, cum, ag, kk, tmp, k2, bv, E = T('sgm'), T('cum'), T('ag'), T('kk'), T('tmp'), T('k2'), T('bv'), T('E')
        kt, bt, kh, bh = T('kt', dt=BF16), T('bt', dt=BF16), T('kh'), T('bh')
        at16, rt16 = T('at16', dt=BF16), T('rt16', dt=BF16)
        D = T('D', [64, NCH, CH])
        AQ = [dict(Aab=T('Aab', dt=BF16), Q0=T('Q0', dt=BF16)) for _ in range(2)]
        Pp = [T('Pa', dt=BF16), T('Pb', dt=BF16)]
        Qp = [T('Qa', dt=BF16), T('Qb', dt=BF16)]
        Nx = T('Nx', dt=BF16)
        Ny = T('Ny', dt=BF16)
        HB = []
        for i in range(3):
            HB.append(dict(at=T('at'), AakT=T('AakT'), vT=T('vT'), N=T('N'), rt=T('rt'), ArbT=T('ArbT'), ArkT=T('ArkT'),
                           bhT=T('bhT'), khT=T('khT'), WC=T('WC', [64, NCH, 1]), bonus=T('bonus'), gh=T('gh')))
        X = T('X', [64, 64])
        U = T('U', [64, 64])
        no, nosq, nmean, nmsq = T('no'), T('nosq'), T('nmean'), T('nmsq')
        orw = T('orw', [64, TT], BF16)

        def col(name, h=0):
            return c64[:, C64[name] + h:C64[name] + h + 1]

        def ocol(name, h=0):
            return omu[:, C64[name] + h:C64[name] + h + 1]

        def load_shift(out, raw, tt, row0, nrows, mu_ap, omu_ap, rd):
            t0 = tt * TT
            if tt == 0:
                p.op('pool', lambda: nc.gpsimd.memset(raw[0:nrows, 0:1], 0.0), w=[raw])
                p.dma('sp', raw[0:nrows, 1:], SC['hT'][row0:row0 + nrows, 0:TT], r=hk('hT', row0, nrows, tt), w=[raw])
            else:
                p.dma('sp', raw[0:nrows, :], SC['hT'][row0:row0 + nrows, t0 - 1:t0 + TT], r=hk('hT', row0, nrows, tt, True), w=[raw])
            p.ts(out[:, :], raw[0:nrows, 1:TT + 1], omu_ap, None, ALU.mult, r=[raw] + rd, w=[out])
            p.stt(out[:, :], raw[0:nrows, 0:TT], mu_ap, out[:, :], ALU.mult, ALU.add, r=[raw, out] + rd, w=[out])

        def tile_prep(tt):
            load_shift(wl, raw64, tt, RWW, 64, col('mu_w'), ocol('mu_w'), [c64, omu])
            load_shift(al, raw64, tt, RWA, 64, col('mu_a'), ocol('mu_a'), [c64, omu])
            mg = C128['mu_g']
            load_shift(gl1, raw128, tt, RWG, 128, c128[:, mg:mg + 1], omug[:, 0:1], [c128, omug])
            load_shift(gl2, raw128, tt, RWG + 128, 32, c128[0:32, mg + 1:mg + 2], omug[0:32, 1:2], [c128, omug])
            p.act(wl[:, :], wl[:, :], AF.Tanh, r=[wl], w=[wl])
            p.act(gl1[:, :], gl1[:, :], AF.Sigmoid, r=[gl1], w=[gl1])
            p.act(gl2[:, :], gl2[:, :], AF.Sigmoid, r=[gl2], w=[gl2])

        def cslices(t):
            return [t[:, c * 64:(c + 1) * 64] for c in range(NCH)]

        def stage1a(tt, h, B, aq):
            p.ps_pool = POOLA
            if h == 0:
                tile_prep(tt)
            hs = slice(h * 64, (h + 1) * 64)
            load_shift(r, raw64, tt, RWR + h * 64, 64, col('mu_r', h), ocol('mu_r', h), [c64, omu])
            load_shift(k, raw64, tt, RWK + h * 64, 64, col('mu_k', h), ocol('mu_k', h), [c64, omu])
            load_shift(v, raw64, tt, RWV + h * 64, 64, col('mu_v', h), ocol('mu_v', h), [c64, omu])
            ps = p.psum()
            p.mm(ps[0:64, :], [(w2[:, hs], wl[:, :])], r=[w2, wl], w=[ps])
            p.act(sgm[:, :], ps[0:64, :], AF.Sigmoid, bias=col('w0', h), r=[ps, c64], w=[sgm])
            p.op('dve', lambda: nc.vector.tensor_tensor_scan(out=cum[:, :], data0=cst.m01[:, :], data1=sgm[:, :], initial=0.0,
                                                             op0=ALU.mult, op1=ALU.add), r=[cst.m01, sgm], w=[cum])
            ps = p.psum()
            p.mm(ps[0:64, :], [(a2[:, hs], al[:, :])], r=[a2, al], w=[ps])
            p.act(ag[:, :], ps[0:64, :], AF.Sigmoid, bias=col('a0', h), r=[ps, c64], w=[ag])
            ps = p.psum()
            p.mm(ps[0:64, :], [(g2a[:, hs], gl1[:, :]), (g2b[:, hs], gl2[:, :])], r=[g2a, g2b, gl1, gl2], w=[ps])
            p.copy(B['gh'][:, :], ps[0:64, :], r=[ps], w=[B['gh']], eng='act')
            p.ts(kk[:, :], k[:, :], col('k_k', h), None, ALU.mult, r=[k, c64], w=[kk])
            p.act(tmp[:, :], kk[:, :], AF.Square, r=[kk], w=[tmp])
            ps = p.psum()
            p.mm(ps[0:64, :], [(cst.ones64[:, :], tmp[:, :])], r=[cst.ones64, tmp], w=[ps])
            p.act(tmp[:, :], ps[0:64, :], AF.Sqrt, r=[ps], w=[tmp])
            p.ts(tmp[:, :], tmp[:, :], 1e-12, None, ALU.max, r=[tmp], w=[tmp])
            p.op('dve', lambda: nc.vector.reciprocal(out=tmp[:, :], in_=tmp[:, :]), r=[tmp], w=[tmp])
            p.tt(kk[:, :], kk[:, :], tmp[:, :], ALU.mult, r=[kk, tmp], w=[kk])
            p.ts(tmp[:, :], ag[:, :], col('k_a', h), ocol('k_a', h), ALU.mult, ALU.add, r=[ag, c64, omu], w=[tmp])
            p.tt(k2[:, :], k[:, :], tmp[:, :], ALU.mult, r=[k, tmp], w=[k2])
            p.tt(bv[:, :], kk[:, :], ag[:, :], ALU.mult, r=[kk, ag], w=[bv])
            rt, at = B['rt'], B['at']
            p.act(E[:, :], cum[:, :], AF.Exp, scale=-C0, r=[cum], w=[E])
            p.tt(rt[:, :], r[:, :], E[:, :], ALU.mult, r=[r, E], w=[rt])
            p.copy(rt16[:, :], rt[:, :], r=[rt], w=[rt16], eng='act')
            p.act(E[:, :], cum[:, :], AF.Exp, scale=C0, r=[cum], w=[E])
            p.tt(kt[:, :], k2[:, :], E[:, :], ALU.mult, r=[k2, E], w=[kt])
            p.tt(bt[:, :], bv[:, :], E[:, :], ALU.mult, r=[bv, E], w=[bt])
            p.tt(tmp[:, :], cum[:, :], sgm[:, :], ALU.subtract, r=[cum, sgm], w=[tmp])
            p.act(E[:, :], tmp[:, :], AF.Exp, scale=-C0, r=[tmp], w=[E])
            p.stt(at[:, :], kk[:, :], -1.0, E[:, :], ALU.mult, ALU.mult, r=[kk, E], w=[at])
            p.copy(at16[:, :], at[:, :], r=[at], w=[at16], eng='act')
            c3 = cview(cum)
            p.tt(D[:, :, :], c3[:, :, CH - 1:CH].to_broadcast([64, NCH, CH]), c3, ALU.subtract, r=[cum], w=[D])
            p.act(D[:, :, :], D[:, :, :], AF.Exp, scale=-C0, r=[D], w=[D])
            Df = D[:, :, :].rearrange("p c t -> p (c t)")
            p.tt(kh[:, :], k2[:, :], Df, ALU.mult, r=[k2, D], w=[kh])
            p.tt(bh[:, :], bv[:, :], Df, ALU.mult, r=[bv, D], w=[bh])
            p.act(B['WC'][:, :, :], c3[:, :, CH - 1:CH], AF.Exp, scale=-C0, r=[cum], w=[B['WC']])
            p.stt(tmp[:, :], r[:, :], col('r_k', h), k2[:, :], ALU.mult, ALU.mult, r=[r, k2, c64], w=[tmp])
            ps = p.psum()
            p.mm(ps[0:64, :], [(cst.ones64[:, :], tmp[:, :])], r=[cst.ones64, tmp], w=[ps])
            p.tt(B['bonus'][:, :], ps[0:64, :], v[:, :], ALU.mult, r=[ps, v], w=[B['bonus']])
            for src, dn in ((v, 'vT'), (kh, 'khT'), (bh, 'bhT')):
                psT = p.psum()
                p.transposes(list(zip([psT[0:64, c * 64:(c + 1) * 64] for c in range(NCH)], cslices(src))),
                             cst.ident[:, :], r=[src, cst.ident], w=[psT])
                p.copy(B[dn][:, :], psT[0:64, :], r=[psT], w=[B[dn]], eng='act')

            def amat(lt, rh, mask, d):
                psA = p.psum()
                p.mms([(psA[0:64, c * 64:(c + 1) * 64], [(lt[:, c * 64:(c + 1) * 64], rh[:, c * 64:(c + 1) * 64])]) for c in range(NCH)],
                      r=[lt, rh], w=[psA])
                p.tt(d[:, :], psA[0:64, :], mask[:, :, :].rearrange("p c t -> p (c t)"), ALU.mult, r=[psA, mask], w=[d])
            amat(bt, at16, cst.mgt, aq['Q0'])
            amat(bt, rt16, cst.mge, B['ArbT'])
            amat(kt, at16, cst.mgt, B['AakT'])
            amat(kt, rt16, cst.mge, B['ArkT'])
            amat(at16, bt, cst.mlt, aq['Aab'])

        def stage1b(B, aq):
            p.ps_pool = POOLB
            P_, Q_ = aq['Aab'], aq['Q0']
            Ns = [Nx, Ny]
            N_ = Ns[0]
            p.tt(N_[:, :], Q_[:, :], cst.id8[:, :, :].rearrange("p c t -> p (c t)"), ALU.add, r=[Q_, cst.id8], w=[N_])
            for lev in range(5):
                psP = p.psum()
                p.mms([(psP[0:64, c * 64:(c + 1) * 64], [(Q_[:, c * 64:(c + 1) * 64], P_[:, c * 64:(c + 1) * 64])]) for c in range(NCH)],
                      r=[P_, Q_], w=[psP])
                if lev < 4:
                    psQ = p.psum()
                    p.mms([(psQ[0:64, c * 64:(c + 1) * 64], [(P_[:, c * 64:(c + 1) * 64], Q_[:, c * 64:(c + 1) * 64])]) for c in range(NCH)],
                          r=[P_, Q_], w=[psQ])
                P2 = Pp[lev % 2]
                p.copy(P2[:, :], psP[0:64, :], r=[psP], w=[P2], eng='act')
                if lev < 4:
                    Q2 = Qp[lev % 2]
                    p.copy(Q2[:, :], psQ[0:64, :], r=[psQ], w=[Q2], eng='dve')
                psN = p.psum()
                p.mms([(psN[0:64, c * 64:(c + 1) * 64], [(P2[:, c * 64:(c + 1) * 64], N_[:, c * 64:(c + 1) * 64])]) for c in range(NCH)],
                      r=[P2, N_], w=[psN])
                N2 = Ns[(lev + 1) % 2] if lev < 4 else B['N']
                p.tt(N2[:, :], N_[:, :], psN[0:64, :], ALU.add, r=[N_, psN], w=[N2])
                P_, N_ = P2, N2
                if lev < 4:
                    Q_ = Q2
            assert N_ is B['N']

        def chain(tt, h, B, psY):
            p.ps_pool = POOLC
            at, AakT, vT, N_, rt, ArbT, ArkT, bhT, khT, WC = (B[x] for x in ('at', 'AakT', 'vT', 'N', 'rt', 'ArbT', 'ArkT', 'bhT', 'khT', 'WC'))
            for c in range(NCH):
                cs = slice(c * 64, (c + 1) * 64)
                psX = p.psum()
                p.mm(psX[0:64, 0:64], [(at[:, cs], Tst[h][:, :]), (AakT[:, cs], vT[:, cs])], r=[at, Tst[h], AakT, vT], w=[psX])
                p.copy(X[:, :], psX[0:64, 0:64], r=[psX], w=[X], eng='act')
                psU = p.psum()
                p.mm(psU[0:64, 0:64], [(N_[:, cs], X[:, :])], r=[N_, X], w=[psU])
                p.copy(U[:, :], psU[0:64, 0:64], r=[psU], w=[U], eng='dve')
                p.mm(psY[0:64, cs], [(Tst[h][:, :], rt[:, cs]), (U[:, :], ArbT[:, cs]), (vT[:, cs], ArkT[:, cs])],
                     r=[Tst[h], rt, U, ArbT, vT, ArkT], w=[psY])
                psT2 = p.psum()
                p.mm(psT2[0:64, 0:64], [(bhT[:, cs], U[:, :]), (khT[:, cs], vT[:, cs])], r=[bhT, U, khT, vT], w=[psT2])
                p.stt(Tst[h][:, :], Tst[h][:, :], WC[:, c, :], psT2[0:64, 0:64], ALU.mult, ALU.add, r=[Tst[h], WC, psT2], w=[Tst[h]])

        def norm(tt, h, B, psY):
            p.ps_pool = POOLA
            tsl = slice(tt * TT, (tt + 1) * TT)
            ones = cst.onesm64
            p.copy(no[:, :], psY[0:64, :], r=[psY], w=[no], eng='act')
            p.act(nosq[:, :], psY[0:64, :], AF.Square, r=[psY], w=[nosq])
            psM = p.psum()
            p.mm(psM[0:64, :], [(ones[:, :], no[:, :])], r=[ones, no], w=[psM])
            psQ2 = p.psum()
            p.mm(psQ2[0:64, :], [(ones[:, :], nosq[:, :])], r=[ones, nosq], w=[psQ2])
            p.copy(nmean[:, :], psM[0:64, :], r=[psM], w=[nmean], eng='act')
            p.act(nmsq[:, :], psM[0:64, :], AF.Square, r=[psM], w=[nmsq])
            p.tt(nosq[:, :], psQ2[0:64, :], nmsq[:, :], ALU.subtract, r=[psQ2, nmsq], w=[nosq])
            p.ts(nosq[:, :], nosq[:, :], 0.0, 64e-5, ALU.max, ALU.add, r=[nosq], w=[nosq])
            p.act(nmsq[:, :], nosq[:, :], AF.Sqrt, r=[nosq], w=[nmsq])
            p.op('dve', lambda: nc.vector.reciprocal(out=nosq[:, :], in_=nmsq[:, :]), r=[nmsq], w=[nosq])
            p.tt(no[:, :], no[:, :], nmean[:, :], ALU.subtract, r=[no, nmean], w=[no])
            p.tt(no[:, :], no[:, :], nosq[:, :], ALU.mult, r=[no, nosq], w=[no])
            p.ts(no[:, :], no[:, :], col('lnx_g', h), col('lnx_b', h), ALU.mult, ALU.add, r=[no, c64], w=[no])
            p.tt(no[:, :], no[:, :], B['bonus'][:, :], ALU.add, r=[no, B['bonus']], w=[no])
            p.tt(orw[:, :], no[:, :], B['gh'][:, :], ALU.mult, r=[no, B['gh']], w=[orw])
            p.dma('sp', SC['orT'][h * 64:(h + 1) * 64, tsl], orw[:, :], r=[orw], w=[('orT', h, tt)])

        seq = [(tt, h) for tt in range(NT) for h in range(8)]
        NS = len(seq)

        def A_stream(n):
            def f():
                if 0 <= n - 1 < NS:
                    t_, h_ = seq[n - 1]
                    norm(t_, h_, HB[(n - 1) % 3], psYs[(n - 1) % 2])
                if n + 2 < NS:
                    t_, h_ = seq[n + 2]
                    stage1a(t_, h_, HB[(n + 2) % 3], AQ[(n + 2) % 2])
            return p.record(f)

        def B_stream(n):
            def f():
                if n + 1 < NS:
                    stage1b(HB[(n + 1) % 3], AQ[(n + 1) % 2])
            return p.record(f)

        def C_stream(n):
            def f():
                if 0 <= n < NS:
                    t_, h_ = seq[n]
                    chain(t_, h_, HB[n % 3], psYs[n % 2])
            return p.record(f)
        for n in range(-2, NS + 1):
            lists = [C_stream(n), B_stream(n), A_stream(n)]
            if cstream is not None and 0 <= n < NS:
                lists.append(cstream.take(NS - n))
            p.play(lists)
    p.ps_pool = list(range(8))
```

```python
import numpy as np
from contextlib import ExitStack, contextmanager
import concourse.bass as bass
import concourse.mybir as mybir
from concourse.bass_utils import run_bass_kernel_spmd

F32 = mybir.dt.float32
BF16 = mybir.dt.bfloat16
I32 = mybir.dt.int32
AF = mybir.ActivationFunctionType
ALU = mybir.AluOpType

T = 4096
TT = 512
NT = T // TT
CH = 64
NCH = TT // CH
DM = 1024
NIN = 7472
NMIX = 4400
NMIXP = 4480
ALPHA = 4.0 ** 0.25
FD = 2816
FE = 3584
NE = 8
C0 = float(np.exp(-0.5))

Q0, K0, V0, R0, DEC0, LX0, LG0, RW0 = 0, 256, 512, 1024, 1536, 1552, 2064, 2576
RWR, RWK, RWV, RWW, RWA, RWG = RW0, RW0 + 512, RW0 + 1024, RW0 + 1536, RW0 + 1600, RW0 + 1664

C128 = {}
_o = 0
for _n, _c in [('b_in', 35), ('b_gate', 24), ('gla_ng', 4), ('gla_nb', 4), ('conv_w', 16), ('conv_b', 4),
               ('b_r', 4), ('b_i', 4), ('lam', 4), ('ln_mix_g', 8), ('ln_mix_b', 8), ('ln_ffn_g', 8),
               ('ln_ffn_b', 8), ('mu_g', 2)]:
    C128[_n] = _o
    _o += _c
N128 = _o
C64 = {}
_o = 0
for _n, _c in [('gla_bd', 4), ('mu_r', 8), ('mu_k', 8), ('mu_v', 8), ('mu_w', 1), ('mu_a', 1), ('w0', 8), ('a0', 8),
               ('k_k', 8), ('k_a', 8), ('r_k', 8), ('lnx_g', 8), ('lnx_b', 8)]:
    C64[_n] = _o
    _o += _c
N64 = _o


def _cols(v, P):
    v = np.asarray(v, np.float32).reshape(-1)
    n = -(-v.size // P) * P
    pad = np.zeros(n, np.float32)
    pad[:v.size] = v
    return pad.reshape(n // P, P).T


def pack_cols(inp, l):
    c128 = np.zeros((128, N128), np.float32)
    c64 = np.zeros((64, N64), np.float32)

    def put(dst, table, name, arr, P):
        a = _cols(arr, P)
        dst[:, table[name]:table[name] + a.shape[1]] = a
    put(c128, C128, 'b_in', inp['b_in'][l][:NMIX], 128)
    put(c128, C128, 'b_gate', inp['b_in'][l][NMIX:], 128)
    put(c128, C128, 'gla_ng', inp['gla_norm_g'][l], 128)
    put(c128, C128, 'gla_nb', inp['gla_norm_b'][l], 128)
    put(c128, C128, 'conv_w', inp['lru_conv_w'][l], 128)
    put(c128, C128, 'conv_b', inp['lru_conv_b'][l], 128)
    put(c128, C128, 'b_r', inp['lru_b_r'][l], 128)
    put(c128, C128, 'b_i', inp['lru_b_i'][l], 128)
    put(c128, C128, 'lam', inp['lru_lambda'][l], 128)
    put(c128, C128, 'ln_mix_g', inp['ln_mix_g'][l], 128)
    put(c128, C128, 'ln_mix_b', inp['ln_mix_b'][l], 128)
    put(c128, C128, 'ln_ffn_g', inp['ln_ffn_g'][l], 128)
    put(c128, C128, 'ln_ffn_b', inp['ln_ffn_b'][l], 128)
    mu = inp['rwkv_mu'][l]
    put(c128, C128, 'mu_g', mu[1664:1824], 128)
    put(c64, C64, 'gla_bd', inp['gla_b_decay'][l], 64)
    put(c64, C64, 'mu_r', mu[0:512], 64)
    put(c64, C64, 'mu_k', mu[512:1024], 64)
    put(c64, C64, 'mu_v', mu[1024:1536], 64)
    put(c64, C64, 'mu_w', mu[1536:1600], 64)
    put(c64, C64, 'mu_a', mu[1600:1664], 64)
    put(c64, C64, 'w0', inp['rwkv_w0'][l], 64)
    put(c64, C64, 'a0', inp['rwkv_a0'][l], 64)
    put(c64, C64, 'k_k', inp['rwkv_k_k'][l], 64)
    put(c64, C64, 'k_a', inp['rwkv_k_a'][l], 64)
    put(c64, C64, 'r_k', inp['rwkv_r_k'][l], 64)
    put(c64, C64, 'lnx_g', inp['rwkv_lnx_g'][l], 64)
    put(c64, C64, 'lnx_b', inp['rwkv_lnx_b'][l], 64)
    return c128, c64


class Tl:
    def __init__(self, t, key):
        self.t = t
        self.key = key

    def __getitem__(self, idx):
        return self.t[idx]


def _keys(xs):
    out = []
    for x in xs:
        if isinstance(x, Tl):
            out.append(x.key)
        elif isinstance(x, list):
            out.extend(_keys(x))
        else:
            out.append(x)
    return out


class Prog:
    NDMA = 4

    def __init__(self, nc, es):
        self.nc = nc
        self.es = es
        self.engs = {'pe': nc.tensor, 'act': nc.scalar, 'dve': nc.vector, 'pool': nc.gpsimd, 'sp': nc.sync}
        self.sem = {}
        self.cnt = {}
        for n in self.engs:
            self.sem[n] = es.enter_context(nc.semaphore('s_' + n))
            self.cnt[n] = 0
        self.dq = {}
        for q in ('sp', 'pool', 'act'):
            sems = []
            for i in range(self.NDMA):
                nm = 'd_%s%d' % (q, i)
                self.sem[nm] = es.enter_context(nc.semaphore(nm))
                self.cnt[nm] = 0
                sems.append(nm)
            self.dq[q] = [sems, 0]
        self.seen = {}
        self.rec = None
        self.lw = {}
        self.rd = {}
        self.nwait = 0
        self.ninst = 0
        self.uid = 0
        self.ps = [Tl(es.enter_context(nc.psum_tensor('ps%d' % i, [128, 512], F32)), 'ps%d' % i) for i in range(8)]
        self.ps_rots = {}
        self.ps_pool = list(range(8))

    def _wait(self, eng, deps):
        e = self.engs[eng]
        for s, v in deps:
            if v <= 0 or self.seen.get((eng, s), 0) >= v:
                continue
            e.wait_ge(self.sem[s], v)
            self.nwait += 1
            self.seen[(eng, s)] = v

    def _deps(self, reads, writes):
        deps = {}
        for k in reads:
            if k in self.lw:
                s, v = self.lw[k]
                deps[s] = max(deps.get(s, 0), v)
        for k in writes:
            if k in self.lw:
                s, v = self.lw[k]
                deps[s] = max(deps.get(s, 0), v)
            for s, v in self.rd.get(k, {}).items():
                deps[s] = max(deps.get(s, 0), v)
        return list(deps.items())

    def _mark(self, s, v, reads, writes):
        for k in writes:
            self.lw[k] = (s, v)
            self.rd[k] = {}
        for k in reads:
            d = self.rd.setdefault(k, {})
            d[s] = max(d.get(s, 0), v)

    def op(self, eng, fn, r=(), w=()):
        if self.rec is not None:
            self.rec.append(('op', eng, fn, r, w, None))
            return
        reads, writes = _keys(r), _keys(w)
        self._wait(eng, self._deps(reads, writes))
        inst = fn()
        self.cnt[eng] += 1
        inst.then_inc(self.sem[eng], 1)
        self.ninst += 1
        self._mark(eng, self.cnt[eng], reads, writes)

    def dma(self, q, out, in_, r=(), w=(), **kw):
        if self.rec is not None:
            self.rec.append(('dma', q, (out, in_), r, w, kw))
            return
        reads, writes = _keys(r), _keys(w)
        sems, i = self.dq[q]
        s = sems[i % self.NDMA]
        self.dq[q][1] = i + 1
        deps = self._deps(reads, writes)
        deps.append((s, self.cnt[s]))
        self._wait(q, deps)
        inst = self.engs[q].dma_start(out=out, in_=in_, **kw)
        self.cnt[s] += 16
        inst.then_inc(self.sem[s], 16)
        self.ninst += 1
        self._mark(s, self.cnt[s], reads, writes)

    def record(self, fn):
        self.rec = []
        fn()
        L = self.rec
        self.rec = None
        return L

    def play(self, lists):
        lists = [L for L in lists if L]
        items = []
        for L in lists:
            n = float(len(L))
            items.extend(((i + 0.5) / n, j, i, it) for j, L2 in enumerate([L]) for i, it in enumerate(L))
        tagged = []
        for li, L in enumerate(lists):
            n = float(len(L))
            for i, it in enumerate(L):
                tagged.append(((i + 0.5) / n, li, i, it))
        tagged.sort(key=lambda t: (t[0], t[1], t[2]))
        for _, _, _, it in tagged:
            kind, a, b, r, w, kw = it
            if kind == 'op':
                self.op(a, b, r, w)
            else:
                self.dma(a, b[0], b[1], r, w, **kw)

    def barrier(self):
        for e in self.engs:
            self._wait(e, [(s, v) for s, v in self.cnt.items()])

    def sb(self, stack, name, shape, dt=F32):
        self.uid += 1
        t = stack.enter_context(self.nc.sbuf_tensor('%s_%d' % (name, self.uid), shape, dt))
        return Tl(t, '%s_%d' % (name, self.uid))

    @contextmanager
    def scope(self):
        with ExitStack() as st:
            yield st
            self.barrier()

    def psum(self):
        key = tuple(self.ps_pool)
        rot = self.ps_rots.get(key, 0)
        self.ps_rots[key] = rot + 1
        return self.ps[self.ps_pool[rot % len(self.ps_pool)]]

    def act(self, out, in_, func, bias=0.0, scale=1.0, r=(), w=()):
        nc = self.nc
        self.op('act', lambda: nc.scalar.activation(out=out, in_=in_, func=func, bias=bias, scale=scale), r, w)

    def tt(self, out, in0, in1, op, r=(), w=(), eng='dve'):
        e = self.engs[eng]
        self.op(eng, lambda: e.tensor_tensor(out=out, in0=in0, in1=in1, op=op), r, w)

    def ts(self, out, in0, s1, s2, op0, op1=None, r=(), w=(), eng='dve'):
        e = self.engs[eng]
        if op1 is None:
            self.op(eng, lambda: e.tensor_scalar(out=out, in0=in0, scalar1=s1, scalar2=None, op0=op0), r, w)
        else:
            self.op(eng, lambda: e.tensor_scalar(out=out, in0=in0, scalar1=s1, scalar2=s2, op0=op0, op1=op1), r, w)

    def stt(self, out, in0, scalar, in1, op0, op1, r=(), w=()):
        nc = self.nc
        self.op('dve', lambda: nc.vector.scalar_tensor_tensor(out=out, in0=in0, scalar=scalar, in1=in1, op0=op0, op1=op1), r, w)

    def copy(self, out, in_, r=(), w=(), eng='dve'):
        nc = self.nc
        if eng == 'act':
            self.op('act', lambda: nc.scalar.copy(out=out, in_=in_), r, w)
        else:
            e = self.engs[eng]
            self.op(eng, lambda: e.tensor_copy(out=out, in_=in_), r, w)

    def mm(self, out, pairs, r=(), w=()):
        nc = self.nc
        n = len(pairs)

        def f():
            inst = None
            for i, (lt, rh) in enumerate(pairs):
                inst = nc.tensor.matmul(out, lt, rh, start=(i == 0), stop=(i == n - 1))
            return inst
        self.op('pe', f, r, w)

    def mms(self, groups, r=(), w=()):
        nc = self.nc

        def f():
            inst = None
            for out, pairs in groups:
                n = len(pairs)
                for i, (lt, rh) in enumerate(pairs):
                    inst = nc.tensor.matmul(out, lt, rh, start=(i == 0), stop=(i == n - 1))
            return inst
        self.op('pe', f, r, w)

    def transposes(self, items, ident, r=(), w=()):
        nc = self.nc

        def f():
            inst = None
            for out, in_ in items:
                inst = nc.tensor.transpose(out, in_, ident)
            return inst
        self.op('pe', f, r, w)


def hk(name, r0, nrows, tt, halo=False):
    ks = []
    for c in range(r0 // 128, (r0 + nrows - 1) // 128 + 1):
        ks.append((name, c, tt))
        if halo and tt > 0:
            ks.append((name, c, tt - 1))
    return ks


USED = []


def build(stop_after=None, dbg=(), tlen=4096, layers=(0, 1)):
    global T, NT
    T = tlen
    NT = T // TT
    del USED[:]
    nc = bass.Bass("TRN2", target_bir_lowering=False)
    SHAPES = {'xT': [DM, T], 'w_in': [2, DM, NIN], 'c128': [2, 128, N128], 'c64': [2, 64, N64], 'b_v': [2, 512],
              'gla_wup': [2, 16, 256], 'lru_w_r': [2, 8, 64, 64], 'lru_w_i': [2, 8, 64, 64], 'rwkv_w2': [2, 64, 512],
              'rwkv_a2': [2, 64, 512], 'rwkv_g2': [2, 160, 512], 'p_gla': [2, 512, DM], 'p_lru': [2, 512, DM],
              'p_rwkv': [2, 512, DM], 'w_out': [2, DM, DM], 'ffn_w_gate': [DM, FD], 'ffn_w_up': [DM, FD],
              'ffn_w_down': [FD, DM], 'moe_w_router': [DM, NE], 'moe_w_gate': [NE, DM, FE], 'moe_w_up': [NE, DM, FE],
              'moe_w_down': [NE, FE, DM]}

    class Lazy(dict):
        def __missing__(self, name):
            ap = nc.dram_tensor(name, SHAPES[name], F32, kind="ExternalInput").ap()
            self[name] = ap
            USED.append(name)
            return ap
    IN = Lazy()
    outT = nc.dram_tensor('outT', [DM, T], F32, kind="ExternalOutput").ap()

    SC = {}

    def dsc(name, shape, dt=F32):
        kind = "ExternalOutput" if name in dbg else "Internal"
        SC[name] = nc.dram_tensor(name, shape, dt, kind=kind).ap()
    dsc('hT', [NMIXP, T])
    dsc('vtok', [T, 512])
    dsc('ogT', [512, T], BF16)
    dsc('olT', [512, T], BF16)
    dsc('orT', [512, T], BF16)
    dsc('x1T', [DM, T])
    dsc('x2T', [DM, T])
    dsc('wb_in', [2, DM, NIN], BF16)
    dsc('wb_pg', [2, 512, DM], BF16)
    dsc('wb_pl', [2, 512, DM], BF16)
    dsc('wb_pr', [2, 512, DM], BF16)
    dsc('wb_out', [2, DM, DM], BF16)
    dsc('wb_fg', [DM, FD], BF16)
    dsc('wb_fu', [DM, FD], BF16)
    dsc('wb_fd', [FD, DM], BF16)
    dsc('wb_mg', [NE, DM, FE], BF16)
    dsc('wb_mu', [NE, DM, FE], BF16)
    dsc('wb_md', [NE, FE, DM], BF16)

    with ExitStack() as es:
        p = Prog(nc, es)
        _build_body(nc, p, IN, SC, outT, stop_after, layers)
        p.barrier()
        print('program: ninst', p.ninst, 'nwait', p.nwait)
    return nc


def flat128(ap2d_elems, ap):
    return ap


def conv_weights(nc, p, pairs):
    CW = 7168
    with p.scope() as st:
        fb = [p.sb(st, 'cvf', [128, CW], F32) for _ in range(2)]
        bb = [p.sb(st, 'cvb', [128, CW], BF16) for _ in range(2)]
        rnd = 0
        engs = ['pool', 'act', 'dve']
        for key, src, dst, nel in pairs:
            M = nel // 128
            s2 = src.rearrange("(p m) -> p m", p=128)
            d2 = dst.rearrange("(p m) -> p m", p=128)
            c0 = 0
            while c0 < M:
                cw = min(CW, M - c0)
                f, b = fb[rnd % 2], bb[rnd % 2]
                p.dma('sp', f[:, 0:cw], s2[:, c0:c0 + cw], w=[f])
                p.copy(b[:, 0:cw], f[:, 0:cw], r=[f], w=[b], eng=engs[rnd % 3])
                p.dma('act', d2[:, c0:c0 + cw], b[:, 0:cw], r=[b], w=[('cv', rnd)])
                c0 += cw
                rnd += 1


class ConvStream:
    CW = 1792

    def __init__(self, nc, p, st, pairs):
        self.p = p
        CW = self.CW
        fb = [p.sb(st, 'cvf2', [128, CW]) for _ in range(2)]
        bb = [p.sb(st, 'cvb2', [128, CW], BF16) for _ in range(2)]

        def rec():
            rnd = 0
            for key, src, dst, nel in pairs:
                M = nel // 128
                assert M % CW == 0
                s2 = src.rearrange("(p m) -> p m", p=128)
                d2 = dst.rearrange("(p m) -> p m", p=128)
                for c0 in range(0, M, CW):
                    f, b = fb[rnd % 2], bb[rnd % 2]
                    p.dma('sp', f[:, :], s2[:, c0:c0 + CW], w=[f])
                    p.copy(b[:, :], f[:, :], r=[f], w=[b], eng='act')
                    p.dma('sp', d2[:, c0:c0 + CW], b[:, :], r=[b], w=[('cv2', rnd)])
                    rnd += 1
        self.ops = p.record(rec)
        self.pos = 0

    def take(self, steps_left):
        left = len(self.ops) - self.pos
        if left <= 0:
            return []
        rounds = -(-(left // 3) // max(1, steps_left))
        n = min(left, rounds * 3)
        out = self.ops[self.pos:self.pos + n]
        self.pos += n
        return out

    def flush(self):
        rest = self.ops[self.pos:]
        self.pos = len(self.ops)
        if rest:
            self.p.play([rest])


def _build_body(nc, p, IN, SC, outT, stop_after, layers):
    def fl(ap):
        nd = len(ap.shape)
        if nd == 2:
            return ap.rearrange("a b -> (a b)")
        if nd == 3:
            return ap.rearrange("a b c -> (a b c)")
        return ap

    pairs = []
    for l in layers:
        pairs.append((('wb_in', l), fl(IN['w_in'][l]), fl(SC['wb_in'][l]), DM * NIN))
        pairs.append((('wb_pg', l), fl(IN['p_gla'][l]), fl(SC['wb_pg'][l]), 512 * DM))
        pairs.append((('wb_pl', l), fl(IN['p_lru'][l]), fl(SC['wb_pl'][l]), 512 * DM))
        pairs.append((('wb_pr', l), fl(IN['p_rwkv'][l]), fl(SC['wb_pr'][l]), 512 * DM))
        pairs.append((('wb_out', l), fl(IN['w_out'][l]), fl(SC['wb_out'][l]), DM * DM))
    if 0 in layers:
        pairs.append((('wb_fg', 0), fl(IN['ffn_w_gate']), fl(SC['wb_fg']), DM * FD))
        pairs.append((('wb_fu', 0), fl(IN['ffn_w_up']), fl(SC['wb_fu']), DM * FD))
        pairs.append((('wb_fd', 0), fl(IN['ffn_w_down']), fl(SC['wb_fd']), DM * FD))
    conv_weights(nc, p, pairs)
    es_conv = ExitStack()
    cstream = None
    if 1 in layers:
        mpairs = [(('wb_mg', 0), fl(IN['moe_w_gate']), fl(SC['wb_mg']), NE * DM * FE),
                  (('wb_mu', 0), fl(IN['moe_w_up']), fl(SC['wb_mu']), NE * DM * FE),
                  (('wb_md', 0), fl(IN['moe_w_down']), fl(SC['wb_md']), NE * DM * FE)]
        cstream = ConvStream(nc, p, es_conv, mpairs)
    if stop_after == ('conv', 0):
        return
    for l in layers:
        xin = IN['xT'] if l == 0 else SC['x2T']
        phase1(nc, p, IN, SC, l, xin)
        if stop_after == ('p1', l):
            return
        if mixers(nc, p, IN, SC, l, stop_after, cstream if l == 0 else None):
            return
        if l == 0 and cstream is not None:
            cstream.flush()
            p.barrier()
            es_conv.close()
        last = (l == layers[-1]) and l == 1
        phase_tail(nc, p, IN, SC, l, xin, outT if last else SC['x2T'], 'outT' if last else 'x2T')
        if stop_after == ('tail', l):
            return


def phase1(nc, p, IN, SC, l, xin):
    with p.scope() as st:
        xb = p.sb(st, 'xb', [128, 8, T], BF16)
        xf = [p.sb(st, 'xf', [128, 8, TT], F32) for _ in range(2)]
        c128 = p.sb(st, 'c128', [128, N128])
        p.dma('sp', c128[:, :], IN['c128'][l], w=[c128])
        xin3 = xin.rearrange("(kc p) t -> p kc t", p=128)
        for tt in range(NT):
            f = xf[tt % 2]
            rkeys = [('x2T', c, tt) for c in range(8)] if l > 0 else []
            p.dma('sp', f[:, :, :], xin3[:, :, tt * TT:(tt + 1) * TT], r=rkeys, w=[f])
            p.copy(xb[:, :, tt * TT:(tt + 1) * TT], f[:, :, :], r=[f], w=[('xb', tt)], eng=['act', 'dve'][tt % 2])
        wg = [p.sb(st, 'wg', [128, 8, 512], BF16) for _ in range(2)]
        stg = [p.sb(st, 'stg', [128, TT], F32) for _ in range(4)]
        w3 = SC['wb_in'][l].rearrange("(kc p) n -> p kc n", p=128)
        si = 0
        ngroups = (NMIX + 511) // 512
        for g in range(ngroups):
            n0 = g * 512
            if n0 == V0:
                continue
            gw = min(512, NMIX - n0)
            W = wg[g % 2]
            p.dma('sp', W[:, :, 0:gw], w3[:, :, n0:n0 + gw], w=[W])
            for tt in range(NT):
                for j in range((gw + 127) // 128):
                    cw = min(128, gw - j * 128)
                    ps = p.psum()
                    p.mm(ps[0:cw, :], [(W[:, kc, j * 128:j * 128 + cw], xb[:, kc, tt * TT:(tt + 1) * TT]) for kc in range(8)],
                         r=[W, ('xb', tt)], w=[ps])
                    s = stg[si % 4]
                    si += 1
                    row = n0 + j * 128
                    col = C128['b_in'] + row // 128
                    p.act(s[0:cw, :], ps[0:cw, :], AF.Identity, bias=c128[0:cw, col:col + 1], r=[ps, c128], w=[s])
                    p.dma('sp', SC['hT'][row:row + cw, tt * TT:(tt + 1) * TT], s[0:cw, :], r=[s], w=[('hT', row // 128, tt)])
        bv = p.sb(st, 'bv', [128, 512])
        p.dma('sp', bv[:, :], IN['b_v'][l].partition_broadcast(128), w=[bv])
        W = wg[ngroups % 2]
        p.dma('sp', W[:, :, :], w3[:, :, V0:V0 + 512], w=[W])
        for tb in range(T // 128):
            ps = p.psum()
            p.mm(ps[:, :], [(xb[:, kc, tb * 128:(tb + 1) * 128], W[:, kc, :]) for kc in range(8)], r=[W, ('xb', tb // 4)], w=[ps])
            s = stg[si % 4]
            si += 1
            p.tt(s[:, :], ps[:, :], bv[:, :], ALU.add, r=[ps, bv], w=[s])
            p.dma('sp', SC['vtok'][tb * 128:(tb + 1) * 128, :], s[:, :], r=[s], w=[('vtok', tb)])


def make_in_maps(inputs):
    inp = {k: np.asarray(v) for k, v in inputs.items()}
    cc = [pack_cols(inp, l) for l in range(2)]
    shared = {
        'w_in': inp['w_in'],
        'c128': np.stack([cc[0][0], cc[1][0]]),
        'c64': np.stack([cc[0][1], cc[1][1]]),
        'b_v': np.ascontiguousarray(inp['b_in'][:, V0:V0 + 512]),
        'gla_wup': inp['gla_w_decay_up'],
        'lru_w_r': inp['lru_w_r'], 'lru_w_i': inp['lru_w_i'],
        'rwkv_w2': inp['rwkv_w2'], 'rwkv_a2': inp['rwkv_a2'], 'rwkv_g2': inp['rwkv_g2'],
        'p_gla': inp['p_gla'], 'p_lru': inp['p_lru'], 'p_rwkv': inp['p_rwkv'], 'w_out': inp['w_out'],
        'ffn_w_gate': inp['ffn_w_gate'][0], 'ffn_w_up': inp['ffn_w_up'][0], 'ffn_w_down': inp['ffn_w_down'][0],
        'moe_w_router': inp['moe_w_router'][0], 'moe_w_gate': inp['moe_w_gate'][0], 'moe_w_up': inp['moe_w_up'][0],
        'moe_w_down': inp['moe_w_down'][0],
    }
    shared = {k: np.ascontiguousarray(v, dtype=np.float32) for k, v in shared.items()}
    maps = []
    for b in range(8):
        m = {k: v for k, v in shared.items() if k in USED}
        m['xT'] = np.ascontiguousarray(inp['x'][b, :T].T)
        maps.append(m)
    return maps


def kernel(**inputs):
    nc = build()
    maps = make_in_maps(inputs)
    res = run_bass_kernel_spmd(nc, maps, core_ids=list(range(8)))
    out = np.stack([np.ascontiguousarray(res.results[b]['outT'].T) for b in range(8)])
    return out.astype(np.float32)


class Consts:
    pass


def make_consts(nc, p, st):
    c = Consts()
    c.ident = p.sb(st, 'ident', [64, 64])
    c.ones64 = p.sb(st, 'ones64', [64, 64])
    c.onesm128 = p.sb(st, 'onesm128', [128, 128])
    c.onesm64 = p.sb(st, 'onesm64', [64, 64])
    c.m01 = p.sb(st, 'm01', [64, TT])
    c.mge = p.sb(st, 'mge', [64, NCH, CH])
    c.mgt = p.sb(st, 'mgt', [64, NCH, CH])
    c.mlt = p.sb(st, 'mlt', [64, NCH, CH])
    c.id8 = p.sb(st, 'id8', [64, NCH, CH])
    ones8 = p.sb(st, 'ones8', [64, NCH, CH])
    p.op('pool', lambda: nc.gpsimd.memset(c.ones64[:, :], 1.0), w=[c.ones64])
    p.op('pool', lambda: nc.gpsimd.memset(c.onesm128[:, :], 1.0 / 128.0), w=[c.onesm128])
    p.op('pool', lambda: nc.gpsimd.memset(c.onesm64[:, :], 1.0 / 64.0), w=[c.onesm64])
    p.op('pool', lambda: nc.gpsimd.memset(ones8[:, :, :], 1.0), w=[ones8])
    p.op('pool', lambda: nc.gpsimd.memset(c.m01[:, :], 1.0), w=[c.m01])
    m3 = c.m01[:, :].rearrange("p (c t) -> p c t", t=CH)
    p.op('pool', lambda: nc.gpsimd.memset(m3[:, :, 0:1], 0.0), r=[c.m01], w=[c.m01])
    pat = [[0, NCH], [1, CH]]
    p.op('pool', lambda: nc.gpsimd.affine_select(out=c.mge[:, :, :], in_=ones8[:, :, :], pattern=pat, compare_op=ALU.is_ge,
                                                 fill=0.0, base=0, channel_multiplier=-1), r=[ones8], w=[c.mge])
    p.op('pool', lambda: nc.gpsimd.affine_select(out=c.mgt[:, :, :], in_=ones8[:, :, :], pattern=pat, compare_op=ALU.is_gt,
                                                 fill=0.0, base=0, channel_multiplier=-1), r=[ones8], w=[c.mgt])
    pat2 = [[0, NCH], [-1, CH]]
    p.op('pool', lambda: nc.gpsimd.affine_select(out=c.mlt[:, :, :], in_=ones8[:, :, :], pattern=pat2, compare_op=ALU.is_gt,
                                                 fill=0.0, base=0, channel_multiplier=1), r=[ones8], w=[c.mlt])
    p.tt(c.id8[:, :, :], c.mge[:, :, :], c.mgt[:, :, :], ALU.subtract, r=[c.mge, c.mgt], w=[c.id8])
    p.copy(c.ident[:, :], c.id8[:, 0, :], r=[c.id8], w=[c.ident])
    return c


def cview(t):
    return t[:, :].rearrange("p (c t) -> p c t", t=CH)


def fm_norm(nc, p, st, cst, ps, P, eps, gcol, bcol, tag, tiles=None):
    ones = cst.onesm128 if P == 128 else cst.onesm64
    if tiles is not None:
        o, osq, mean, msq = tiles
    else:
        o = p.sb(st, tag + 'o', [P, TT])
        osq = p.sb(st, tag + 'osq', [P, TT])
    p.copy(o[:, :], ps[0:P, :], r=[ps], w=[o], eng='act')
    p.act(osq[:, :], ps[0:P, :], AF.Square, r=[ps], w=[osq])
    psM = p.psum()
    p.mm(psM[0:P, :], [(ones[:, :], o[:, :])], r=[ones, o], w=[psM])
    psQ = p.psum()
    p.mm(psQ[0:P, :], [(ones[:, :], osq[:, :])], r=[ones, osq], w=[psQ])
    if tiles is None:
        mean = p.sb(st, tag + 'mean', [P, TT])
        msq = p.sb(st, tag + 'msq', [P, TT])
    p.copy(mean[:, :], psM[0:P, :], r=[psM], w=[mean], eng='act')
    p.act(msq[:, :], psM[0:P, :], AF.Square, r=[psM], w=[msq])
    var = osq
    p.tt(var[:, :], psQ[0:P, :], msq[:, :], ALU.subtract, r=[psQ, msq], w=[var])
    p.ts(var[:, :], var[:, :], 0.0, eps, ALU.max, ALU.add, r=[var], w=[var])
    p.act(msq[:, :], var[:, :], AF.Sqrt, r=[var], w=[msq])
    p.op('dve', lambda: nc.vector.reciprocal(out=var[:, :], in_=msq[:, :]), r=[msq], w=[var])
    p.tt(o[:, :], o[:, :], mean[:, :], ALU.subtract, r=[o, mean], w=[o])
    p.tt(o[:, :], o[:, :], var[:, :], ALU.mult, r=[o, var], w=[o])
    p.ts(o[:, :], o[:, :], gcol, bcol, ALU.mult, ALU.add, r=[o], w=[o])
    return o


def phase_gla(nc, p, IN, SC, l, cst, c128, c64, st0):
    p.ps_pool = [0, 1, 2, 3, 4]
    if True:
        wup = p.sb(st0, 'wup', [16, 256])
        p.dma('sp', wup[:, :], IN['gla_wup'][l], w=[wup])
        negbd = p.sb(st0, 'negbd', [64, 4])
        p.ts(negbd[:, :], c64[:, C64['gla_bd']:C64['gla_bd'] + 4], -1.0, None, ALU.mult, r=[c64], w=[negbd])
        S = [p.sb(st0, 'S%d' % h, [64, NCH + 1, 128]) for h in range(4)]
        for h in range(4):
            p.op('pool', lambda h=h: nc.gpsimd.memset(S[h][:, 0, :], 0.0), w=[S[h]])
        G = {}
        for nm, shp, dt in [('dec', [16, TT], F32), ('q', [64, TT], F32), ('k', [64, TT], F32), ('v', [64, NCH, 128], F32),
                            ('r', [128, TT], F32), ('l1', [64, TT], F32), ('cum', [64, TT], F32), ('E', [64, TT], F32),
                            ('qd', [64, TT], F32), ('ki', [64, TT], F32), ('ke', [64, TT], F32), ('D', [64, NCH, CH], F32),
                            ('dend', [64, NCH, 1], F32), ('keT', [64, TT], F32), ('sc', [64, TT], F32), ('dst', [64, NCH, 128], F32),
                            ('no', [128, TT], F32), ('nosq', [128, TT], F32), ('nmean', [128, TT], F32), ('nmsq', [128, TT], F32),
                            ('og', [128, TT], BF16)]:
            G[nm] = p.sb(st0, 'g_' + nm, shp, dt)
        for tt in range(NT):
            tsl = slice(tt * TT, (tt + 1) * TT)
            for h in range(4):
                if True:
                    st = None
                    dec = G['dec']
                    p.dma('sp', dec[:, :], SC['hT'][DEC0:DEC0 + 16, tsl], r=hk('hT', DEC0, 16, tt), w=[dec])
                    q = G['q']
                    k = G['k']
                    v = G['v']
                    r = G['r']
                    p.dma('sp', q[:, :], SC['hT'][Q0 + h * 64:Q0 + (h + 1) * 64, tsl], r=hk('hT', Q0 + h * 64, 64, tt), w=[q])
                    p.dma('sp', k[:, :], SC['hT'][K0 + h * 64:K0 + (h + 1) * 64, tsl], r=hk('hT', K0 + h * 64, 64, tt), w=[k])
                    p.dma('sp', v[:, :, :], SC['vtok'][tsl, h * 128:(h + 1) * 128].rearrange("(c t) e -> t c e", t=CH),
                          r=[('vtok', tb) for tb in range(tt * 4, tt * 4 + 4)], w=[v])
                    p.dma('sp', r[:, :], SC['hT'][R0 + h * 128:R0 + (h + 1) * 128, tsl], r=hk('hT', R0 + h * 128, 128, tt), w=[r])
                    ps = p.psum()
                    p.mm(ps[0:64, :], [(wup[:, h * 64:(h + 1) * 64], dec[:, :])], r=[wup, dec], w=[ps])
                    l1 = G['l1']
                    p.act(l1[:, :], ps[0:64, :], AF.Exp, bias=negbd[:, h:h + 1], scale=-1.0, r=[ps, negbd], w=[l1])
                    p.act(l1[:, :], l1[:, :], AF.Ln, bias=1.0, scale=1.0, r=[l1], w=[l1])
                    cum = G['cum']
                    p.op('dve', lambda: nc.vector.tensor_tensor_scan(out=cum[:, :], data0=cst.m01[:, :], data1=l1[:, :], initial=0.0,
                                                                     op0=ALU.mult, op1=ALU.add), r=[cst.m01, l1], w=[cum])
                    E = G['E']
                    qd = G['qd']
                    ki = G['ki']
                    ke = G['ke']
                    p.act(E[:, :], cum[:, :], AF.Exp, scale=-1.0 / 16.0, r=[cum], w=[E])
                    p.stt(qd[:, :], q[:, :], 0.125, E[:, :], ALU.mult, ALU.mult, r=[q, E], w=[qd])
                    p.act(E[:, :], cum[:, :], AF.Exp, scale=1.0 / 16.0, r=[cum], w=[E])
                    p.tt(ki[:, :], k[:, :], E[:, :], ALU.mult, r=[k, E], w=[ki])
                    c3 = cview(cum)
                    D = G['D']
                    p.tt(D[:, :, :], c3[:, :, CH - 1:CH].to_broadcast([64, NCH, CH]), c3, ALU.subtract, r=[cum], w=[D])
                    p.act(D[:, :, :], D[:, :, :], AF.Exp, scale=-1.0 / 16.0, r=[D], w=[D])
                    p.tt(ke[:, :], k[:, :], D[:, :, :].rearrange("p c t -> p (c t)"), ALU.mult, r=[k, D], w=[ke])
                    dend = G['dend']
                    p.act(dend[:, :, :], c3[:, :, CH - 1:CH], AF.Exp, scale=-1.0 / 16.0, r=[cum], w=[dend])
                    psT = p.psum()
                    p.transposes([(psT[0:64, c * 64:(c + 1) * 64], ke[:, c * 64:(c + 1) * 64]) for c in range(NCH)], cst.ident[:, :],
                                 r=[ke, cst.ident], w=[psT])
                    keT = G['keT']
                    p.copy(keT[:, :], psT[0:64, :], r=[psT], w=[keT], eng='act')
                    psS = p.psum()
                    p.mms([(psS[0:64, c * 64:(c + 1) * 64], [(ki[:, c * 64:(c + 1) * 64], qd[:, c * 64:(c + 1) * 64])]) for c in range(NCH)],
                          r=[ki, qd], w=[psS])
                    sc = G['sc']
                    p.tt(sc[:, :], psS[0:64, :], cst.mge[:, :, :].rearrange("p c t -> p (c t)"), ALU.mult, r=[psS, cst.mge], w=[sc])
                    dst = G['dst']
                    for half in range(2):
                        psD = p.psum()
                        p.mms([(psD[0:64, cc * 128:(cc + 1) * 128], [(keT[:, (half * 4 + cc) * 64:(half * 4 + cc + 1) * 64], v[:, half * 4 + cc, :])])
                               for cc in range(4)], r=[keT, v], w=[psD])
                        p.copy(dst[:, half * 4:half * 4 + 4, :].rearrange("p c e -> p (c e)"), psD[0:64, :], r=[psD], w=[dst],
                               eng=['act', 'dve'][half])
                    for c in range(NCH):
                        p.stt(S[h][:, c + 1, :], S[h][:, c, :], dend[:, c, :], dst[:, c, :], ALU.mult, ALU.add, r=[S[h], dend, dst], w=[S[h]])
                    psO = p.psum()
                    p.mms([(psO[:, c * 64:(c + 1) * 64], [(v[:, c, :], sc[:, c * 64:(c + 1) * 64]), (S[h][:, c, :], qd[:, c * 64:(c + 1) * 64])])
                           for c in range(NCH)], r=[v, sc, S[h], qd], w=[psO])
                    p.copy(S[h][:, 0, :], S[h][:, NCH, :], r=[S[h]], w=[S[h]], eng='pool')
                    y = fm_norm(nc, p, st, cst, psO, 128, 1e-5, c128[:, C128['gla_ng'] + h:C128['gla_ng'] + h + 1],
                                c128[:, C128['gla_nb'] + h:C128['gla_nb'] + h + 1], 'gn', tiles=(G['no'], G['nosq'], G['nmean'], G['nmsq']))
                    p.act(r[:, :], r[:, :], AF.Silu, r=[r], w=[r])
                    og = G['og']
                    p.tt(og[:, :], y[:, :], r[:, :], ALU.mult, r=[y, r], w=[og])
                    p.dma('sp', SC['ogT'][h * 128:(h + 1) * 128, tsl], og[:, :], r=[og], w=[('ogT', h, tt)])


def phase_lru(nc, p, IN, SC, l, cst, c128, c64, st0):
    p.ps_pool = [5, 6, 7]
    if True:
        wr = p.sb(st0, 'wr', [128, 4, 128])
        wi = p.sb(st0, 'wi', [128, 4, 128])
        for wt, nm in ((wr, 'lru_w_r'), (wi, 'lru_w_i')):
            p.op('pool', lambda wt=wt: nc.gpsimd.memset(wt[:, :, :], 0.0), w=[wt])
            for g in range(8):
                o = (g % 2) * 64
                p.dma('sp', wt[o:o + 64, g // 2, o:o + 64], IN[nm][l, g], r=[wt], w=[wt])
        cex = p.sb(st0, 'cex', [128, 4])
        lam = c128[:, C128['lam']:C128['lam'] + 4]
        p.act(cex[:, :], lam, AF.Exp, scale=-1.0, r=[c128], w=[cex])
        p.act(cex[:, :], cex[:, :], AF.Ln, bias=1.0, r=[cex], w=[cex])
        p.ts(cex[:, :], cex[:, :], -8.0, None, ALU.mult, r=[cex], w=[cex])
        hprev = p.sb(st0, 'hprev', [128, 4])
        p.op('pool', lambda: nc.gpsimd.memset(hprev[:, :], 0.0), w=[hprev])
        LT = {}
        for nm, shp, dt in [('xh', [128, TT + 3], F32), ('gate', [128, TT], F32), ('xc', [128, TT], F32), ('a', [128, TT], F32),
                            ('ii', [128, TT], F32), ('ml', [128, TT], F32), ('hh', [128, TT], F32), ('ol', [128, TT], BF16)]:
            LT[nm] = p.sb(st0, 'l_' + nm, shp, dt)
        for tt in range(NT):
            t0 = tt * TT
            tsl = slice(t0, t0 + TT)
            for g in range(4):
                if True:
                    xh = LT['xh']
                    rows = slice(LX0 + g * 128, LX0 + (g + 1) * 128)
                    if tt == 0:
                        p.op('pool', lambda: nc.gpsimd.memset(xh[:, 0:3], 0.0), w=[xh])
                        p.dma('sp', xh[:, 3:], SC['hT'][rows, tsl], r=hk('hT', LX0 + g * 128, 128, tt), w=[xh])
                    else:
                        p.dma('sp', xh[:, :], SC['hT'][rows, t0 - 3:t0 + TT], r=hk('hT', LX0 + g * 128, 128, tt, True), w=[xh])
                    gate = LT['gate']
                    p.dma('sp', gate[:, :], SC['hT'][LG0 + g * 128:LG0 + (g + 1) * 128, tsl], r=hk('hT', LG0 + g * 128, 128, tt), w=[gate])
                    xc = LT['xc']
                    cw = C128['conv_w']
                    p.ts(xc[:, :], xh[:, 0:TT], c128[:, cw + g:cw + g + 1], c128[:, C128['conv_b'] + g:C128['conv_b'] + g + 1],
                         ALU.mult, ALU.add, r=[xh, c128], w=[xc])
                    for j in range(1, 4):
                        p.stt(xc[:, :], xh[:, j:j + TT], c128[:, cw + j * 4 + g:cw + j * 4 + g + 1], xc[:, :], ALU.mult, ALU.add,
                              r=[xh, xc, c128], w=[xc])
                    psr = p.psum()
                    p.mm(psr[:, :], [(wr[:, g, :], xc[:, :])], r=[wr, xc], w=[psr])
                    psi = p.psum()
                    p.mm(psi[:, :], [(wi[:, g, :], xc[:, :])], r=[wi, xc], w=[psi])
                    a = LT['a']
                    ii = LT['ii']
                    ml = LT['ml']
                    p.act(a[:, :], psr[:, :], AF.Sigmoid, bias=c128[:, C128['b_r'] + g:C128['b_r'] + g + 1], r=[psr, c128], w=[a])
                    p.act(ii[:, :], psi[:, :], AF.Sigmoid, bias=c128[:, C128['b_i'] + g:C128['b_i'] + g + 1], r=[psi, c128], w=[ii])
                    p.act(a[:, :], a[:, :], AF.Exp, scale=cex[:, g:g + 1], r=[a, cex], w=[a])
                    p.act(ml[:, :], a[:, :], AF.Square, r=[a], w=[ml])
                    p.act(ml[:, :], ml[:, :], AF.Sqrt, bias=1.0, scale=-1.0, r=[ml], w=[ml])
                    p.tt(ii[:, :], ii[:, :], xc[:, :], ALU.mult, r=[ii, xc], w=[ii])
                    p.tt(ii[:, :], ii[:, :], ml[:, :], ALU.mult, r=[ii, ml], w=[ii])
                    hh = LT['hh']
                    p.op('dve', lambda g=g: nc.vector.tensor_tensor_scan(out=hh[:, :], data0=a[:, :], data1=ii[:, :], initial=hprev[:, g:g + 1],
                                                                     op0=ALU.mult, op1=ALU.add), r=[a, ii, hprev], w=[hh])
                    p.copy(hprev[:, g:g + 1], hh[:, TT - 1:TT], r=[hh], w=[hprev], eng='dve')
                    p.act(ml[:, :], gate[:, :], AF.Square, r=[gate], w=[ml])
                    p.ts(ml[:, :], ml[:, :], 0.044715, 1.0, ALU.mult, ALU.add, r=[ml], w=[ml])
                    p.tt(ml[:, :], ml[:, :], gate[:, :], ALU.mult, r=[ml, gate], w=[ml])
                    p.act(ml[:, :], ml[:, :], AF.Sigmoid, scale=1.5957691216057308, r=[ml], w=[ml])
                    p.tt(ml[:, :], ml[:, :], gate[:, :], ALU.mult, r=[ml, gate], w=[ml])
                    ol = LT['ol']
                    p.tt(ol[:, :], ml[:, :], hh[:, :], ALU.mult, r=[ml, hh], w=[ol])
                    p.dma('sp', SC['olT'][g * 128:(g + 1) * 128, tsl], ol[:, :], r=[ol], w=[('olT', g, tt)])


def phase_rwkv(nc, p, IN, SC, l, cst, c128, c64):
    YB = 7
    p.ps_pool = [0, 1, 2, 3, 4, 5, 6]
    with p.scope() as st0:
        w2 = p.sb(st0, 'w2', [64, 512])
        a2 = p.sb(st0, 'a2', [64, 512])
        g2a = p.sb(st0, 'g2a', [128, 512])
        g2b = p.sb(st0, 'g2b', [32, 512])
        p.dma('sp', w2[:, :], IN['rwkv_w2'][l], w=[w2])
        p.dma('sp', a2[:, :], IN['rwkv_a2'][l], w=[a2])
        p.dma('sp', g2a[:, :], IN['rwkv_g2'][l, 0:128, :], w=[g2a])
        p.dma('sp', g2b[:, :], IN['rwkv_g2'][l, 128:160, :], w=[g2b])
        nmu = C64['mu_a'] + 1 - C64['mu_r']
        omu = p.sb(st0, 'omu', [64, N64])
        p.ts(omu[:, :], c64[:, :], -1.0, 1.0, ALU.mult, ALU.add, r=[c64], w=[omu])
        omug = p.sb(st0, 'omug', [128, 2])
        p.ts(omug[:, :], c128[:, C128['mu_g']:C128['mu_g'] + 2], -1.0, 1.0, ALU.mult, ALU.add, r=[c128], w=[omug])
        Tst = [p.sb(st0, 'Tst%d' % h, [64, 64]) for h in range(8)]
        for h in range(8):
            p.op('pool', lambda: nc.gpsimd.memset(Tst[h][:, :], 0.0), w=[Tst[h]])
        psY = p.ps[YB]

        def load_shift(st, tt, row0, nrows, mu_ap, omu_ap, rd, tag):
            t0 = tt * TT
            raw = p.sb(st, tag + 'raw', [nrows, TT + 1])
            if tt == 0:
                p.op('pool', lambda: nc.gpsimd.memset(raw[:, 0:1], 0.0), w=[raw])
                p.dma('sp', raw[:, 1:], SC['hT'][row0:row0 + nrows, 0:TT], r=hk('hT', row0, nrows, tt), w=[raw])
            else:
                p.dma('sp', raw[:, :], SC['hT'][row0:row0 + nrows, t0 - 1:t0 + TT], r=hk('hT', row0, nrows, tt, True), w=[raw])
            out = p.sb(st, tag, [nrows, TT])
            p.ts(out[:, :], raw[:, 1:TT + 1], omu_ap, None, ALU.mult, r=[raw] + rd, w=[out])
            p.stt(out[:, :], raw[:, 0:TT], mu_ap, out[:, :], ALU.mult, ALU.add, r=[raw, out] + rd, w=[out])
            return out

        def col(name, h=0):
            return c64[:, C64[name] + h:C64[name] + h + 1]

        def ocol(name, h=0):
            return omu[:, C64[name] + h:C64[name] + h + 1]

        for tt in range(NT):
            tsl = slice(tt * TT, (tt + 1) * TT)
            with p.scope() as stt_:
                wl = load_shift(stt_, tt, RWW, 64, col('mu_w'), ocol('mu_w'), [c64, omu], 'wl')
                al = load_shift(stt_, tt, RWA, 64, col('mu_a'), ocol('mu_a'), [c64, omu], 'al')
                mg = C128['mu_g']
                gl1 = load_shift(stt_, tt, RWG, 128, c128[:, mg:mg + 1], omug[:, 0:1], [c128, omug], 'gl1')
                gl2 = load_shift(stt_, tt, RWG + 128, 32, c128[0:32, mg + 1:mg + 2], omug[0:32, 1:2], [c128, omug], 'gl2')
                p.act(wl[:, :], wl[:, :], AF.Tanh, r=[wl], w=[wl])
                p.act(gl1[:, :], gl1[:, :], AF.Sigmoid, r=[gl1], w=[gl1])
                p.act(gl2[:, :], gl2[:, :], AF.Sigmoid, r=[gl2], w=[gl2])
                for h in range(8):
                    hs = slice(h * 64, (h + 1) * 64)
                    with p.scope() as st:
                        r = load_shift(st, tt, RWR + h * 64, 64, col('mu_r', h), ocol('mu_r', h), [c64, omu], 'r')
                        k = load_shift(st, tt, RWK + h * 64, 64, col('mu_k', h), ocol('mu_k', h), [c64, omu], 'k')
                        v = load_shift(st, tt, RWV + h * 64, 64, col('mu_v', h), ocol('mu_v', h), [c64, omu], 'v')

                        def new(tag, shape=None, dt=F32):
                            return p.sb(st, tag, shape or [64, TT], dt)
                        ps = p.psum()
                        p.mm(ps[0:64, :], [(w2[:, hs], wl[:, :])], r=[w2, wl], w=[ps])
                        sgm = new('sgm')
                        p.act(sgm[:, :], ps[0:64, :], AF.Sigmoid, bias=col('w0', h), r=[ps, c64], w=[sgm])
                        cum = new('cum')
                        p.op('dve', lambda: nc.vector.tensor_tensor_scan(out=cum[:, :], data0=cst.m01[:, :], data1=sgm[:, :], initial=0.0,
                                                                         op0=ALU.mult, op1=ALU.add), r=[cst.m01, sgm], w=[cum])
                        ps = p.psum()
                        p.mm(ps[0:64, :], [(a2[:, hs], al[:, :])], r=[a2, al], w=[ps])
                        ag = new('ag')
                        p.act(ag[:, :], ps[0:64, :], AF.Sigmoid, bias=col('a0', h), r=[ps, c64], w=[ag])
                        ps = p.psum()
                        p.mm(ps[0:64, :], [(g2a[:, hs], gl1[:, :]), (g2b[:, hs], gl2[:, :])], r=[g2a, g2b, gl1, gl2], w=[ps])
                        gh = new('gh')
                        p.copy(gh[:, :], ps[0:64, :], r=[ps], w=[gh], eng='act')
                        kk = new('kk')
                        tmp = new('tmp')
                        p.ts(kk[:, :], k[:, :], col('k_k', h), None, ALU.mult, r=[k, c64], w=[kk])
                        p.act(tmp[:, :], kk[:, :], AF.Square, r=[kk], w=[tmp])
                        ps = p.psum()
                        p.mm(ps[0:64, :], [(cst.ones64[:, :], tmp[:, :])], r=[cst.ones64, tmp], w=[ps])
                        p.act(tmp[:, :], ps[0:64, :], AF.Sqrt, r=[ps], w=[tmp])
                        p.ts(tmp[:, :], tmp[:, :], 1e-12, None, ALU.max, r=[tmp], w=[tmp])
                        p.op('dve', lambda: nc.vector.reciprocal(out=tmp[:, :], in_=tmp[:, :]), r=[tmp], w=[tmp])
                        p.tt(kk[:, :], kk[:, :], tmp[:, :], ALU.mult, r=[kk, tmp], w=[kk])
                        p.ts(tmp[:, :], ag[:, :], col('k_a', h), ocol('k_a', h), ALU.mult, ALU.add, r=[ag, c64, omu], w=[tmp])
                        k2 = new('k2')
                        p.tt(k2[:, :], k[:, :], tmp[:, :], ALU.mult, r=[k, tmp], w=[k2])
                        bv = new('bv')
                        p.tt(bv[:, :], kk[:, :], ag[:, :], ALU.mult, r=[kk, ag], w=[bv])
                        E = new('E')
                        rt, kt, bt, at, kh, bh = new('rt'), new('kt'), new('bt'), new('at'), new('kh'), new('bh')
                        p.act(E[:, :], cum[:, :], AF.Exp, scale=-C0, r=[cum], w=[E])
                        p.tt(rt[:, :], r[:, :], E[:, :], ALU.mult, r=[r, E], w=[rt])
                        p.act(E[:, :], cum[:, :], AF.Exp, scale=C0, r=[cum], w=[E])
                        p.tt(kt[:, :], k2[:, :], E[:, :], ALU.mult, r=[k2, E], w=[kt])
                        p.tt(bt[:, :], bv[:, :], E[:, :], ALU.mult, r=[bv, E], w=[bt])
                        p.tt(tmp[:, :], cum[:, :], sgm[:, :], ALU.subtract, r=[cum, sgm], w=[tmp])
                        p.act(E[:, :], tmp[:, :], AF.Exp, scale=-C0, r=[tmp], w=[E])
                        p.stt(at[:, :], kk[:, :], -1.0, E[:, :], ALU.mult, ALU.mult, r=[kk, E], w=[at])
                        c3 = cview(cum)
                        D = new('D', [64, NCH, CH])
                        p.tt(D[:, :, :], c3[:, :, CH - 1:CH].to_broadcast([64, NCH, CH]), c3, ALU.subtract, r=[cum], w=[D])
                        p.act(D[:, :, :], D[:, :, :], AF.Exp, scale=-C0, r=[D], w=[D])
                        Df = D[:, :, :].rearrange("p c t -> p (c t)")
                        p.tt(kh[:, :], k2[:, :], Df, ALU.mult, r=[k2, D], w=[kh])
                        p.tt(bh[:, :], bv[:, :], Df, ALU.mult, r=[bv, D], w=[bh])
                        WC = new('WC', [64, NCH, 1])
                        p.act(WC[:, :, :], c3[:, :, CH - 1:CH], AF.Exp, scale=-C0, r=[cum], w=[WC])
                        p.stt(tmp[:, :], r[:, :], col('r_k', h), k2[:, :], ALU.mult, ALU.mult, r=[r, k2, c64], w=[tmp])
                        ps = p.psum()
                        p.mm(ps[0:64, :], [(cst.ones64[:, :], tmp[:, :])], r=[cst.ones64, tmp], w=[ps])
                        bonus = new('bonus')
                        p.tt(bonus[:, :], ps[0:64, :], v[:, :], ALU.mult, r=[ps, v], w=[bonus])
                        toks = []
                        for src, tag in ((v, 'vT'), (kh, 'khT'), (bh, 'bhT')):
                            psT = p.psum()
                            p.transposes([(psT[0:64, c * 64:(c + 1) * 64], src[:, c * 64:(c + 1) * 64]) for c in range(NCH)], cst.ident[:, :],
                                         r=[src, cst.ident], w=[psT])
                            d = new(tag)
                            p.copy(d[:, :], psT[0:64, :], r=[psT], w=[d], eng='act')
                            toks.append(d)
                        vT, khT, bhT = toks

                        def amat(lt, rh, mask, tag):
                            psA = p.psum()
                            p.mms([(psA[0:64, c * 64:(c + 1) * 64], [(lt[:, c * 64:(c + 1) * 64], rh[:, c * 64:(c + 1) * 64])]) for c in range(NCH)],
                                  r=[lt, rh], w=[psA])
                            d = new(tag)
                            p.tt(d[:, :], psA[0:64, :], mask[:, :, :].rearrange("p c t -> p (c t)"), ALU.mult, r=[psA, mask], w=[d])
                            return d
                        AabT = amat(bt, at, cst.mgt, 'AabT')
                        ArbT = amat(bt, rt, cst.mge, 'ArbT')
                        AakT = amat(kt, at, cst.mgt, 'AakT')
                        ArkT = amat(kt, rt, cst.mge, 'ArkT')
                        Aab = amat(at, bt, cst.mlt, 'Aab')
                        P_, Q_ = Aab, AabT
                        N_ = new('N0')
                        p.tt(N_[:, :], Q_[:, :], cst.id8[:, :, :].rearrange("p c t -> p (c t)"), ALU.add, r=[Q_, cst.id8], w=[N_])
                        for lev in range(5):
                            psP = p.psum()
                            p.mms([(psP[0:64, c * 64:(c + 1) * 64], [(Q_[:, c * 64:(c + 1) * 64], P_[:, c * 64:(c + 1) * 64])]) for c in range(NCH)],
                                  r=[P_, Q_], w=[psP])
                            if lev < 4:
                                psQ = p.psum()
                                p.mms([(psQ[0:64, c * 64:(c + 1) * 64], [(P_[:, c * 64:(c + 1) * 64], Q_[:, c * 64:(c + 1) * 64])]) for c in range(NCH)],
                                      r=[P_, Q_], w=[psQ])
                            P2 = new('P%d' % lev)
                            p.copy(P2[:, :], psP[0:64, :], r=[psP], w=[P2], eng='act')
                            if lev < 4:
                                Q2 = new('Q%d' % lev)
                                p.copy(Q2[:, :], psQ[0:64, :], r=[psQ], w=[Q2], eng='dve')
                            psN = p.psum()
                            p.mms([(psN[0:64, c * 64:(c + 1) * 64], [(P2[:, c * 64:(c + 1) * 64], N_[:, c * 64:(c + 1) * 64])]) for c in range(NCH)],
                                  r=[P2, N_], w=[psN])
                            N2 = new('N%d' % (lev + 1))
                            p.tt(N2[:, :], N_[:, :], psN[0:64, :], ALU.add, r=[N_, psN], w=[N2])
                            P_, N_ = P2, N2
                            if lev < 4:
                                Q_ = Q2
                        X = new('X', [64, 64])
                        U = new('U', [64, 64])
                        for c in range(NCH):
                            cs = slice(c * 64, (c + 1) * 64)
                            psX = p.psum()
                            p.mm(psX[0:64, 0:64], [(at[:, cs], Tst[h][:, :]), (AakT[:, cs], vT[:, cs])], r=[at, Tst[h], AakT, vT], w=[psX])
                            p.copy(X[:, :], psX[0:64, 0:64], r=[psX], w=[X], eng='act')
                            psU = p.psum()
                            p.mm(psU[0:64, 0:64], [(N_[:, cs], X[:, :])], r=[N_, X], w=[psU])
                            p.copy(U[:, :], psU[0:64, 0:64], r=[psU], w=[U], eng='dve')
                            p.mm(psY[0:64, cs], [(Tst[h][:, :], rt[:, cs]), (U[:, :], ArbT[:, cs]), (vT[:, cs], ArkT[:, cs])],
                                 r=[Tst[h], rt, U, ArbT, vT, ArkT], w=[psY])
                            psT2 = p.psum()
                            p.mm(psT2[0:64, 0:64], [(bhT[:, cs], U[:, :]), (khT[:, cs], vT[:, cs])], r=[bhT, U, khT, vT], w=[psT2])
                            p.stt(Tst[h][:, :], Tst[h][:, :], WC[:, c, :], psT2[0:64, 0:64], ALU.mult, ALU.add, r=[Tst[h], WC, psT2], w=[Tst[h]])
                        y = fm_norm(nc, p, st, cst, psY, 64, 64e-5, col('lnx_g', h), col('lnx_b', h), 'rn')
                        p.tt(y[:, :], y[:, :], bonus[:, :], ALU.add, r=[y, bonus], w=[y])
                        orw = new('orw', [64, TT], BF16)
                        p.tt(orw[:, :], y[:, :], gh[:, :], ALU.mult, r=[y, gh], w=[orw])
                        p.dma('sp', SC['orT'][h * 64:(h + 1) * 64, tsl], orw[:, :], r=[orw], w=[('orT', h, tt)])
    p.ps_pool = list(range(8))


def mixers(nc, p, IN, SC, l, stop_after, cstream=None):
    with p.scope() as st:
        cst = make_consts(nc, p, st)
        c128 = p.sb(st, 'c128m', [128, N128])
        c64 = p.sb(st, 'c64m', [64, N64])
        p.dma('sp', c128[:, :], IN['c128'][l], w=[c128])
        p.dma('sp', c64[:, :], IN['c64'][l], w=[c64])
        with p.scope() as stg:
            Lg = p.record(lambda: phase_gla(nc, p, IN, SC, l, cst, c128, c64, stg))
            Ll = p.record(lambda: phase_lru(nc, p, IN, SC, l, cst, c128, c64, stg))
            p.ps_pool = list(range(8))
            p.play([Lg, Ll])
        if stop_after == ('lru', l):
            return True
        phase_rwkv3(nc, p, IN, SC, l, cst, c128, c64, cstream)
        if stop_after == ('rwkv', l):
            return True
    return False


def ln_fm(nc, p, st, onesD, z, out, c128, gname, bname, tag):
    zsq = p.sb(st, tag + 'zsq', [128, 8, TT])
    p.act(zsq[:, :, :], z[:, :, :], AF.Square, r=[z], w=[zsq])
    psM = p.psum()
    p.mm(psM[:, :], [(onesD[:, :], z[:, kc, :]) for kc in range(8)], r=[onesD, z], w=[psM])
    psQ = p.psum()
    p.mm(psQ[:, :], [(onesD[:, :], zsq[:, kc, :]) for kc in range(8)], r=[onesD, zsq], w=[psQ])
    mean = p.sb(st, tag + 'mean', [128, TT])
    rstd = p.sb(st, tag + 'rstd', [128, TT])
    tmp = p.sb(st, tag + 'tmp', [128, TT])
    p.copy(mean[:, :], psM[:, :], r=[psM], w=[mean], eng='act')
    p.act(tmp[:, :], psM[:, :], AF.Square, r=[psM], w=[tmp])
    p.tt(rstd[:, :], psQ[:, :], tmp[:, :], ALU.subtract, r=[psQ, tmp], w=[rstd])
    p.ts(rstd[:, :], rstd[:, :], 0.0, 1e-5, ALU.max, ALU.add, r=[rstd], w=[rstd])
    p.act(tmp[:, :], rstd[:, :], AF.Sqrt, r=[rstd], w=[tmp])
    p.op('dve', lambda: nc.vector.reciprocal(out=rstd[:, :], in_=tmp[:, :]), r=[tmp], w=[rstd])
    tks = [p.sb(st, tag + 'tk', [128, TT]) for _ in range(2)]
    for kc in range(8):
        eng = 'dve' if kc % 2 == 0 else 'pool'
        tk = tks[kc % 2]
        p.tt(tk[:, :], z[:, kc, :], mean[:, :], ALU.subtract, r=[z, mean], w=[tk], eng=eng)
        p.tt(tk[:, :], tk[:, :], rstd[:, :], ALU.mult, r=[tk, rstd], w=[tk], eng=eng)
        p.ts(out[:, kc, :], tk[:, :], c128[:, C128[gname] + kc:C128[gname] + kc + 1], c128[:, C128[bname] + kc:C128[bname] + kc + 1],
             ALU.mult, ALU.add, r=[tk, c128], w=[(out.key, kc)], eng='dve')
    p._mark('dve', p.cnt['dve'], [], [out.key])


def swiglu_bufs(p, st, F, nwd=1):
    nfc = F // 128
    return dict(hmid=p.sb(st, 'hmid', [128, nfc, TT], BF16),
                wgs=[p.sb(st, 'wgs', [128, 8, 512], BF16) for _ in range(2)],
                wus=[p.sb(st, 'wus', [128, 8, 512], BF16) for _ in range(2)],
                sgt=[p.sb(st, 'sgt', [128, TT]) for _ in range(2)],
                wd=[p.sb(st, 'wd', [128, nfc, 512], BF16) for _ in range(nwd)], cnt=[0, 0])


def swiglu_fm(nc, p, st, xb, wg_d, wu_d, wd_d, F, sink, bufs=None):
    nfc = F // 128
    if bufs is None:
        bufs = swiglu_bufs(p, st, F)
    hmid, wgs, wus, sgt = bufs['hmid'], bufs['wgs'], bufs['wus'], bufs['sgt']
    wg3 = wg_d.rearrange("(kc p) f -> p kc f", p=128)
    wu3 = wu_d.rearrange("(kc p) f -> p kc f", p=128)
    for f0 in range(0, F, 512):
        fw = min(512, F - f0)
        gi = bufs['cnt'][0]
        bufs['cnt'][0] += 1
        wg, wu = wgs[gi % 2], wus[gi % 2]
        p.dma('sp', wg[:, :, 0:fw], wg3[:, :, f0:f0 + fw], w=[wg])
        p.dma('sp', wu[:, :, 0:fw], wu3[:, :, f0:f0 + fw], w=[wu])
        for j in range(fw // 128):
            fc = f0 // 128 + j
            psG = p.psum()
            p.mm(psG[:, :], [(wg[:, kc, j * 128:(j + 1) * 128], xb[:, kc, :]) for kc in range(8)], r=[wg, xb], w=[psG])
            psU = p.psum()
            p.mm(psU[:, :], [(wu[:, kc, j * 128:(j + 1) * 128], xb[:, kc, :]) for kc in range(8)], r=[wu, xb], w=[psU])
            sg = sgt[fc % 2]
            p.act(sg[:, :], psG[:, :], AF.Silu, r=[psG], w=[sg])
            p.tt(hmid[:, fc, :], sg[:, :], psU[:, :], ALU.mult, r=[sg, psU], w=[hmid])
    wd3 = wd_d.rearrange("(fc p) m -> p fc m", p=128)
    for half in range(2):
        wi = bufs['cnt'][1]
        bufs['cnt'][1] += 1
        wd = bufs['wd'][wi % len(bufs['wd'])]
        for f0 in range(0, nfc, 7):
            f1 = min(nfc, f0 + 7)
            p.dma('sp', wd[:, f0:f1, :], wd3[:, f0:f1, half * 512:(half + 1) * 512], w=[(wd.key, f0)])
        for mm_ in range(4):
            mo = half * 4 + mm_
            ps = p.psum()
            p.mm(ps[:, :], [(wd[:, fc, mm_ * 128:(mm_ + 1) * 128], hmid[:, fc, :]) for fc in range(nfc)],
                 r=[(wd.key, f0) for f0 in range(0, nfc, 7)] + [hmid], w=[ps])
            sink(mo, ps)


def phase_tail(nc, p, IN, SC, l, xin, outd, outkey):
    with p.scope() as st0:
        c128 = p.sb(st0, 'c128t', [128, N128])
        p.dma('sp', c128[:, :], IN['c128'][l], w=[c128])
        onesD = p.sb(st0, 'onesD', [128, 128])
        p.op('pool', lambda: nc.gpsimd.memset(onesD[:, :], 1.0 / 1024.0), w=[onesD])
        if l == 1:
            wrt = p.sb(st0, 'wrt', [128, 8, NE])
            p.dma('sp', wrt[:, :, :], IN['moe_w_router'].rearrange("(kc p) e -> p kc e", p=128), w=[wrt])
            id128 = p.sb(st0, 'id128', [128, 128])
            ones_ = p.sb(st0, 'ones_', [128, 128])
            p.op('pool', lambda: nc.gpsimd.memset(ones_[:, :], 1.0), w=[ones_])
            p.op('pool', lambda: nc.gpsimd.affine_select(out=id128[:, :], in_=ones_[:, :], pattern=[[1, 128]], compare_op=ALU.is_equal,
                                                         fill=0.0, base=0, channel_multiplier=-1), r=[ones_], w=[id128])
            sel = p.sb(st0, 'sel', [8, NE, 128])
            ones3 = p.sb(st0, 'ones3', [8, NE, 128])
            p.op('pool', lambda: nc.gpsimd.memset(ones3[:, :, :], 1.0), w=[ones3])
            p.op('pool', lambda: nc.gpsimd.affine_select(out=sel[:, :, :], in_=ones3[:, :, :], pattern=[[-1, NE], [0, 128]],
                                                         compare_op=ALU.is_equal, fill=0.0, base=0, channel_multiplier=1), r=[ones3], w=[sel])
        xin3 = xin.rearrange("(kc p) t -> p kc t", p=128)
        out3 = outd.rearrange("(kc p) t -> p kc t", p=128)
        wbp = [SC['wb_pg'][l], SC['wb_pl'][l], SC['wb_pr'][l]]
        obd = [SC['ogT'], SC['olT'], SC['orT']]
        for tt in range(NT):
            tsl = slice(tt * TT, (tt + 1) * TT)
            with p.scope() as stx:
                x1 = p.sb(stx, 'x1', [128, 8, TT])
                x1b = p.sb(stx, 'x1b', [128, 8, TT], BF16)
                with p.scope() as st:
                    xf = p.sb(st, 'xf', [128, 8, TT])
                    xb = p.sb(st, 'xb', [128, 8, TT], BF16)
                    rk = [('x2T', c, tt) for c in range(8)] if l > 0 else []
                    p.dma('sp', xf[:, :, :], xin3[:, :, tsl], r=rk, w=[xf])
                    p.copy(xb[:, :, :], xf[:, :, :], r=[xf], w=[xb], eng='act')
                    acc = p.sb(st, 'acc', [128, 8, TT])
                    mb = p.sb(st, 'mb', [128, 8, TT], BF16)
                    wgh = [p.sb(st, 'wgh', [128, 8, 512], BF16) for _ in range(2)]
                    wph = [p.sb(st, 'wph', [128, 4, 512], BF16) for _ in range(2)]
                    obs = [p.sb(st, 'ob', [128, 4, TT], BF16) for _ in range(2)]
                    sig = [p.sb(st, 'sig', [128, TT]) for _ in range(2)]
                    for b in range(3):
                        for hf in range(2):
                            g0 = NMIX + b * 1024 + hf * 512
                            p.dma('sp', wgh[hf][:, :, :], SC['wb_in'][l].rearrange("(kc p) n -> p kc n", p=128)[:, :, g0:g0 + 512], w=[wgh[hf]])
                            p.dma('sp', wph[hf][:, :, :], wbp[b].rearrange("(fc p) m -> p fc m", p=128)[:, :, hf * 512:(hf + 1) * 512], w=[wph[hf]])
                        ob = obs[b % 2]
                        nrow = 128 if b < 2 else 64
                        okeys = [(['ogT', 'olT', 'orT'][b], i, tt) for i in range(512 // nrow)]
                        p.dma('sp', ob[:, :, :], obd[b].rearrange("(fc p) t -> p fc t", p=128)[:, :, tsl], r=okeys, w=[ob])
                        for mc in range(8):
                            ps1 = p.psum()
                            wgt, wpt, mq = wgh[mc // 4], wph[mc // 4], mc % 4
                            p.mm(ps1[:, :], [(wgt[:, kc, mq * 128:(mq + 1) * 128], xb[:, kc, :]) for kc in range(8)], r=[wgt, xb], w=[ps1])
                            ps2 = p.psum()
                            p.mm(ps2[:, :], [(wpt[:, fc, mq * 128:(mq + 1) * 128], ob[:, fc, :]) for fc in range(4)], r=[wpt, ob], w=[ps2])
                            sg = sig[mc % 2]
                            col = C128['b_gate'] + b * 8 + mc
                            p.act(sg[:, :], ps1[:, :], AF.Sigmoid, bias=c128[:, col:col + 1], r=[ps1, c128], w=[sg])
                            ak = (acc.key, mc)
                            if b == 0:
                                p.tt(acc[:, mc, :], sg[:, :], ps2[:, :], ALU.mult, r=[sg, ps2], w=[ak])
                            else:
                                p.tt(sg[:, :], sg[:, :], ps2[:, :], ALU.mult, r=[sg, ps2], w=[sg])
                                if b == 1:
                                    p.tt(acc[:, mc, :], acc[:, mc, :], sg[:, :], ALU.add, r=[ak, sg], w=[ak], eng='pool')
                                else:
                                    p.tt(mb[:, mc, :], acc[:, mc, :], sg[:, :], ALU.add, r=[ak, sg], w=[(mb.key, mc)], eng='pool')
                    p._mark('pool', p.cnt['pool'], [], [mb.key])
                    for hf in range(2):
                        p.dma('sp', wgh[hf][:, :, :], SC['wb_out'][l].rearrange("(kc p) n -> p kc n", p=128)[:, :, hf * 512:(hf + 1) * 512], w=[wgh[hf]])
                    z = acc
                    for mo in range(8):
                        wo, mq = wgh[mo // 4], mo % 4
                        ps = p.psum()
                        p.mm(ps[:, :], [(wo[:, kc, mq * 128:(mq + 1) * 128], mb[:, kc, :]) for kc in range(8)], r=[wo, mb], w=[ps])
                        p.stt(z[:, mo, :], xf[:, mo, :], ALPHA, ps[:, :], ALU.mult, ALU.add, r=[xf, ps, (acc.key, mo)], w=[(acc.key, mo)])
                    p._mark('dve', p.cnt['dve'], [], [z.key])
                    ln_fm(nc, p, st, onesD, z, x1, c128, 'ln_mix_g', 'ln_mix_b', 'l1')
                    p.copy(x1b[:, :, :], x1[:, :, :], r=[x1], w=[x1b], eng='act')
                with p.scope() as st:
                    z2 = p.sb(st, 'z2', [128, 8, TT])
                    if l == 0:
                        def sink(mo, ps):
                            p.stt(z2[:, mo, :], x1[:, mo, :], ALPHA, ps[:, :], ALU.mult, ALU.add, r=[x1, ps], w=[(z2.key, mo)])
                        swiglu_fm(nc, p, st, x1b, SC['wb_fg'], SC['wb_fu'], SC['wb_fd'], FD, sink, bufs=swiglu_bufs(p, st, FD, nwd=2))
                        p._mark('dve', p.cnt['dve'], [], [z2.key])
                    else:
                        wts = p.sb(st, 'wts', [128, 4, NE])
                        for tb in range(4):
                            psl = p.psum()
                            p.mm(psl[:, 0:NE], [(x1[:, kc, tb * 128:(tb + 1) * 128], wrt[:, kc, :]) for kc in range(8)], r=[x1, wrt], w=[psl])
                            lg = p.sb(st, 'lg', [128, NE])
                            p.copy(lg[:, :], psl[:, 0:NE], r=[psl], w=[lg], eng='act')
                            m1 = p.sb(st, 'm1', [128, 1])
                            m2 = p.sb(st, 'm2', [128, 1])
                            t8 = p.sb(st, 't8', [128, NE])
                            p.op('dve', lambda: nc.vector.tensor_reduce(out=m1[:, :], in_=lg[:, :], axis=mybir.AxisListType.X, op=ALU.max), r=[lg], w=[m1])
                            p.ts(t8[:, :], lg[:, :], m1[:, 0:1], -1e30, ALU.is_equal, ALU.mult, r=[lg, m1], w=[t8])
                            p.tt(t8[:, :], t8[:, :], lg[:, :], ALU.add, r=[t8, lg], w=[t8])
                            p.op('dve', lambda: nc.vector.tensor_reduce(out=m2[:, :], in_=t8[:, :], axis=mybir.AxisListType.X, op=ALU.max), r=[t8], w=[m2])
                            p.ts(t8[:, :], lg[:, :], m2[:, 0:1], None, ALU.is_ge, r=[lg, m2], w=[t8])
                            p.ts(m1[:, :], m1[:, :], -1.0, None, ALU.mult, r=[m1], w=[m1])
                            p.act(lg[:, :], lg[:, :], AF.Exp, bias=m1[:, 0:1], r=[lg, m1], w=[lg])
                            p.tt(lg[:, :], lg[:, :], t8[:, :], ALU.mult, r=[lg, t8], w=[lg])
                            p.op('dve', lambda: nc.vector.tensor_reduce(out=m2[:, :], in_=lg[:, :], axis=mybir.AxisListType.X, op=ALU.add), r=[lg], w=[m2])
                            p.op('dve', lambda: nc.vector.reciprocal(out=m2[:, :], in_=m2[:, :]), r=[m2], w=[m2])
                            p.ts(wts[:, tb, :], lg[:, :], m2[:, 0:1], None, ALU.mult, r=[lg, m2], w=[wts])
                        psw = p.psum()
                        p.transposes([(psw[0:NE, tb * 128:(tb + 1) * 128], wts[:, tb, :]) for tb in range(4)], id128[:, :], r=[wts, id128], w=[psw])
                        wT = p.sb(st, 'wT', [NE, TT])
                        p.copy(wT[:, :], psw[0:NE, :], r=[psw], w=[wT], eng='act')
                        accm = z2
                        mb_ = swiglu_bufs(p, st, FE, nwd=2)
                        wbes = [p.sb(st, 'wbe', [128, TT]) for _ in range(2)]
                        tmpm = [p.sb(st, 'tmpm', [128, TT]) for _ in range(2)]
                        for e in range(NE):
                            psb = p.psum()
                            p.mm(psb[:, :], [(sel[:, e, :], wT[:, :])], r=[sel, wT], w=[psb])
                            wbe = wbes[e % 2]
                            p.copy(wbe[:, :], psb[:, :], r=[psb], w=[wbe], eng='act')

                            def sink(mo, ps, e=e, wbe=wbe, tmpm=tmpm):
                                ak = (accm.key, mo)
                                if e == 0:
                                    p.tt(accm[:, mo, :], ps[:, :], wbe[:, :], ALU.mult, r=[ps, wbe], w=[ak])
                                else:
                                    tm = tmpm[mo % 2]
                                    p.tt(tm[:, :], ps[:, :], wbe[:, :], ALU.mult, r=[ps, wbe], w=[tm])
                                    p.tt(accm[:, mo, :], accm[:, mo, :], tm[:, :], ALU.add, r=[ak, tm], w=[ak], eng='pool')
                            swiglu_fm(nc, p, st, x1b, SC['wb_mg'][e], SC['wb_mu'][e], SC['wb_md'][e], FE, sink, bufs=mb_)
                        p._mark('pool', p.cnt['pool'], [], [accm.key])
                        for mo in range(8):
                            p.stt(z2[:, mo, :], x1[:, mo, :], ALPHA, accm[:, mo, :], ALU.mult, ALU.add, r=[x1, accm], w=[(z2.key, mo)])
                        p._mark('dve', p.cnt['dve'], [], [z2.key])
                    x2 = x1
                    ln_fm(nc, p, st, onesD, z2, x2, c128, 'ln_ffn_g', 'ln_ffn_b', 'l2')
                    p.dma('sp', out3[:, :, tsl], x2[:, :, :], r=[x2], w=[(outkey, c, tt) for c in range(8)])


def phase_rwkv2(nc, p, IN, SC, l, cst, c128, c64, cstream=None):
    YB = 7
    POOL1 = [0, 1, 2, 3]
    POOL2 = [4, 5, 6]
    with p.scope() as st0:
        w2 = p.sb(st0, 'w2', [64, 512])
        a2 = p.sb(st0, 'a2', [64, 512])
        g2a = p.sb(st0, 'g2a', [128, 512])
        g2b = p.sb(st0, 'g2b', [32, 512])
        p.dma('sp', w2[:, :], IN['rwkv_w2'][l], w=[w2])
        p.dma('sp', a2[:, :], IN['rwkv_a2'][l], w=[a2])
        p.dma('sp', g2a[:, :], IN['rwkv_g2'][l, 0:128, :], w=[g2a])
        p.dma('sp', g2b[:, :], IN['rwkv_g2'][l, 128:160, :], w=[g2b])
        omu = p.sb(st0, 'omu', [64, N64])
        p.ts(omu[:, :], c64[:, :], -1.0, 1.0, ALU.mult, ALU.add, r=[c64], w=[omu])
        omug = p.sb(st0, 'omug', [128, 2])
        p.ts(omug[:, :], c128[:, C128['mu_g']:C128['mu_g'] + 2], -1.0, 1.0, ALU.mult, ALU.add, r=[c128], w=[omug])
        Tst = [p.sb(st0, 'Tst%d' % h, [64, 64]) for h in range(8)]
        for h in range(8):
            p.op('pool', lambda: nc.gpsimd.memset(Tst[h][:, :], 0.0), w=[Tst[h]])
        psY = p.ps[YB]

        def T(tag, shape=None, dt=F32):
            return p.sb(st0, tag, shape or [64, TT], dt)
        raw64 = T('raw64', [64, TT + 1])
        raw128 = T('raw128', [128, TT + 1])
        wl, al = T('wl'), T('al')
        gl1 = T('gl1', [128, TT])
        gl2 = T('gl2', [32, TT])
        r, k, v = T('r'), T('k'), T('v')
        sgm, cum, ag, kk, tmp, k2, bv, E = T('sgm'), T('cum'), T('ag'), T('kk'), T('tmp'), T('k2'), T('bv'), T('E')
        kt, bt, kh, bh = T('kt', dt=BF16), T('bt', dt=BF16), T('kh'), T('bh')
        at16, rt16 = T('at16', dt=BF16), T('rt16', dt=BF16)
        D = T('D', [64, NCH, CH])
        Aab = T('Aab', dt=BF16)
        Pp = [T('Pa', dt=BF16), T('Pb', dt=BF16)]
        Qp = [T('Qa', dt=BF16), T('Qb', dt=BF16)]
        Nx = T('Nx', dt=BF16)
        Ny = T('Ny', dt=BF16)
        HB = []
        for i in range(2):
            HB.append(dict(at=T('at'), AakT=T('AakT'), vT=T('vT'), N=T('N'), rt=T('rt'), ArbT=T('ArbT'), ArkT=T('ArkT'),
                           bhT=T('bhT'), khT=T('khT'), WC=T('WC', [64, NCH, 1]), bonus=T('bonus'), gh=T('gh')))
        X = T('X', [64, 64])
        U = T('U', [64, 64])
        no, nosq, nmean, nmsq = T('no'), T('nosq'), T('nmean'), T('nmsq')
        orw = T('orw', [64, TT], BF16)

        def col(name, h=0):
            return c64[:, C64[name] + h:C64[name] + h + 1]

        def ocol(name, h=0):
            return omu[:, C64[name] + h:C64[name] + h + 1]

        def load_shift(out, raw, tt, row0, nrows, mu_ap, omu_ap, rd):
            t0 = tt * TT
            if tt == 0:
                p.op('pool', lambda: nc.gpsimd.memset(raw[0:nrows, 0:1], 0.0), w=[raw])
                p.dma('sp', raw[0:nrows, 1:], SC['hT'][row0:row0 + nrows, 0:TT], r=hk('hT', row0, nrows, tt), w=[raw])
            else:
                p.dma('sp', raw[0:nrows, :], SC['hT'][row0:row0 + nrows, t0 - 1:t0 + TT], r=hk('hT', row0, nrows, tt, True), w=[raw])
            p.ts(out[:, :], raw[0:nrows, 1:TT + 1], omu_ap, None, ALU.mult, r=[raw] + rd, w=[out])
            p.stt(out[:, :], raw[0:nrows, 0:TT], mu_ap, out[:, :], ALU.mult, ALU.add, r=[raw, out] + rd, w=[out])

        def tile_prep(tt):
            load_shift(wl, raw64, tt, RWW, 64, col('mu_w'), ocol('mu_w'), [c64, omu])
            load_shift(al, raw64, tt, RWA, 64, col('mu_a'), ocol('mu_a'), [c64, omu])
            mg = C128['mu_g']
            load_shift(gl1, raw128, tt, RWG, 128, c128[:, mg:mg + 1], omug[:, 0:1], [c128, omug])
            load_shift(gl2, raw128, tt, RWG + 128, 32, c128[0:32, mg + 1:mg + 2], omug[0:32, 1:2], [c128, omug])
            p.act(wl[:, :], wl[:, :], AF.Tanh, r=[wl], w=[wl])
            p.act(gl1[:, :], gl1[:, :], AF.Sigmoid, r=[gl1], w=[gl1])
            p.act(gl2[:, :], gl2[:, :], AF.Sigmoid, r=[gl2], w=[gl2])

        def cslices(t):
            return [t[:, c * 64:(c + 1) * 64] for c in range(NCH)]

        def stage1(tt, h, B):
            p.ps_pool = POOL1
            if h == 0:
                tile_prep(tt)
            hs = slice(h * 64, (h + 1) * 64)
            load_shift(r, raw64, tt, RWR + h * 64, 64, col('mu_r', h), ocol('mu_r', h), [c64, omu])
            load_shift(k, raw64, tt, RWK + h * 64, 64, col('mu_k', h), ocol('mu_k', h), [c64, omu])
            load_shift(v, raw64, tt, RWV + h * 64, 64, col('mu_v', h), ocol('mu_v', h), [c64, omu])
            ps = p.psum()
            p.mm(ps[0:64, :], [(w2[:, hs], wl[:, :])], r=[w2, wl], w=[ps])
            p.act(sgm[:, :], ps[0:64, :], AF.Sigmoid, bias=col('w0', h), r=[ps, c64], w=[sgm])
            p.op('dve', lambda: nc.vector.tensor_tensor_scan(out=cum[:, :], data0=cst.m01[:, :], data1=sgm[:, :], initial=0.0,
                                                             op0=ALU.mult, op1=ALU.add), r=[cst.m01, sgm], w=[cum])
            ps = p.psum()
            p.mm(ps[0:64, :], [(a2[:, hs], al[:, :])], r=[a2, al], w=[ps])
            p.act(ag[:, :], ps[0:64, :], AF.Sigmoid, bias=col('a0', h), r=[ps, c64], w=[ag])
            ps = p.psum()
            p.mm(ps[0:64, :], [(g2a[:, hs], gl1[:, :]), (g2b[:, hs], gl2[:, :])], r=[g2a, g2b, gl1, gl2], w=[ps])
            p.copy(B['gh'][:, :], ps[0:64, :], r=[ps], w=[B['gh']], eng='act')
            p.ts(kk[:, :], k[:, :], col('k_k', h), None, ALU.mult, r=[k, c64], w=[kk])
            p.act(tmp[:, :], kk[:, :], AF.Square, r=[kk], w=[tmp])
            ps = p.psum()
            p.mm(ps[0:64, :], [(cst.ones64[:, :], tmp[:, :])], r=[cst.ones64, tmp], w=[ps])
            p.act(tmp[:, :], ps[0:64, :], AF.Sqrt, r=[ps], w=[tmp])
            p.ts(tmp[:, :], tmp[:, :], 1e-12, None, ALU.max, r=[tmp], w=[tmp])
            p.op('dve', lambda: nc.vector.reciprocal(out=tmp[:, :], in_=tmp[:, :]), r=[tmp], w=[tmp])
            p.tt(kk[:, :], kk[:, :], tmp[:, :], ALU.mult, r=[kk, tmp], w=[kk])
            p.ts(tmp[:, :], ag[:, :], col('k_a', h), ocol('k_a', h), ALU.mult, ALU.add, r=[ag, c64, omu], w=[tmp])
            p.tt(k2[:, :], k[:, :], tmp[:, :], ALU.mult, r=[k, tmp], w=[k2])
            p.tt(bv[:, :], kk[:, :], ag[:, :], ALU.mult, r=[kk, ag], w=[bv])
            rt, at = B['rt'], B['at']
            p.act(E[:, :], cum[:, :], AF.Exp, scale=-C0, r=[cum], w=[E])
            p.tt(rt[:, :], r[:, :], E[:, :], ALU.mult, r=[r, E], w=[rt])
            p.copy(rt16[:, :], rt[:, :], r=[rt], w=[rt16], eng='act')
            p.act(E[:, :], cum[:, :], AF.Exp, scale=C0, r=[cum], w=[E])
            p.tt(kt[:, :], k2[:, :], E[:, :], ALU.mult, r=[k2, E], w=[kt])
            p.tt(bt[:, :], bv[:, :], E[:, :], ALU.mult, r=[bv, E], w=[bt])
            p.tt(tmp[:, :], cum[:, :], sgm[:, :], ALU.subtract, r=[cum, sgm], w=[tmp])
            p.act(E[:, :], tmp[:, :], AF.Exp, scale=-C0, r=[tmp], w=[E])
            p.stt(at[:, :], kk[:, :], -1.0, E[:, :], ALU.mult, ALU.mult, r=[kk, E], w=[at])
            p.copy(at16[:, :], at[:, :], r=[at], w=[at16], eng='act')
            c3 = cview(cum)
            p.tt(D[:, :, :], c3[:, :, CH - 1:CH].to_broadcast([64, NCH, CH]), c3, ALU.subtract, r=[cum], w=[D])
            p.act(D[:, :, :], D[:, :, :], AF.Exp, scale=-C0, r=[D], w=[D])
            Df = D[:, :, :].rearrange("p c t -> p (c t)")
            p.tt(kh[:, :], k2[:, :], Df, ALU.mult, r=[k2, D], w=[kh])
            p.tt(bh[:, :], bv[:, :], Df, ALU.mult, r=[bv, D], w=[bh])
            p.act(B['WC'][:, :, :], c3[:, :, CH - 1:CH], AF.Exp, scale=-C0, r=[cum], w=[B['WC']])
            p.stt(tmp[:, :], r[:, :], col('r_k', h), k2[:, :], ALU.mult, ALU.mult, r=[r, k2, c64], w=[tmp])
            ps = p.psum()
            p.mm(ps[0:64, :], [(cst.ones64[:, :], tmp[:, :])], r=[cst.ones64, tmp], w=[ps])
            p.tt(B['bonus'][:, :], ps[0:64, :], v[:, :], ALU.mult, r=[ps, v], w=[B['bonus']])
            for src, dn in ((v, 'vT'), (kh, 'khT'), (bh, 'bhT')):
                psT = p.psum()
                p.transposes(list(zip([psT[0:64, c * 64:(c + 1) * 64] for c in range(NCH)], cslices(src))),
                             cst.ident[:, :], r=[src, cst.ident], w=[psT])
                p.copy(B[dn][:, :], psT[0:64, :], r=[psT], w=[B[dn]], eng='act')

            def amat(lt, rh, mask, d):
                psA = p.psum()
                p.mms([(psA[0:64, c * 64:(c + 1) * 64], [(lt[:, c * 64:(c + 1) * 64], rh[:, c * 64:(c + 1) * 64])]) for c in range(NCH)],
                      r=[lt, rh], w=[psA])
                p.tt(d[:, :], psA[0:64, :], mask[:, :, :].rearrange("p c t -> p (c t)"), ALU.mult, r=[psA, mask], w=[d])
            amat(bt, at16, cst.mgt, Qp[0])
            amat(bt, rt16, cst.mge, B['ArbT'])
            amat(kt, at16, cst.mgt, B['AakT'])
            amat(kt, rt16, cst.mge, B['ArkT'])
            amat(at16, bt, cst.mlt, Aab)
            P_, Q_ = Aab, Qp[0]
            Ns = [Nx, Ny]
            N_ = Ns[0]
            p.tt(N_[:, :], Q_[:, :], cst.id8[:, :, :].rearrange("p c t -> p (c t)"), ALU.add, r=[Q_, cst.id8], w=[N_])
            for lev in range(5):
                psP = p.psum()
                p.mms([(psP[0:64, c * 64:(c + 1) * 64], [(Q_[:, c * 64:(c + 1) * 64], P_[:, c * 64:(c + 1) * 64])]) for c in range(NCH)],
                      r=[P_, Q_], w=[psP])
                if lev < 4:
                    psQ = p.psum()
                    p.mms([(psQ[0:64, c * 64:(c + 1) * 64], [(P_[:, c * 64:(c + 1) * 64], Q_[:, c * 64:(c + 1) * 64])]) for c in range(NCH)],
                          r=[P_, Q_], w=[psQ])
                P2 = Pp[lev % 2]
                p.copy(P2[:, :], psP[0:64, :], r=[psP], w=[P2], eng='act')
                if lev < 4:
                    Q2 = Qp[(lev + 1) % 2]
                    p.copy(Q2[:, :], psQ[0:64, :], r=[psQ], w=[Q2], eng='dve')
                psN = p.psum()
                p.mms([(psN[0:64, c * 64:(c + 1) * 64], [(P2[:, c * 64:(c + 1) * 64], N_[:, c * 64:(c + 1) * 64])]) for c in range(NCH)],
                      r=[P2, N_], w=[psN])
                N2 = Ns[(lev + 1) % 2] if lev < 4 else B['N']
                p.tt(N2[:, :], N_[:, :], psN[0:64, :], ALU.add, r=[N_, psN], w=[N2])
                P_, N_ = P2, N2
                if lev < 4:
                    Q_ = Q2
            assert N_ is B['N']

        def stage2(tt, h, B):
            p.ps_pool = POOL2
            tsl = slice(tt * TT, (tt + 1) * TT)
            at, AakT, vT, N_, rt, ArbT, ArkT, bhT, khT, WC = (B[x] for x in ('at', 'AakT', 'vT', 'N', 'rt', 'ArbT', 'ArkT', 'bhT', 'khT', 'WC'))
            for c in range(NCH):
                cs = slice(c * 64, (c + 1) * 64)
                psX = p.psum()
                p.mm(psX[0:64, 0:64], [(at[:, cs], Tst[h][:, :]), (AakT[:, cs], vT[:, cs])], r=[at, Tst[h], AakT, vT], w=[psX])
                p.copy(X[:, :], psX[0:64, 0:64], r=[psX], w=[X], eng='act')
                psU = p.psum()
                p.mm(psU[0:64, 0:64], [(N_[:, cs], X[:, :])], r=[N_, X], w=[psU])
                p.copy(U[:, :], psU[0:64, 0:64], r=[psU], w=[U], eng='dve')
                p.mm(psY[0:64, cs], [(Tst[h][:, :], rt[:, cs]), (U[:, :], ArbT[:, cs]), (vT[:, cs], ArkT[:, cs])],
                     r=[Tst[h], rt, U, ArbT, vT, ArkT], w=[psY])
                psT2 = p.psum()
                p.mm(psT2[0:64, 0:64], [(bhT[:, cs], U[:, :]), (khT[:, cs], vT[:, cs])], r=[bhT, U, khT, vT], w=[psT2])
                p.stt(Tst[h][:, :], Tst[h][:, :], WC[:, c, :], psT2[0:64, 0:64], ALU.mult, ALU.add, r=[Tst[h], WC, psT2], w=[Tst[h]])
            ones = cst.onesm64
            p.copy(no[:, :], psY[0:64, :], r=[psY], w=[no], eng='act')
            p.act(nosq[:, :], psY[0:64, :], AF.Square, r=[psY], w=[nosq])
            psM = p.psum()
            p.mm(psM[0:64, :], [(ones[:, :], no[:, :])], r=[ones, no], w=[psM])
            psQ2 = p.psum()
            p.mm(psQ2[0:64, :], [(ones[:, :], nosq[:, :])], r=[ones, nosq], w=[psQ2])
            p.copy(nmean[:, :], psM[0:64, :], r=[psM], w=[nmean], eng='act')
            p.act(nmsq[:, :], psM[0:64, :], AF.Square, r=[psM], w=[nmsq])
            p.tt(nosq[:, :], psQ2[0:64, :], nmsq[:, :], ALU.subtract, r=[psQ2, nmsq], w=[nosq])
            p.ts(nosq[:, :], nosq[:, :], 0.0, 64e-5, ALU.max, ALU.add, r=[nosq], w=[nosq])
            p.act(nmsq[:, :], nosq[:, :], AF.Sqrt, r=[nosq], w=[nmsq])
            p.op('dve', lambda: nc.vector.reciprocal(out=nosq[:, :], in_=nmsq[:, :]), r=[nmsq], w=[nosq])
            p.tt(no[:, :], no[:, :], nmean[:, :], ALU.subtract, r=[no, nmean], w=[no])
            p.tt(no[:, :], no[:, :], nosq[:, :], ALU.mult, r=[no, nosq], w=[no])
            p.ts(no[:, :], no[:, :], col('lnx_g', h), col('lnx_b', h), ALU.mult, ALU.add, r=[no, c64], w=[no])
            p.tt(no[:, :], no[:, :], B['bonus'][:, :], ALU.add, r=[no, B['bonus']], w=[no])
            p.tt(orw[:, :], no[:, :], B['gh'][:, :], ALU.mult, r=[no, B['gh']], w=[orw])
            p.dma('sp', SC['orT'][h * 64:(h + 1) * 64, tsl], orw[:, :], r=[orw], w=[('orT', h, tt)])

        seq = [(tt, h) for tt in range(NT) for h in range(8)]
        L1 = p.record(lambda: stage1(seq[0][0], seq[0][1], HB[0]))
        p.play([L1])
        for n, (tt, h) in enumerate(seq):
            L2 = p.record(lambda: stage2(tt, h, HB[n % 2]))
            lists = [L2]
            if n + 1 < len(seq):
                tn, hn = seq[n + 1]
                lists.append(p.record(lambda: stage1(tn, hn, HB[(n + 1) % 2])))
            if cstream is not None:
                lists.append(cstream.take(len(seq) - n))
            p.play(lists)
    p.ps_pool = list(range(8))


def phase_rwkv3(nc, p, IN, SC, l, cst, c128, c64, cstream=None):
    POOLA = [0, 1]
    POOLB = [2, 3]
    POOLC = [4, 5]
    with p.scope() as st0:
        w2 = p.sb(st0, 'w2', [64, 512])
        a2 = p.sb(st0, 'a2', [64, 512])
        g2a = p.sb(st0, 'g2a', [128, 512])
        g2b = p.sb(st0, 'g2b', [32, 512])
        p.dma('sp', w2[:, :], IN['rwkv_w2'][l], w=[w2])
        p.dma('sp', a2[:, :], IN['rwkv_a2'][l], w=[a2])
        p.dma('sp', g2a[:, :], IN['rwkv_g2'][l, 0:128, :], w=[g2a])
        p.dma('sp', g2b[:, :], IN['rwkv_g2'][l, 128:160, :], w=[g2b])
        omu = p.sb(st0, 'omu', [64, N64])
        p.ts(omu[:, :], c64[:, :], -1.0, 1.0, ALU.mult, ALU.add, r=[c64], w=[omu])
        omug = p.sb(st0, 'omug', [128, 2])
        p.ts(omug[:, :], c128[:, C128['mu_g']:C128['mu_g'] + 2], -1.0, 1.0, ALU.mult, ALU.add, r=[c128], w=[omug])
        Tst = [p.sb(st0, 'Tst%d' % h, [64, 64]) for h in range(8)]
        for h in range(8):
            p.op('pool', lambda: nc.gpsimd.memset(Tst[h][:, :], 0.0), w=[Tst[h]])
        psYs = [p.ps[6], p.ps[7]]

        def T(tag, shape=None, dt=F32):
            return p.sb(st0, tag, shape or [64, TT], dt)
        raw64 = T('raw64', [64, TT + 1])
        raw3 = [T('raw3', [64, TT + 1]) for _ in range(3)]
        raw128 = T('raw128', [128, TT + 1])
        wl, al = T('wl'), T('al')
        gl1 = T('gl1', [128, TT])
        gl2 = T('gl2', [32, TT])
        r, k, v = T('r'), T('k'), T('v')
        sgm, cum, ag, kk, tmp, k2, bv, E = T('sgm'), T('cum'), T('ag'), T('kk'), T('tmp'), T('k2'), T('bv'), T('E')
        kt, bt, kh, bh = T('kt', dt=BF16), T('bt', dt=BF16), T('kh'), T('bh')
        at16, rt16 = T('at16', dt=BF16), T('rt16', dt=BF16)
        D = T('D', [64, NCH, CH])
        AQ = [dict(Aab=T('Aab', dt=BF16), Q0=T('Q0', dt=BF16)) for _ in range(2)]
        Pp = [T('Pa', dt=BF16), T('Pb', dt=BF16)]
        Qp = [T('Qa', dt=BF16), T('Qb', dt=BF16)]
        Nx = T('Nx', dt=BF16)
        Ny = T('Ny', dt=BF16)
        HB = []
        for i in range(3):
            HB.append(dict(at=T('at'), AakT=T('AakT'), vT=T('vT'), N=T('N'), rt=T('rt'), ArbT=T('ArbT'), ArkT=T('ArkT'),
                           bhT=T('bhT'), khT=T('khT'), WC=T('WC', [64, NCH, 1]), bonus=T('bonus'), gh=T('gh')))
        X = T('X', [64, 64])
        U = T('U', [64, 64])
        no, nosq, nmean, nmsq = T('no'), T('nosq'), T('nmean'), T('nmsq')
        orw = T('orw', [64, TT], BF16)

        def col(name, h=0):
            return c64[:, C64[name] + h:C64[name] + h + 1]

        def ocol(name, h=0):
            return omu[:, C64[name] + h:C64[name] + h + 1]

        def load_shift(out, raw, tt, row0, nrows, mu_ap, omu_ap, rd):
            t0 = tt * TT
            if tt == 0:
                p.op('pool', lambda: nc.gpsimd.memset(raw[0:nrows, 0:1], 0.0), w=[raw])
                p.dma('sp', raw[0:nrows, 1:], SC['hT'][row0:row0 + nrows, 0:TT], r=hk('hT', row0, nrows, tt), w=[raw])
            else:
                p.dma('sp', raw[0:nrows, :], SC['hT'][row0:row0 + nrows, t0 - 1:t0 + TT], r=hk('hT', row0, nrows, tt, True), w=[raw])
            p.act(out[:, :], raw[0:nrows, 1:TT + 1], AF.Identity, scale=omu_ap, r=[raw] + rd, w=[out])
            p.stt(out[:, :], raw[0:nrows, 0:TT], mu_ap, out[:, :], ALU.mult, ALU.add, r=[raw, out] + rd, w=[out])

        def tile_prep(tt):
            load_shift(wl, raw64, tt, RWW, 64, col('mu_w'), ocol('mu_w'), [c64, omu])
            load_shift(al, raw64, tt, RWA, 64, col('mu_a'), ocol('mu_a'), [c64, omu])
            mg = C128['mu_g']
            load_shift(gl1, raw128, tt, RWG, 128, c128[:, mg:mg + 1], omug[:, 0:1], [c128, omug])
            load_shift(gl2, raw128, tt, RWG + 128, 32, c128[0:32, mg + 1:mg + 2], omug[0:32, 1:2], [c128, omug])
            p.act(wl[:, :], wl[:, :], AF.Tanh, r=[wl], w=[wl])
            p.act(gl1[:, :], gl1[:, :], AF.Sigmoid, r=[gl1], w=[gl1])
            p.act(gl2[:, :], gl2[:, :], AF.Sigmoid, r=[gl2], w=[gl2])

        def cslices(t):
            return [t[:, c * 64:(c + 1) * 64] for c in range(NCH)]

        def stage1a(tt, h, B, aq):
            p.ps_pool = POOLA
            if h == 0:
                tile_prep(tt)
            hs = slice(h * 64, (h + 1) * 64)
            load_shift(r, raw3[0], tt, RWR + h * 64, 64, col('mu_r', h), ocol('mu_r', h), [c64, omu])
            load_shift(k, raw3[1], tt, RWK + h * 64, 64, col('mu_k', h), ocol('mu_k', h), [c64, omu])
            load_shift(v, raw3[2], tt, RWV + h * 64, 64, col('mu_v', h), ocol('mu_v', h), [c64, omu])
            ps = p.psum()
            p.mm(ps[0:64, :], [(w2[:, hs], wl[:, :])], r=[w2, wl], w=[ps])
            p.act(sgm[:, :], ps[0:64, :], AF.Sigmoid, bias=col('w0', h), r=[ps, c64], w=[sgm])
            p.op('dve', lambda: nc.vector.tensor_tensor_scan(out=cum[:, :], data0=cst.m01[:, :], data1=sgm[:, :], initial=0.0,
                                                             op0=ALU.mult, op1=ALU.add), r=[cst.m01, sgm], w=[cum])
            ps = p.psum()
            p.mm(ps[0:64, :], [(a2[:, hs], al[:, :])], r=[a2, al], w=[ps])
            p.act(ag[:, :], ps[0:64, :], AF.Sigmoid, bias=col('a0', h), r=[ps, c64], w=[ag])
            ps = p.psum()
            p.mm(ps[0:64, :], [(g2a[:, hs], gl1[:, :]), (g2b[:, hs], gl2[:, :])], r=[g2a, g2b, gl1, gl2], w=[ps])
            p.copy(B['gh'][:, :], ps[0:64, :], r=[ps], w=[B['gh']], eng='act')
            p.act(kk[:, :], k[:, :], AF.Identity, scale=col('k_k', h), r=[k, c64], w=[kk])
            p.act(tmp[:, :], kk[:, :], AF.Square, r=[kk], w=[tmp])
            ps = p.psum()
            p.mm(ps[0:64, :], [(cst.ones64[:, :], tmp[:, :])], r=[cst.ones64, tmp], w=[ps])
            p.act(tmp[:, :], ps[0:64, :], AF.Sqrt, r=[ps], w=[tmp])
            p.ts(tmp[:, :], tmp[:, :], 1e-12, None, ALU.max, r=[tmp], w=[tmp])
            p.op('dve', lambda: nc.vector.reciprocal(out=tmp[:, :], in_=tmp[:, :]), r=[tmp], w=[tmp])
            p.tt(kk[:, :], kk[:, :], tmp[:, :], ALU.mult, r=[kk, tmp], w=[kk])
            p.act(tmp[:, :], ag[:, :], AF.Identity, bias=ocol('k_a', h), scale=col('k_a', h), r=[ag, c64, omu], w=[tmp])
            p.tt(k2[:, :], k[:, :], tmp[:, :], ALU.mult, r=[k, tmp], w=[k2])
            p.tt(bv[:, :], kk[:, :], ag[:, :], ALU.mult, r=[kk, ag], w=[bv])
            rt, at = B['rt'], B['at']
            p.act(E[:, :], cum[:, :], AF.Exp, scale=-C0, r=[cum], w=[E])
            p.tt(rt[:, :], r[:, :], E[:, :], ALU.mult, r=[r, E], w=[rt])
            p.copy(rt16[:, :], rt[:, :], r=[rt], w=[rt16], eng='act')
            p.act(E[:, :], cum[:, :], AF.Exp, scale=C0, r=[cum], w=[E])
            p.tt(kt[:, :], k2[:, :], E[:, :], ALU.mult, r=[k2, E], w=[kt])
            p.tt(bt[:, :], bv[:, :], E[:, :], ALU.mult, r=[bv, E], w=[bt])
            p.tt(tmp[:, :], cum[:, :], sgm[:, :], ALU.subtract, r=[cum, sgm], w=[tmp])
            p.act(E[:, :], tmp[:, :], AF.Exp, scale=-C0, r=[tmp], w=[E])
            p.stt(at[:, :], kk[:, :], -1.0, E[:, :], ALU.mult, ALU.mult, r=[kk, E], w=[at])
            p.copy(at16[:, :], at[:, :], r=[at], w=[at16], eng='act')
            c3 = cview(cum)
            p.tt(D[:, :, :], c3[:, :, CH - 1:CH].to_broadcast([64, NCH, CH]), c3, ALU.subtract, r=[cum], w=[D])
            p.act(D[:, :, :], D[:, :, :], AF.Exp, scale=-C0, r=[D], w=[D])
            Df = D[:, :, :].rearrange("p c t -> p (c t)")
            p.tt(kh[:, :], k2[:, :], Df, ALU.mult, r=[k2, D], w=[kh])
            p.tt(bh[:, :], bv[:, :], Df, ALU.mult, r=[bv, D], w=[bh])
            p.act(B['WC'][:, :, :], c3[:, :, CH - 1:CH], AF.Exp, scale=-C0, r=[cum], w=[B['WC']])
            p.stt(tmp[:, :], r[:, :], col('r_k', h), k2[:, :], ALU.mult, ALU.mult, r=[r, k2, c64], w=[tmp])
            ps = p.psum()
            p.mm(ps[0:64, :], [(cst.ones64[:, :], tmp[:, :])], r=[cst.ones64, tmp], w=[ps])
            p.tt(B['bonus'][:, :], ps[0:64, :], v[:, :], ALU.mult, r=[ps, v], w=[B['bonus']])
            for src, dn in ((v, 'vT'), (kh, 'khT'), (bh, 'bhT')):
                psT = p.psum()
                p.transposes(list(zip([psT[0:64, c * 64:(c + 1) * 64] for c in range(NCH)], cslices(src))),
                             cst.ident[:, :], r=[src, cst.ident], w=[psT])
                p.copy(B[dn][:, :], psT[0:64, :], r=[psT], w=[B[dn]], eng='act')

            def amat(lt, rh, mask, d):
                psA = p.psum()
                p.mms([(psA[0:64, c * 64:(c + 1) * 64], [(lt[:, c * 64:(c + 1) * 64], rh[:, c * 64:(c + 1) * 64])]) for c in range(NCH)],
                      r=[lt, rh], w=[psA])
                p.tt(d[:, :], psA[0:64, :], mask[:, :, :].rearrange("p c t -> p (c t)"), ALU.mult, r=[psA, mask], w=[d])
            amat(bt, at16, cst.mgt, aq['Q0'])
            amat(bt, rt16, cst.mge, B['ArbT'])
            amat(kt, at16, cst.mgt, B['AakT'])
            amat(kt, rt16, cst.mge, B['ArkT'])
            amat(at16, bt, cst.mlt, aq['Aab'])

        def stage1b(B, aq):
            p.ps_pool = POOLB
            P_, Q_ = aq['Aab'], aq['Q0']
            Ns = [Nx, Ny]
            N_ = Ns[0]
            p.tt(N_[:, :], Q_[:, :], cst.id8[:, :, :].rearrange("p c t -> p (c t)"), ALU.add, r=[Q_, cst.id8], w=[N_])
            for lev in range(5):
                psP = p.psum()
                p.mms([(psP[0:64, c * 64:(c + 1) * 64], [(Q_[:, c * 64:(c + 1) * 64], P_[:, c * 64:(c + 1) * 64])]) for c in range(NCH)],
                      r=[P_, Q_], w=[psP])
                if lev < 4:
                    psQ = p.psum()
                    p.mms([(psQ[0:64, c * 64:(c + 1) * 64], [(P_[:, c * 64:(c + 1) * 64], Q_[:, c * 64:(c + 1) * 64])]) for c in range(NCH)],
                          r=[P_, Q_], w=[psQ])
                P2 = Pp[lev % 2]
                p.copy(P2[:, :], psP[0:64, :], r=[psP], w=[P2], eng='act')
                if lev < 4:
                    Q2 = Qp[lev % 2]
                    p.copy(Q2[:, :], psQ[0:64, :], r=[psQ], w=[Q2], eng='dve')
                psN = p.psum()
                p.mms([(psN[0:64, c * 64:(c + 1) * 64], [(P2[:, c * 64:(c + 1) * 64], N_[:, c * 64:(c + 1) * 64])]) for c in range(NCH)],
                      r=[P2, N_], w=[psN])
                N2 = Ns[(lev + 1) % 2] if lev < 4 else B['N']
                p.tt(N2[:, :], N_[:, :], psN[0:64, :], ALU.add, r=[N_, psN], w=[N2])
                P_, N_ = P2, N2
                if lev < 4:
                    Q_ = Q2
            assert N_ is B['N']

        def chain(tt, h, B, psY):
            p.ps_pool = POOLC
            at, AakT, vT, N_, rt, ArbT, ArkT, bhT, khT, WC = (B[x] for x in ('at', 'AakT', 'vT', 'N', 'rt', 'ArbT', 'ArkT', 'bhT', 'khT', 'WC'))
            for c in range(NCH):
                cs = slice(c * 64, (c + 1) * 64)
                psX = p.psum()
                p.mm(psX[0:64, 0:64], [(at[:, cs], Tst[h][:, :]), (AakT[:, cs], vT[:, cs])], r=[at, Tst[h], AakT, vT], w=[psX])
                p.copy(X[:, :], psX[0:64, 0:64], r=[psX], w=[X], eng='act')
                psU = p.psum()
                p.mm(psU[0:64, 0:64], [(N_[:, cs], X[:, :])], r=[N_, X], w=[psU])
                p.copy(U[:, :], psU[0:64, 0:64], r=[psU], w=[U], eng='dve')
                p.mm(psY[0:64, cs], [(Tst[h][:, :], rt[:, cs]), (U[:, :], ArbT[:, cs]), (vT[:, cs], ArkT[:, cs])],
                     r=[Tst[h], rt, U, ArbT, vT, ArkT], w=[psY])
                psT2 = p.psum()
                p.mm(psT2[0:64, 0:64], [(bhT[:, cs], U[:, :]), (khT[:, cs], vT[:, cs])], r=[bhT, U, khT, vT], w=[psT2])
                p.stt(Tst[h][:, :], Tst[h][:, :], WC[:, c, :], psT2[0:64, 0:64], ALU.mult, ALU.add, r=[Tst[h], WC, psT2], w=[Tst[h]])

        def norm(tt, h, B, psY):
            p.ps_pool = POOLA
            tsl = slice(tt * TT, (tt + 1) * TT)
            ones = cst.onesm64
            p.copy(no[:, :], psY[0:64, :], r=[psY], w=[no], eng='act')
            p.act(nosq[:, :], psY[0:64, :], AF.Square, r=[psY], w=[nosq])
            psM = p.psum()
            p.mm(psM[0:64, :], [(ones[:, :], no[:, :])], r=[ones, no], w=[psM])
            psQ2 = p.psum()
            p.mm(psQ2[0:64, :], [(ones[:, :], nosq[:, :])], r=[ones, nosq], w=[psQ2])
            p.copy(nmean[:, :], psM[0:64, :], r=[psM], w=[nmean], eng='act')
            p.act(nmsq[:, :], psM[0:64, :], AF.Square, r=[psM], w=[nmsq])
            p.tt(nosq[:, :], psQ2[0:64, :], nmsq[:, :], ALU.subtract, r=[psQ2, nmsq], w=[nosq])
            p.ts(nosq[:, :], nosq[:, :], 0.0, 64e-5, ALU.max, ALU.add, r=[nosq], w=[nosq])
            p.act(nmsq[:, :], nosq[:, :], AF.Sqrt, r=[nosq], w=[nmsq])
            p.op('dve', lambda: nc.vector.reciprocal(out=nosq[:, :], in_=nmsq[:, :]), r=[nmsq], w=[nosq])
            p.tt(no[:, :], no[:, :], nmean[:, :], ALU.subtract, r=[no, nmean], w=[no])
            p.tt(no[:, :], no[:, :], nosq[:, :], ALU.mult, r=[no, nosq], w=[no])
            p.ts(no[:, :], no[:, :], col('lnx_g', h), col('lnx_b', h), ALU.mult, ALU.add, r=[no, c64], w=[no])
            p.tt(no[:, :], no[:, :], B['bonus'][:, :], ALU.add, r=[no, B['bonus']], w=[no])
            p.tt(orw[:, :], no[:, :], B['gh'][:, :], ALU.mult, r=[no, B['gh']], w=[orw])
            p.dma('sp', SC['orT'][h * 64:(h + 1) * 64, tsl], orw[:, :], r=[orw], w=[('orT', h, tt)])

        seq = [(tt, h) for tt in range(NT) for h in range(8)]
        NS = len(seq)

        def A_stream(n):
            def f():
                if 0 <= n - 1 < NS:
                    t_, h_ = seq[n - 1]
                    norm(t_, h_, HB[(n - 1) % 3], psYs[(n - 1) % 2])
                if n + 2 < NS:
                    t_, h_ = seq[n + 2]
                    stage1a(t_, h_, HB[(n + 2) % 3], AQ[(n + 2) % 2])
            return p.record(f)

        def B_stream(n):
            def f():
                if n + 1 < NS:
                    stage1b(HB[(n + 1) % 3], AQ[(n + 1) % 2])
            return p.record(f)

        def C_stream(n):
            def f():
                if 0 <= n < NS:
                    t_, h_ = seq[n]
                    chain(t_, h_, HB[n % 3], psYs[n % 2])
            return p.record(f)
        for n in range(-2, NS + 1):
            lists = [C_stream(n), B_stream(n), A_stream(n)]
            if cstream is not None and 0 <= n < NS:
                lists.append(cstream.take(NS - n))
            p.play(lists)
    p.ps_pool = list(range(8))
```

```python
import numpy as np
from contextlib import ExitStack, contextmanager
import concourse.bass as bass
import concourse.mybir as mybir
from concourse.bass_utils import run_bass_kernel_spmd

F32 = mybir.dt.float32
BF16 = mybir.dt.bfloat16
I32 = mybir.dt.int32
AF = mybir.ActivationFunctionType
ALU = mybir.AluOpType

T = 4096
TT = 512
NT = T // TT
CH = 64
NCH = TT // CH
DM = 1024
NIN = 7472
NMIX = 4400
NMIXP = 4480
ALPHA = 4.0 ** 0.25
FD = 2816
FE = 3584
NE = 8
C0 = float(np.exp(-0.5))

Q0, K0, V0, R0, DEC0, LX0, LG0, RW0 = 0, 256, 512, 1024, 1536, 1552, 2064, 2576
RWR, RWK, RWV, RWW, RWA, RWG = RW0, RW0 + 512, RW0 + 1024, RW0 + 1536, RW0 + 1600, RW0 + 1664

C128 = {}
_o = 0
for _n, _c in [('b_in', 35), ('b_gate', 24), ('gla_ng', 4), ('gla_nb', 4), ('conv_w', 16), ('conv_b', 4),
               ('b_r', 4), ('b_i', 4), ('lam', 4), ('ln_mix_g', 8), ('ln_mix_b', 8), ('ln_ffn_g', 8),
               ('ln_ffn_b', 8), ('mu_g', 2)]:
    C128[_n] = _o
    _o += _c
N128 = _o
C64 = {}
_o = 0
for _n, _c in [('gla_bd', 4), ('mu_r', 8), ('mu_k', 8), ('mu_v', 8), ('mu_w', 1), ('mu_a', 1), ('w0', 8), ('a0', 8),
               ('k_k', 8), ('k_a', 8), ('r_k', 8), ('lnx_g', 8), ('lnx_b', 8)]:
    C64[_n] = _o
    _o += _c
N64 = _o


def _cols(v, P):
    v = np.asarray(v, np.float32).reshape(-1)
    n = -(-v.size // P) * P
    pad = np.zeros(n, np.float32)
    pad[:v.size] = v
    return pad.reshape(n // P, P).T


def pack_cols(inp, l):
    c128 = np.zeros((128, N128), np.float32)
    c64 = np.zeros((64, N64), np.float32)

    def put(dst, table, name, arr, P):
        a = _cols(arr, P)
        dst[:, table[name]:table[name] + a.shape[1]] = a
    put(c128, C128, 'b_in', inp['b_in'][l][:NMIX], 128)
    put(c128, C128, 'b_gate', inp['b_in'][l][NMIX:], 128)
    put(c128, C128, 'gla_ng', inp['gla_norm_g'][l], 128)
    put(c128, C128, 'gla_nb', inp['gla_norm_b'][l], 128)
    put(c128, C128, 'conv_w', inp['lru_conv_w'][l], 128)
    put(c128, C128, 'conv_b', inp['lru_conv_b'][l], 128)
    put(c128, C128, 'b_r', inp['lru_b_r'][l], 128)
    put(c128, C128, 'b_i', inp['lru_b_i'][l], 128)
    put(c128, C128, 'lam', inp['lru_lambda'][l], 128)
    put(c128, C128, 'ln_mix_g', inp['ln_mix_g'][l], 128)
    put(c128, C128, 'ln_mix_b', inp['ln_mix_b'][l], 128)
    put(c128, C128, 'ln_ffn_g', inp['ln_ffn_g'][l], 128)
    put(c128, C128, 'ln_ffn_b', inp['ln_ffn_b'][l], 128)
    mu = inp['rwkv_mu'][l]
    put(c128, C128, 'mu_g', mu[1664:1824], 128)
    put(c64, C64, 'gla_bd', inp['gla_b_decay'][l], 64)
    put(c64, C64, 'mu_r', mu[0:512], 64)
    put(c64, C64, 'mu_k', mu[512:1024], 64)
    put(c64, C64, 'mu_v', mu[1024:1536], 64)
    put(c64, C64, 'mu_w', mu[1536:1600], 64)
    put(c64, C64, 'mu_a', mu[1600:1664], 64)
    put(c64, C64, 'w0', inp['rwkv_w0'][l], 64)
    put(c64, C64, 'a0', inp['rwkv_a0'][l], 64)
    put(c64, C64, 'k_k', inp['rwkv_k_k'][l], 64)
    put(c64, C64, 'k_a', inp['rwkv_k_a'][l], 64)
    put(c64, C64, 'r_k', inp['rwkv_r_k'][l], 64)
    put(c64, C64, 'lnx_g', inp['rwkv_lnx_g'][l], 64)
    put(c64, C64, 'lnx_b', inp['rwkv_lnx_b'][l], 64)
    return c128, c64


class Tl:
    def __init__(self, t, key):
        self.t = t
        self.key = key

    def __getitem__(self, idx):
        return self.t[idx]


def _keys(xs):
    out = []
    for x in xs:
        if isinstance(x, Tl):
            out.append(x.key)
        elif isinstance(x, list):
            out.extend(_keys(x))
        else:
            out.append(x)
    return out


class Prog:
    NDMA = 4

    def __init__(self, nc, es):
        self.nc = nc
        self.es = es
        self.engs = {'pe': nc.tensor, 'act': nc.scalar, 'dve': nc.vector, 'pool': nc.gpsimd, 'sp': nc.sync}
        self.sem = {}
        self.cnt = {}
        for n in self.engs:
            self.sem[n] = es.enter_context(nc.semaphore('s_' + n))
            self.cnt[n] = 0
        self.dq = {}
        for q in ('sp', 'pool', 'act'):
            sems = []
            for i in range(self.NDMA):
                nm = 'd_%s%d' % (q, i)
                self.sem[nm] = es.enter_context(nc.semaphore(nm))
                self.cnt[nm] = 0
                sems.append(nm)
            self.dq[q] = [sems, 0]
        self.seen = {}
        self.rec = None
        self.lw = {}
        self.rd = {}
        self.nwait = 0
        self.ninst = 0
        self.uid = 0
        self.ps = [Tl(es.enter_context(nc.psum_tensor('ps%d' % i, [128, 512], F32)), 'ps%d' % i) for i in range(8)]
        self.ps_rots = {}
        self.ps_pool = list(range(8))

    def _wait(self, eng, deps):
        e = self.engs[eng]
        for s, v in deps:
            if v <= 0 or self.seen.get((eng, s), 0) >= v:
                continue
            e.wait_ge(self.sem[s], v)
            self.nwait += 1
            self.seen[(eng, s)] = v

    def _deps(self, reads, writes):
        deps = {}
        for k in reads:
            if k in self.lw:
                s, v = self.lw[k]
                deps[s] = max(deps.get(s, 0), v)
        for k in writes:
            if k in self.lw:
                s, v = self.lw[k]
                deps[s] = max(deps.get(s, 0), v)
            for s, v in self.rd.get(k, {}).items():
                deps[s] = max(deps.get(s, 0), v)
        return list(deps.items())

    def _mark(self, s, v, reads, writes):
        for k in writes:
            self.lw[k] = (s, v)
            self.rd[k] = {}
        for k in reads:
            d = self.rd.setdefault(k, {})
            d[s] = max(d.get(s, 0), v)

    def op(self, eng, fn, r=(), w=()):
        if self.rec is not None:
            self.rec.append(('op', eng, fn, r, w, None))
            return
        reads, writes = _keys(r), _keys(w)
        self._wait(eng, self._deps(reads, writes))
        inst = fn()
        self.cnt[eng] += 1
        inst.then_inc(self.sem[eng], 1)
        self.ninst += 1
        self._mark(eng, self.cnt[eng], reads, writes)

    def dma(self, q, out, in_, r=(), w=(), **kw):
        if self.rec is not None:
            self.rec.append(('dma', q, (out, in_), r, w, kw))
            return
        reads, writes = _keys(r), _keys(w)
        sems, i = self.dq[q]
        s = sems[i % self.NDMA]
        self.dq[q][1] = i + 1
        deps = self._deps(reads, writes)
        deps.append((s, self.cnt[s]))
        self._wait(q, deps)
        inst = self.engs[q].dma_start(out=out, in_=in_, **kw)
        self.cnt[s] += 16
        inst.then_inc(self.sem[s], 16)
        self.ninst += 1
        self._mark(s, self.cnt[s], reads, writes)

    def record(self, fn):
        self.rec = []
        fn()
        L = self.rec
        self.rec = None
        return L

    def play(self, lists):
        lists = [L for L in lists if L]
        items = []
        for L in lists:
            n = float(len(L))
            items.extend(((i + 0.5) / n, j, i, it) for j, L2 in enumerate([L]) for i, it in enumerate(L))
        tagged = []
        for li, L in enumerate(lists):
            n = float(len(L))
            for i, it in enumerate(L):
                tagged.append(((i + 0.5) / n, li, i, it))
        tagged.sort(key=lambda t: (t[0], t[1], t[2]))
        for _, _, _, it in tagged:
            kind, a, b, r, w, kw = it
            if kind == 'op':
                self.op(a, b, r, w)
            else:
                self.dma(a, b[0], b[1], r, w, **kw)

    def barrier(self):
        for e in self.engs:
            self._wait(e, [(s, v) for s, v in self.cnt.items()])

    def sb(self, stack, name, shape, dt=F32):
        self.uid += 1
        t = stack.enter_context(self.nc.sbuf_tensor('%s_%d' % (name, self.uid), shape, dt))
        return Tl(t, '%s_%d' % (name, self.uid))

    @contextmanager
    def scope(self):
        with ExitStack() as st:
            yield st
            self.barrier()

    def psum(self):
        key = tuple(self.ps_pool)
        rot = self.ps_rots.get(key, 0)
        self.ps_rots[key] = rot + 1
        return self.ps[self.ps_pool[rot % len(self.ps_pool)]]

    def act(self, out, in_, func, bias=0.0, scale=1.0, r=(), w=()):
        nc = self.nc
        self.op('act', lambda: nc.scalar.activation(out=out, in_=in_, func=func, bias=bias, scale=scale), r, w)

    def tt(self, out, in0, in1, op, r=(), w=(), eng='dve'):
        e = self.engs[eng]
        self.op(eng, lambda: e.tensor_tensor(out=out, in0=in0, in1=in1, op=op), r, w)

    def ts(self, out, in0, s1, s2, op0, op1=None, r=(), w=(), eng='dve'):
        e = self.engs[eng]
        if op1 is None:
            self.op(eng, lambda: e.tensor_scalar(out=out, in0=in0, scalar1=s1, scalar2=None, op0=op0), r, w)
        else:
            self.op(eng, lambda: e.tensor_scalar(out=out, in0=in0, scalar1=s1, scalar2=s2, op0=op0, op1=op1), r, w)

    def stt(self, out, in0, scalar, in1, op0, op1, r=(), w=()):
        nc = self.nc
        self.op('dve', lambda: nc.vector.scalar_tensor_tensor(out=out, in0=in0, scalar=scalar, in1=in1, op0=op0, op1=op1), r, w)

    def copy(self, out, in_, r=(), w=(), eng='dve'):
        nc = self.nc
        if eng == 'act':
            self.op('act', lambda: nc.scalar.copy(out=out, in_=in_), r, w)
        else:
            e = self.engs[eng]
            self.op(eng, lambda: e.tensor_copy(out=out, in_=in_), r, w)

    def mm(self, out, pairs, r=(), w=()):
        nc = self.nc
        n = len(pairs)

        def f():
            inst = None
            for i, (lt, rh) in enumerate(pairs):
                inst = nc.tensor.matmul(out, lt, rh, start=(i == 0), stop=(i == n - 1))
            return inst
        self.op('pe', f, r, w)

    def mms(self, groups, r=(), w=()):
        nc = self.nc

        def f():
            inst = None
            for out, pairs in groups:
                n = len(pairs)
                for i, (lt, rh) in enumerate(pairs):
                    inst = nc.tensor.matmul(out, lt, rh, start=(i == 0), stop=(i == n - 1))
            return inst
        self.op('pe', f, r, w)

    def transposes(self, items, ident, r=(), w=()):
        nc = self.nc

        def f():
            inst = None
            for out, in_ in items:
                inst = nc.tensor.transpose(out, in_, ident)
            return inst
        self.op('pe', f, r, w)


def hk(name, r0, nrows, tt, halo=False):
    ks = []
    for c in range(r0 // 128, (r0 + nrows - 1) // 128 + 1):
        ks.append((name, c, tt))
        if halo and tt > 0:
            ks.append((name, c, tt - 1))
    return ks


USED = []


def build(stop_after=None, dbg=(), tlen=4096, layers=(0, 1)):
    global T, NT
    T = tlen
    NT = T // TT
    del USED[:]
    nc = bass.Bass("TRN2", target_bir_lowering=False)
    SHAPES = {'xT': [DM, T], 'w_in': [2, DM, NIN], 'c128': [2, 128, N128], 'c64': [2, 64, N64], 'b_v': [2, 512],
              'gla_wup': [2, 16, 256], 'lru_w_r': [2, 8, 64, 64], 'lru_w_i': [2, 8, 64, 64], 'rwkv_w2': [2, 64, 512],
              'rwkv_a2': [2, 64, 512], 'rwkv_g2': [2, 160, 512], 'p_gla': [2, 512, DM], 'p_lru': [2, 512, DM],
              'p_rwkv': [2, 512, DM], 'w_out': [2, DM, DM], 'ffn_w_gate': [DM, FD], 'ffn_w_up': [DM, FD],
              'ffn_w_down': [FD, DM], 'moe_w_router': [DM, NE], 'moe_w_gate': [NE, DM, FE], 'moe_w_up': [NE, DM, FE],
              'moe_w_down': [NE, FE, DM]}

    class Lazy(dict):
        def __missing__(self, name):
            ap = nc.dram_tensor(name, SHAPES[name], F32, kind="ExternalInput").ap()
            self[name] = ap
            USED.append(name)
            return ap
    IN = Lazy()
    outT = nc.dram_tensor('outT', [DM, T], F32, kind="ExternalOutput").ap()

    SC = {}

    def dsc(name, shape, dt=F32):
        kind = "ExternalOutput" if name in dbg else "Internal"
        SC[name] = nc.dram_tensor(name, shape, dt, kind=kind).ap()
    dsc('hT', [NMIXP, T])
    dsc('vtok', [T, 512])
    dsc('ogT', [512, T], BF16)
    dsc('olT', [512, T], BF16)
    dsc('orT', [512, T], BF16)
    dsc('x1T', [DM, T])
    dsc('x2T', [DM, T])
    dsc('wb_in', [2, DM, NIN], BF16)
    dsc('wb_pg', [2, 512, DM], BF16)
    dsc('wb_pl', [2, 512, DM], BF16)
    dsc('wb_pr', [2, 512, DM], BF16)
    dsc('wb_out', [2, DM, DM], BF16)
    dsc('wb_fg', [DM, FD], BF16)
    dsc('wb_fu', [DM, FD], BF16)
    dsc('wb_fd', [FD, DM], BF16)
    dsc('wb_mg', [NE, DM, FE], BF16)
    dsc('wb_mu', [NE, DM, FE], BF16)
    dsc('wb_md', [NE, FE, DM], BF16)

    with ExitStack() as es:
        p = Prog(nc, es)
        _build_body(nc, p, IN, SC, outT, stop_after, layers)
        p.barrier()
        print('program: ninst', p.ninst, 'nwait', p.nwait)
    return nc


def flat128(ap2d_elems, ap):
    return ap


def conv_weights(nc, p, pairs):
    CW = 7168
    with p.scope() as st:
        fb = [p.sb(st, 'cvf', [128, CW], F32) for _ in range(2)]
        bb = [p.sb(st, 'cvb', [128, CW], BF16) for _ in range(2)]
        rnd = 0
        engs = ['pool', 'act', 'dve']
        for key, src, dst, nel in pairs:
            M = nel // 128
            s2 = src.rearrange("(p m) -> p m", p=128)
            d2 = dst.rearrange("(p m) -> p m", p=128)
            c0 = 0
            while c0 < M:
                cw = min(CW, M - c0)
                f, b = fb[rnd % 2], bb[rnd % 2]
                p.dma('sp', f[:, 0:cw], s2[:, c0:c0 + cw], w=[f])
                p.copy(b[:, 0:cw], f[:, 0:cw], r=[f], w=[b], eng=engs[rnd % 3])
                p.dma('act', d2[:, c0:c0 + cw], b[:, 0:cw], r=[b], w=[('cv', rnd)])
                c0 += cw
                rnd += 1


class ConvStream:
    CW = 1792

    def __init__(self, nc, p, st, pairs):
        self.p = p
        CW = self.CW
        fb = [p.sb(st, 'cvf2', [128, CW]) for _ in range(2)]
        bb = [p.sb(st, 'cvb2', [128, CW], BF16) for _ in range(2)]

        def rec():
            rnd = 0
            for key, src, dst, nel in pairs:
                M = nel // 128
                assert M % CW == 0
                s2 = src.rearrange("(p m) -> p m", p=128)
                d2 = dst.rearrange("(p m) -> p m", p=128)
                for c0 in range(0, M, CW):
                    f, b = fb[rnd % 2], bb[rnd % 2]
                    p.dma('sp', f[:, :], s2[:, c0:c0 + CW], w=[f])
                    p.copy(b[:, :], f[:, :], r=[f], w=[b], eng='act')
                    p.dma('sp', d2[:, c0:c0 + CW], b[:, :], r=[b], w=[('cv2', rnd)])
                    rnd += 1
        self.ops = p.record(rec)
        self.pos = 0

    def take(self, steps_left):
        left = len(self.ops) - self.pos
        if left <= 0:
            return []
        rounds = -(-(left // 3) // max(1, steps_left))
        n = min(left, rounds * 3)
        out = self.ops[self.pos:self.pos + n]
        self.pos += n
        return out

    def flush(self):
        rest = self.ops[self.pos:]
        self.pos = len(self.ops)
        if rest:
            self.p.play([rest])


def _build_body(nc, p, IN, SC, outT, stop_after, layers):
    def fl(ap):
        nd = len(ap.shape)
        if nd == 2:
            return ap.rearrange("a b -> (a b)")
        if nd == 3:
            return ap.rearrange("a b c -> (a b c)")
        return ap

    pairs = []
    for l in layers:
        pairs.append((('wb_in', l), fl(IN['w_in'][l]), fl(SC['wb_in'][l]), DM * NIN))
        pairs.append((('wb_pg', l), fl(IN['p_gla'][l]), fl(SC['wb_pg'][l]), 512 * DM))
        pairs.append((('wb_pl', l), fl(IN['p_lru'][l]), fl(SC['wb_pl'][l]), 512 * DM))
        pairs.append((('wb_pr', l), fl(IN['p_rwkv'][l]), fl(SC['wb_pr'][l]), 512 * DM))
        pairs.append((('wb_out', l), fl(IN['w_out'][l]), fl(SC['wb_out'][l]), DM * DM))
    if 0 in layers:
        pairs.append((('wb_fg', 0), fl(IN['ffn_w_gate']), fl(SC['wb_fg']), DM * FD))
        pairs.append((('wb_fu', 0), fl(IN['ffn_w_up']), fl(SC['wb_fu']), DM * FD))
        pairs.append((('wb_fd', 0), fl(IN['ffn_w_down']), fl(SC['wb_fd']), DM * FD))
    conv_weights(nc, p, pairs)
    es_conv = ExitStack()
    cstream = None
    if 1 in layers:
        mpairs = [(('wb_mg', 0), fl(IN['moe_w_gate']), fl(SC['wb_mg']), NE * DM * FE),
                  (('wb_mu', 0), fl(IN['moe_w_up']), fl(SC['wb_mu']), NE * DM * FE),
                  (('wb_md', 0), fl(IN['moe_w_down']), fl(SC['wb_md']), NE * DM * FE)]
        cstream = ConvStream(nc, p, es_conv, mpairs)
    if stop_after == ('conv', 0):
        return
    for l in layers:
        xin = IN['xT'] if l == 0 else SC['x2T']
        phase1(nc, p, IN, SC, l, xin)
        if stop_after == ('p1', l):
            return
        if mixers(nc, p, IN, SC, l, stop_after, cstream if l == 0 else None):
            return
        if l == 0 and cstream is not None:
            cstream.flush()
            p.barrier()
            es_conv.close()
        last = (l == layers[-1]) and l == 1
        phase_tail(nc, p, IN, SC, l, xin, outT if last else SC['x2T'], 'outT' if last else 'x2T')
        if stop_after == ('tail', l):
            return


def phase1(nc, p, IN, SC, l, xin):
    with p.scope() as st:
        xb = p.sb(st, 'xb', [128, 8, T], BF16)
        xf = [p.sb(st, 'xf', [128, 8, TT], F32) for _ in range(2)]
        c128 = p.sb(st, 'c128', [128, N128])
        p.dma('sp', c128[:, :], IN['c128'][l], w=[c128])
        xin3 = xin.rearrange("(kc p) t -> p kc t", p=128)
        for tt in range(NT):
            f = xf[tt % 2]
            rkeys = [('x2T', c, tt) for c in range(8)] if l > 0 else []
            p.dma('sp', f[:, :, :], xin3[:, :, tt * TT:(tt + 1) * TT], r=rkeys, w=[f])
            p.copy(xb[:, :, tt * TT:(tt + 1) * TT], f[:, :, :], r=[f], w=[('xb', tt)], eng=['act', 'dve'][tt % 2])
        wg = [p.sb(st, 'wg', [128, 8, 512], BF16) for _ in range(2)]
        stg = [p.sb(st, 'stg', [128, TT], F32) for _ in range(4)]
        w3 = SC['wb_in'][l].rearrange("(kc p) n -> p kc n", p=128)
        si = 0
        ngroups = (NMIX + 511) // 512
        for g in range(ngroups):
            n0 = g * 512
            if n0 == V0:
                continue
            gw = min(512, NMIX - n0)
            W = wg[g % 2]
            p.dma('sp', W[:, :, 0:gw], w3[:, :, n0:n0 + gw], w=[W])
            for tt in range(NT):
                for j in range((gw + 127) // 128):
                    cw = min(128, gw - j * 128)
                    ps = p.psum()
                    p.mm(ps[0:cw, :], [(W[:, kc, j * 128:j * 128 + cw], xb[:, kc, tt * TT:(tt + 1) * TT]) for kc in range(8)],
                         r=[W, ('xb', tt)], w=[ps])
                    s = stg[si % 4]
                    si += 1
                    row = n0 + j * 128
                    col = C128['b_in'] + row // 128
                    p.act(s[0:cw, :], ps[0:cw, :], AF.Identity, bias=c128[0:cw, col:col + 1], r=[ps, c128], w=[s])
                    p.dma('sp', SC['hT'][row:row + cw, tt * TT:(tt + 1) * TT], s[0:cw, :], r=[s], w=[('hT', row // 128, tt)])
        bv = p.sb(st, 'bv', [128, 512])
        p.dma('sp', bv[:, :], IN['b_v'][l].partition_broadcast(128), w=[bv])
        W = wg[ngroups % 2]
        p.dma('sp', W[:, :, :], w3[:, :, V0:V0 + 512], w=[W])
        for tb in range(T // 128):
            ps = p.psum()
            p.mm(ps[:, :], [(xb[:, kc, tb * 128:(tb + 1) * 128], W[:, kc, :]) for kc in range(8)], r=[W, ('xb', tb // 4)], w=[ps])
            s = stg[si % 4]
            si += 1
            p.tt(s[:, :], ps[:, :], bv[:, :], ALU.add, r=[ps, bv], w=[s])
            p.dma('sp', SC['vtok'][tb * 128:(tb + 1) * 128, :], s[:, :], r=[s], w=[('vtok', tb)])


def make_in_maps(inputs):
    inp = {k: np.asarray(v) for k, v in inputs.items()}
    cc = [pack_cols(inp, l) for l in range(2)]
    shared = {
        'w_in': inp['w_in'],
        'c128': np.stack([cc[0][0], cc[1][0]]),
        'c64': np.stack([cc[0][1], cc[1][1]]),
        'b_v': np.ascontiguousarray(inp['b_in'][:, V0:V0 + 512]),
        'gla_wup': inp['gla_w_decay_up'],
        'lru_w_r': inp['lru_w_r'], 'lru_w_i': inp['lru_w_i'],
        'rwkv_w2': inp['rwkv_w2'], 'rwkv_a2': inp['rwkv_a2'], 'rwkv_g2': inp['rwkv_g2'],
        'p_gla': inp['p_gla'], 'p_lru': inp['p_lru'], 'p_rwkv': inp['p_rwkv'], 'w_out': inp['w_out'],
        'ffn_w_gate': inp['ffn_w_gate'][0], 'ffn_w_up': inp['ffn_w_up'][0], 'ffn_w_down': inp['ffn_w_down'][0],
        'moe_w_router': inp['moe_w_router'][0], 'moe_w_gate': inp['moe_w_gate'][0], 'moe_w_up': inp['moe_w_up'][0],
        'moe_w_down': inp['moe_w_down'][0],
    }
    shared = {k: np.ascontiguousarray(v, dtype=np.float32) for k, v in shared.items()}
    maps = []
    for b in range(8):
        m = {k: v for k, v in shared.items() if k in USED}
        m['xT'] = np.ascontiguousarray(inp['x'][b, :T].T)
        maps.append(m)
    return maps


def kernel(**inputs):
    nc = build()
    maps = make_in_maps(inputs)
    res = run_bass_kernel_spmd(nc, maps, core_ids=list(range(8)))
    out = np.stack([np.ascontiguousarray(res.results[b]['outT'].T) for b in range(8)])
    return out.astype(np.float32)


class Consts:
    pass


def make_consts(nc, p, st):
    c = Consts()
    c.ident = p.sb(st, 'ident', [64, 64])
    c.ones64 = p.sb(st, 'ones64', [64, 64])
    c.onesm128 = p.sb(st, 'onesm128', [128, 128])
    c.onesm64 = p.sb(st, 'onesm64', [64, 64])
    c.m01 = p.sb(st, 'm01', [64, TT])
    c.mge = p.sb(st, 'mge', [64, NCH, CH])
    c.mgt = p.sb(st, 'mgt', [64, NCH, CH])
    c.mlt = p.sb(st, 'mlt', [64, NCH, CH])
    c.id8 = p.sb(st, 'id8', [64, NCH, CH])
    ones8 = p.sb(st, 'ones8', [64, NCH, CH])
    p.op('pool', lambda: nc.gpsimd.memset(c.ones64[:, :], 1.0), w=[c.ones64])
    p.op('pool', lambda: nc.gpsimd.memset(c.onesm128[:, :], 1.0 / 128.0), w=[c.onesm128])
    p.op('pool', lambda: nc.gpsimd.memset(c.onesm64[:, :], 1.0 / 64.0), w=[c.onesm64])
    p.op('pool', lambda: nc.gpsimd.memset(ones8[:, :, :], 1.0), w=[ones8])
    p.op('pool', lambda: nc.gpsimd.memset(c.m01[:, :], 1.0), w=[c.m01])
    m3 = c.m01[:, :].rearrange("p (c t) -> p c t", t=CH)
    p.op('pool', lambda: nc.gpsimd.memset(m3[:, :, 0:1], 0.0), r=[c.m01], w=[c.m01])
    pat = [[0, NCH], [1, CH]]
    p.op('pool', lambda: nc.gpsimd.affine_select(out=c.mge[:, :, :], in_=ones8[:, :, :], pattern=pat, compare_op=ALU.is_ge,
                                                 fill=0.0, base=0, channel_multiplier=-1), r=[ones8], w=[c.mge])
    p.op('pool', lambda: nc.gpsimd.affine_select(out=c.mgt[:, :, :], in_=ones8[:, :, :], pattern=pat, compare_op=ALU.is_gt,
                                                 fill=0.0, base=0, channel_multiplier=-1), r=[ones8], w=[c.mgt])
    pat2 = [[0, NCH], [-1, CH]]
    p.op('pool', lambda: nc.gpsimd.affine_select(out=c.mlt[:, :, :], in_=ones8[:, :, :], pattern=pat2, compare_op=ALU.is_gt,
                                                 fill=0.0, base=0, channel_multiplier=1), r=[ones8], w=[c.mlt])
    p.tt(c.id8[:, :, :], c.mge[:, :, :], c.mgt[:, :, :], ALU.subtract, r=[c.mge, c.mgt], w=[c.id8])
    p.copy(c.ident[:, :], c.id8[:, 0, :], r=[c.id8], w=[c.ident])
    return c


def cview(t):
    return t[:, :].rearrange("p (c t) -> p c t", t=CH)


def fm_norm(nc, p, st, cst, ps, P, eps, gcol, bcol, tag, tiles=None):
    ones = cst.onesm128 if P == 128 else cst.onesm64
    if tiles is not None:
        o, osq, mean, msq = tiles
    else:
        o = p.sb(st, tag + 'o', [P, TT])
        osq = p.sb(st, tag + 'osq', [P, TT])
    p.copy(o[:, :], ps[0:P, :], r=[ps], w=[o], eng='act')
    p.act(osq[:, :], ps[0:P, :], AF.Square, r=[ps], w=[osq])
    psM = p.psum()
    p.mm(psM[0:P, :], [(ones[:, :], o[:, :])], r=[ones, o], w=[psM])
    psQ = p.psum()
    p.mm(psQ[0:P, :], [(ones[:, :], osq[:, :])], r=[ones, osq], w=[psQ])
    if tiles is None:
        mean = p.sb(st, tag + 'mean', [P, TT])
        msq = p.sb(st, tag + 'msq', [P, TT])
    p.copy(mean[:, :], psM[0:P, :], r=[psM], w=[mean], eng='act')
    p.act(msq[:, :], psM[0:P, :], AF.Square, r=[psM], w=[msq])
    var = osq
    p.tt(var[:, :], psQ[0:P, :], msq[:, :], ALU.subtract, r=[psQ, msq], w=[var])
    p.ts(var[:, :], var[:, :], 0.0, eps, ALU.max, ALU.add, r=[var], w=[var])
    p.act(msq[:, :], var[:, :], AF.Sqrt, r=[var], w=[msq])
    p.op('dve', lambda: nc.vector.reciprocal(out=var[:, :], in_=msq[:, :]), r=[msq], w=[var])
    p.tt(o[:, :], o[:, :], mean[:, :], ALU.subtract, r=[o, mean], w=[o])
    p.tt(o[:, :], o[:, :], var[:, :], ALU.mult, r=[o, var], w=[o])
    p.ts(o[:, :], o[:, :], gcol, bcol, ALU.mult, ALU.add, r=[o], w=[o])
    return o


def phase_gla(nc, p, IN, SC, l, cst, c128, c64, st0):
    p.ps_pool = [0, 1, 2, 3, 4]
    if True:
        wup = p.sb(st0, 'wup', [16, 256])
        p.dma('sp', wup[:, :], IN['gla_wup'][l], w=[wup])
        negbd = p.sb(st0, 'negbd', [64, 4])
        p.ts(negbd[:, :], c64[:, C64['gla_bd']:C64['gla_bd'] + 4], -1.0, None, ALU.mult, r=[c64], w=[negbd])
        S = [p.sb(st0, 'S%d' % h, [64, NCH + 1, 128]) for h in range(4)]
        for h in range(4):
            p.op('pool', lambda h=h: nc.gpsimd.memset(S[h][:, 0, :], 0.0), w=[S[h]])
        G = {}
        for nm, shp, dt in [('dec', [16, TT], F32), ('q', [64, TT], F32), ('k', [64, TT], F32), ('v', [64, NCH, 128], F32),
                            ('r', [128, TT], F32), ('l1', [64, TT], F32), ('cum', [64, TT], F32), ('E', [64, TT], F32),
                            ('qd', [64, TT], F32), ('ki', [64, TT], F32), ('ke', [64, TT], F32), ('D', [64, NCH, CH], F32),
                            ('dend', [64, NCH, 1], F32), ('keT', [64, TT], F32), ('sc', [64, TT], F32), ('dst', [64, NCH, 128], F32),
                            ('no', [128, TT], F32), ('nosq', [128, TT], F32), ('nmean', [128, TT], F32), ('nmsq', [128, TT], F32),
                            ('og', [128, TT], BF16)]:
            G[nm] = p.sb(st0, 'g_' + nm, shp, dt)
        for tt in range(NT):
            tsl = slice(tt * TT, (tt + 1) * TT)
            for h in range(4):
                if True:
                    st = None
                    dec = G['dec']
                    p.dma('sp', dec[:, :], SC['hT'][DEC0:DEC0 + 16, tsl], r=hk('hT', DEC0, 16, tt), w=[dec])
                    q = G['q']
                    k = G['k']
                    v = G['v']
                    r = G['r']
                    p.dma('sp', q[:, :], SC['hT'][Q0 + h * 64:Q0 + (h + 1) * 64, tsl], r=hk('hT', Q0 + h * 64, 64, tt), w=[q])
                    p.dma('sp', k[:, :], SC['hT'][K0 + h * 64:K0 + (h + 1) * 64, tsl], r=hk('hT', K0 + h * 64, 64, tt), w=[k])
                    p.dma('sp', v[:, :, :], SC['vtok'][tsl, h * 128:(h + 1) * 128].rearrange("(c t) e -> t c e", t=CH),
                          r=[('vtok', tb) for tb in range(tt * 4, tt * 4 + 4)], w=[v])
                    p.dma('sp', r[:, :], SC['hT'][R0 + h * 128:R0 + (h + 1) * 128, tsl], r=hk('hT', R0 + h * 128, 128, tt), w=[r])
                    ps = p.psum()
                    p.mm(ps[0:64, :], [(wup[:, h * 64:(h + 1) * 64], dec[:, :])], r=[wup, dec], w=[ps])
                    l1 = G['l1']
                    p.act(l1[:, :], ps[0:64, :], AF.Exp, bias=negbd[:, h:h + 1], scale=-1.0, r=[ps, negbd], w=[l1])
                    p.act(l1[:, :], l1[:, :], AF.Ln, bias=1.0, scale=1.0, r=[l1], w=[l1])
                    cum = G['cum']
                    p.op('dve', lambda: nc.vector.tensor_tensor_scan(out=cum[:, :], data0=cst.m01[:, :], data1=l1[:, :], initial=0.0,
                                                                     op0=ALU.mult, op1=ALU.add), r=[cst.m01, l1], w=[cum])
                    E = G['E']
                    qd = G['qd']
                    ki = G['ki']
                    ke = G['ke']
                    p.act(E[:, :], cum[:, :], AF.Exp, scale=-1.0 / 16.0, r=[cum], w=[E])
                    p.stt(qd[:, :], q[:, :], 0.125, E[:, :], ALU.mult, ALU.mult, r=[q, E], w=[qd])
                    p.act(E[:, :], cum[:, :], AF.Exp, scale=1.0 / 16.0, r=[cum], w=[E])
                    p.tt(ki[:, :], k[:, :], E[:, :], ALU.mult, r=[k, E], w=[ki])
                    c3 = cview(cum)
                    D = G['D']
                    p.tt(D[:, :, :], c3[:, :, CH - 1:CH].to_broadcast([64, NCH, CH]), c3, ALU.subtract, r=[cum], w=[D])
                    p.act(D[:, :, :], D[:, :, :], AF.Exp, scale=-1.0 / 16.0, r=[D], w=[D])
                    p.tt(ke[:, :], k[:, :], D[:, :, :].rearrange("p c t -> p (c t)"), ALU.mult, r=[k, D], w=[ke])
                    dend = G['dend']
                    p.act(dend[:, :, :], c3[:, :, CH - 1:CH], AF.Exp, scale=-1.0 / 16.0, r=[cum], w=[dend])
                    psT = p.psum()
                    p.transposes([(psT[0:64, c * 64:(c + 1) * 64], ke[:, c * 64:(c + 1) * 64]) for c in range(NCH)], cst.ident[:, :],
                                 r=[ke, cst.ident], w=[psT])
                    keT = G['keT']
                    p.copy(keT[:, :], psT[0:64, :], r=[psT], w=[keT], eng='act')
                    psS = p.psum()
                    p.mms([(psS[0:64, c * 64:(c + 1) * 64], [(ki[:, c * 64:(c + 1) * 64], qd[:, c * 64:(c + 1) * 64])]) for c in range(NCH)],
                          r=[ki, qd], w=[psS])
                    sc = G['sc']
                    p.tt(sc[:, :], psS[0:64, :], cst.mge[:, :, :].rearrange("p c t -> p (c t)"), ALU.mult, r=[psS, cst.mge], w=[sc])
                    dst = G['dst']
                    for half in range(2):
                        psD = p.psum()
                        p.mms([(psD[0:64, cc * 128:(cc + 1) * 128], [(keT[:, (half * 4 + cc) * 64:(half * 4 + cc + 1) * 64], v[:, half * 4 + cc, :])])
                               for cc in range(4)], r=[keT, v], w=[psD])
                        p.copy(dst[:, half * 4:half * 4 + 4, :].rearrange("p c e -> p (c e)"), psD[0:64, :], r=[psD], w=[dst],
                               eng=['act', 'dve'][half])
                    for c in range(NCH):
                        p.stt(S[h][:, c + 1, :], S[h][:, c, :], dend[:, c, :], dst[:, c, :], ALU.mult, ALU.add, r=[S[h], dend, dst], w=[S[h]])
                    psO = p.psum()
                    p.mms([(psO[:, c * 64:(c + 1) * 64], [(v[:, c, :], sc[:, c * 64:(c + 1) * 64]), (S[h][:, c, :], qd[:, c * 64:(c + 1) * 64])])
                           for c in range(NCH)], r=[v, sc, S[h], qd], w=[psO])
                    p.copy(S[h][:, 0, :], S[h][:, NCH, :], r=[S[h]], w=[S[h]], eng='pool')
                    y = fm_norm(nc, p, st, cst, psO, 128, 1e-5, c128[:, C128['gla_ng'] + h:C128['gla_ng'] + h + 1],
                                c128[:, C128['gla_nb'] + h:C128['gla_nb'] + h + 1], 'gn', tiles=(G['no'], G['nosq'], G['nmean'], G['nmsq']))
                    p.act(r[:, :], r[:, :], AF.Silu, r=[r], w=[r])
                    og = G['og']
                    p.tt(og[:, :], y[:, :], r[:, :], ALU.mult, r=[y, r], w=[og])
                    p.dma('sp', SC['ogT'][h * 128:(h + 1) * 128, tsl], og[:, :], r=[og], w=[('ogT', h, tt)])


def phase_lru(nc, p, IN, SC, l, cst, c128, c64, st0):
    p.ps_pool = [5, 6, 7]
    if True:
        wr = p.sb(st0, 'wr', [128, 4, 128])
        wi = p.sb(st0, 'wi', [128, 4, 128])
        for wt, nm in ((wr, 'lru_w_r'), (wi, 'lru_w_i')):
            p.op('pool', lambda wt=wt: nc.gpsimd.memset(wt[:, :, :], 0.0), w=[wt])
            for g in range(8):
                o = (g % 2) * 64
                p.dma('sp', wt[o:o + 64, g // 2, o:o + 64], IN[nm][l, g], r=[wt], w=[wt])
        cex = p.sb(st0, 'cex', [128, 4])
        lam = c128[:, C128['lam']:C128['lam'] + 4]
        p.act(cex[:, :], lam, AF.Exp, scale=-1.0, r=[c128], w=[cex])
        p.act(cex[:, :], cex[:, :], AF.Ln, bias=1.0, r=[cex], w=[cex])
        p.ts(cex[:, :], cex[:, :], -8.0, None, ALU.mult, r=[cex], w=[cex])
        hprev = p.sb(st0, 'hprev', [128, 4])
        p.op('pool', lambda: nc.gpsimd.memset(hprev[:, :], 0.0), w=[hprev])
        LT = {}
        for nm, shp, dt in [('xh', [128, TT + 3], F32), ('gate', [128, TT], F32), ('xc', [128, TT], F32), ('a', [128, TT], F32),
                            ('ii', [128, TT], F32), ('ml', [128, TT], F32), ('hh', [128, TT], F32), ('ol', [128, TT], BF16)]:
            LT[nm] = p.sb(st0, 'l_' + nm, shp, dt)
        for tt in range(NT):
            t0 = tt * TT
            tsl = slice(t0, t0 + TT)
            for g in range(4):
                if True:
                    xh = LT['xh']
                    rows = slice(LX0 + g * 128, LX0 + (g + 1) * 128)
                    if tt == 0:
                        p.op('pool', lambda: nc.gpsimd.memset(xh[:, 0:3], 0.0), w=[xh])
                        p.dma('sp', xh[:, 3:], SC['hT'][rows, tsl], r=hk('hT', LX0 + g * 128, 128, tt), w=[xh])
                    else:
                        p.dma('sp', xh[:, :], SC['hT'][rows, t0 - 3:t0 + TT], r=hk('hT', LX0 + g * 128, 128, tt, True), w=[xh])
                    gate = LT['gate']
                    p.dma('sp', gate[:, :], SC['hT'][LG0 + g * 128:LG0 + (g + 1) * 128, tsl], r=hk('hT', LG0 + g * 128, 128, tt), w=[gate])
                    xc = LT['xc']
                    cw = C128['conv_w']
                    p.ts(xc[:, :], xh[:, 0:TT], c128[:, cw + g:cw + g + 1], c128[:, C128['conv_b'] + g:C128['conv_b'] + g + 1],
                         ALU.mult, ALU.add, r=[xh, c128], w=[xc])
                    for j in range(1, 4):
                        p.stt(xc[:, :], xh[:, j:j + TT], c128[:, cw + j * 4 + g:cw + j * 4 + g + 1], xc[:, :], ALU.mult, ALU.add,
                              r=[xh, xc, c128], w=[xc])
                    psr = p.psum()
                    p.mm(psr[:, :], [(wr[:, g, :], xc[:, :])], r=[wr, xc], w=[psr])
                    psi = p.psum()
                    p.mm(psi[:, :], [(wi[:, g, :], xc[:, :])], r=[wi, xc], w=[psi])
                    a = LT['a']
                    ii = LT['ii']
                    ml = LT['ml']
                    p.act(a[:, :], psr[:, :], AF.Sigmoid, bias=c128[:, C128['b_r'] + g:C128['b_r'] + g + 1], r=[psr, c128], w=[a])
                    p.act(ii[:, :], psi[:, :], AF.Sigmoid, bias=c128[:, C128['b_i'] + g:C128['b_i'] + g + 1], r=[psi, c128], w=[ii])
                    p.act(a[:, :], a[:, :], AF.Exp, scale=cex[:, g:g + 1], r=[a, cex], w=[a])
                    p.act(ml[:, :], a[:, :], AF.Square, r=[a], w=[ml])
                    p.act(ml[:, :], ml[:, :], AF.Sqrt, bias=1.0, scale=-1.0, r=[ml], w=[ml])
                    p.tt(ii[:, :], ii[:, :], xc[:, :], ALU.mult, r=[ii, xc], w=[ii])
                    p.tt(ii[:, :], ii[:, :], ml[:, :], ALU.mult, r=[ii, ml], w=[ii])
                    hh = LT['hh']
                    p.op('dve', lambda g=g: nc.vector.tensor_tensor_scan(out=hh[:, :], data0=a[:, :], data1=ii[:, :], initial=hprev[:, g:g + 1],
                                                                     op0=ALU.mult, op1=ALU.add), r=[a, ii, hprev], w=[hh])
                    p.copy(hprev[:, g:g + 1], hh[:, TT - 1:TT], r=[hh], w=[hprev], eng='dve')
                    p.act(ml[:, :], gate[:, :], AF.Square, r=[gate], w=[ml])
                    p.ts(ml[:, :], ml[:, :], 0.044715, 1.0, ALU.mult, ALU.add, r=[ml], w=[ml])
                    p.tt(ml[:, :], ml[:, :], gate[:, :], ALU.mult, r=[ml, gate], w=[ml])
                    p.act(ml[:, :], ml[:, :], AF.Sigmoid, scale=1.5957691216057308, r=[ml], w=[ml])
                    p.tt(ml[:, :], ml[:, :], gate[:, :], ALU.mult, r=[ml, gate], w=[ml])
                    ol = LT['ol']
                    p.tt(ol[:, :], ml[:, :], hh[:, :], ALU.mult, r=[ml, hh], w=[ol])
                    p.dma('sp', SC['olT'][g * 128:(g + 1) * 128, tsl], ol[:, :], r=[ol], w=[('olT', g, tt)])


def phase_rwkv(nc, p, IN, SC, l, cst, c128, c64):
    YB = 7
    p.ps_pool = [0, 1, 2, 3, 4, 5, 6]
    with p.scope() as st0:
        w2 = p.sb(st0, 'w2', [64, 512])
        a2 = p.sb(st0, 'a2', [64, 512])
        g2a = p.sb(st0, 'g2a', [128, 512])
        g2b = p.sb(st0, 'g2b', [32, 512])
        p.dma('sp', w2[:, :], IN['rwkv_w2'][l], w=[w2])
        p.dma('sp', a2[:, :], IN['rwkv_a2'][l], w=[a2])
        p.dma('sp', g2a[:, :], IN['rwkv_g2'][l, 0:128, :], w=[g2a])
        p.dma('sp', g2b[:, :], IN['rwkv_g2'][l, 128:160, :], w=[g2b])
        nmu = C64['mu_a'] + 1 - C64['mu_r']
        omu = p.sb(st0, 'omu', [64, N64])
        p.ts(omu[:, :], c64[:, :], -1.0, 1.0, ALU.mult, ALU.add, r=[c64], w=[omu])
        omug = p.sb(st0, 'omug', [128, 2])
        p.ts(omug[:, :], c128[:, C128['mu_g']:C128['mu_g'] + 2], -1.0, 1.0, ALU.mult, ALU.add, r=[c128], w=[omug])
        Tst = [p.sb(st0, 'Tst%d' % h, [64, 64]) for h in range(8)]
        for h in range(8):
            p.op('pool', lambda: nc.gpsimd.memset(Tst[h][:, :], 0.0), w=[Tst[h]])
        psY = p.ps[YB]

        def load_shift(st, tt, row0, nrows, mu_ap, omu_ap, rd, tag):
            t0 = tt * TT
            raw = p.sb(st, tag + 'raw', [nrows, TT + 1])
            if tt == 0:
                p.op('pool', lambda: nc.gpsimd.memset(raw[:, 0:1], 0.0), w=[raw])
                p.dma('sp', raw[:, 1:], SC['hT'][row0:row0 + nrows, 0:TT], r=hk('hT', row0, nrows, tt), w=[raw])
            else:
                p.dma('sp', raw[:, :], SC['hT'][row0:row0 + nrows, t0 - 1:t0 + TT], r=hk('hT', row0, nrows, tt, True), w=[raw])
            out = p.sb(st, tag, [nrows, TT])
            p.ts(out[:, :], raw[:, 1:TT + 1], omu_ap, None, ALU.mult, r=[raw] + rd, w=[out])
            p.stt(out[:, :], raw[:, 0:TT], mu_ap, out[:, :], ALU.mult, ALU.add, r=[raw, out] + rd, w=[out])
            return out

        def col(name, h=0):
            return c64[:, C64[name] + h:C64[name] + h + 1]

        def ocol(name, h=0):
            return omu[:, C64[name] + h:C64[name] + h + 1]

        for tt in range(NT):
            tsl = slice(tt * TT, (tt + 1) * TT)
            with p.scope() as stt_:
                wl = load_shift(stt_, tt, RWW, 64, col('mu_w'), ocol('mu_w'), [c64, omu], 'wl')
                al = load_shift(stt_, tt, RWA, 64, col('mu_a'), ocol('mu_a'), [c64, omu], 'al')
                mg = C128['mu_g']
                gl1 = load_shift(stt_, tt, RWG, 128, c128[:, mg:mg + 1], omug[:, 0:1], [c128, omug], 'gl1')
                gl2 = load_shift(stt_, tt, RWG + 128, 32, c128[0:32, mg + 1:mg + 2], omug[0:32, 1:2], [c128, omug], 'gl2')
                p.act(wl[:, :], wl[:, :], AF.Tanh, r=[wl], w=[wl])
                p.act(gl1[:, :], gl1[:, :], AF.Sigmoid, r=[gl1], w=[gl1])
                p.act(gl2[:, :], gl2[:, :], AF.Sigmoid, r=[gl2], w=[gl2])
                for h in range(8):
                    hs = slice(h * 64, (h + 1) * 64)
                    with p.scope() as st:
                        r = load_shift(st, tt, RWR + h * 64, 64, col('mu_r', h), ocol('mu_r', h), [c64, omu], 'r')
                        k = load_shift(st, tt, RWK + h * 64, 64, col('mu_k', h), ocol('mu_k', h), [c64, omu], 'k')
                        v = load_shift(st, tt, RWV + h * 64, 64, col('mu_v', h), ocol('mu_v', h), [c64, omu], 'v')

                        def new(tag, shape=None, dt=F32):
                            return p.sb(st, tag, shape or [64, TT], dt)
                        ps = p.psum()
                        p.mm(ps[0:64, :], [(w2[:, hs], wl[:, :])], r=[w2, wl], w=[ps])
                        sgm = new('sgm')
                        p.act(sgm[:, :], ps[0:64, :], AF.Sigmoid, bias=col('w0', h), r=[ps, c64], w=[sgm])
                        cum = new('cum')
                        p.op('dve', lambda: nc.vector.tensor_tensor_scan(out=cum[:, :], data0=cst.m01[:, :], data1=sgm[:, :], initial=0.0,
                                                                         op0=ALU.mult, op1=ALU.add), r=[cst.m01, sgm], w=[cum])
                        ps = p.psum()
                        p.mm(ps[0:64, :], [(a2[:, hs], al[:, :])], r=[a2, al], w=[ps])
                        ag = new('ag')
                        p.act(ag[:, :], ps[0:64, :], AF.Sigmoid, bias=col('a0', h), r=[ps, c64], w=[ag])
                        ps = p.psum()
                        p.mm(ps[0:64, :], [(g2a[:, hs], gl1[:, :]), (g2b[:, hs], gl2[:, :])], r=[g2a, g2b, gl1, gl2], w=[ps])
                        gh = new('gh')
                        p.copy(gh[:, :], ps[0:64, :], r=[ps], w=[gh], eng='act')
                        kk = new('kk')
                        tmp = new('tmp')
                        p.ts(kk[:, :], k[:, :], col('k_k', h), None, ALU.mult, r=[k, c64], w=[kk])
                        p.act(tmp[:, :], kk[:, :], AF.Square, r=[kk], w=[tmp])
                        ps = p.psum()
                        p.mm(ps[0:64, :], [(cst.ones64[:, :], tmp[:, :])], r=[cst.ones64, tmp], w=[ps])
                        p.act(tmp[:, :], ps[0:64, :], AF.Sqrt, r=[ps], w=[tmp])
                        p.ts(tmp[:, :], tmp[:, :], 1e-12, None, ALU.max, r=[tmp], w=[tmp])
                        p.op('dve', lambda: nc.vector.reciprocal(out=tmp[:, :], in_=tmp[:, :]), r=[tmp], w=[tmp])
                        p.tt(kk[:, :], kk[:, :], tmp[:, :], ALU.mult, r=[kk, tmp], w=[kk])
                        p.ts(tmp[:, :], ag[:, :], col('k_a', h), ocol('k_a', h), ALU.mult, ALU.add, r=[ag, c64, omu], w=[tmp])
                        k2 = new('k2')
                        p.tt(k2[:, :], k[:, :], tmp[:, :], ALU.mult, r=[k, tmp], w=[k2])
                        bv = new('bv')
                        p.tt(bv[:, :], kk[:, :], ag[:, :], ALU.mult, r=[kk, ag], w=[bv])
                        E = new('E')
                        rt, kt, bt, at, kh, bh = new('rt'), new('kt'), new('bt'), new('at'), new('kh'), new('bh')
                        p.act(E[:, :], cum[:, :], AF.Exp, scale=-C0, r=[cum], w=[E])
                        p.tt(rt[:, :], r[:, :], E[:, :], ALU.mult, r=[r, E], w=[rt])
                        p.act(E[:, :], cum[:, :], AF.Exp, scale=C0, r=[cum], w=[E])
                        p.tt(kt[:, :], k2[:, :], E[:, :], ALU.mult, r=[k2, E], w=[kt])
                        p.tt(bt[:, :], bv[:, :], E[:, :], ALU.mult, r=[bv, E], w=[bt])
                        p.tt(tmp[:, :], cum[:, :], sgm[:, :], ALU.subtract, r=[cum, sgm], w=[tmp])
                        p.act(E[:, :], tmp[:, :], AF.Exp, scale=-C0, r=[tmp], w=[E])
                        p.stt(at[:, :], kk[:, :], -1.0, E[:, :], ALU.mult, ALU.mult, r=[kk, E], w=[at])
                        c3 = cview(cum)
                        D = new('D', [64, NCH, CH])
                        p.tt(D[:, :, :], c3[:, :, CH - 1:CH].to_broadcast([64, NCH, CH]), c3, ALU.subtract, r=[cum], w=[D])
                        p.act(D[:, :, :], D[:, :, :], AF.Exp, scale=-C0, r=[D], w=[D])
                        Df = D[:, :, :].rearrange("p c t -> p (c t)")
                        p.tt(kh[:, :], k2[:, :], Df, ALU.mult, r=[k2, D], w=[kh])
                        p.tt(bh[:, :], bv[:, :], Df, ALU.mult, r=[bv, D], w=[bh])
                        WC = new('WC', [64, NCH, 1])
                        p.act(WC[:, :, :], c3[:, :, CH - 1:CH], AF.Exp, scale=-C0, r=[cum], w=[WC])
                        p.stt(tmp[:, :], r[:, :], col('r_k', h), k2[:, :], ALU.mult, ALU.mult, r=[r, k2, c64], w=[tmp])
                        ps = p.psum()
                        p.mm(ps[0:64, :], [(cst.ones64[:, :], tmp[:, :])], r=[cst.ones64, tmp], w=[ps])
                        bonus = new('bonus')
                        p.tt(bonus[:, :], ps[0:64, :], v[:, :], ALU.mult, r=[ps, v], w=[bonus])
                        toks = []
                        for src, tag in ((v, 'vT'), (kh, 'khT'), (bh, 'bhT')):
                            psT = p.psum()
                            p.transposes([(psT[0:64, c * 64:(c + 1) * 64], src[:, c * 64:(c + 1) * 64]) for c in range(NCH)], cst.ident[:, :],
                                         r=[src, cst.ident], w=[psT])
                            d = new(tag)
                            p.copy(d[:, :], psT[0:64, :], r=[psT], w=[d], eng='act')
                            toks.append(d)
                        vT, khT, bhT = toks

                        def amat(lt, rh, mask, tag):
                            psA = p.psum()
                            p.mms([(psA[0:64, c * 64:(c + 1) * 64], [(lt[:, c * 64:(c + 1) * 64], rh[:, c * 64:(c + 1) * 64])]) for c in range(NCH)],
                                  r=[lt, rh], w=[psA])
                            d = new(tag)
                            p.tt(d[:, :], psA[0:64, :], mask[:, :, :].rearrange("p c t -> p (c t)"), ALU.mult, r=[psA, mask], w=[d])
                            return d
                        AabT = amat(bt, at, cst.mgt, 'AabT')
                        ArbT = amat(bt, rt, cst.mge, 'ArbT')
                        AakT = amat(kt, at, cst.mgt, 'AakT')
                        ArkT = amat(kt, rt, cst.mge, 'ArkT')
                        Aab = amat(at, bt, cst.mlt, 'Aab')
                        P_, Q_ = Aab, AabT
                        N_ = new('N0')
                        p.tt(N_[:, :], Q_[:, :], cst.id8[:, :, :].rearrange("p c t -> p (c t)"), ALU.add, r=[Q_, cst.id8], w=[N_])
                        for lev in range(5):
                            psP = p.psum()
                            p.mms([(psP[0:64, c * 64:(c + 1) * 64], [(Q_[:, c * 64:(c + 1) * 64], P_[:, c * 64:(c + 1) * 64])]) for c in range(NCH)],
                                  r=[P_, Q_], w=[psP])
                            if lev < 4:
                                psQ = p.psum()
                                p.mms([(psQ[0:64, c * 64:(c + 1) * 64], [(P_[:, c * 64:(c + 1) * 64], Q_[:, c * 64:(c + 1) * 64])]) for c in range(NCH)],
                                      r=[P_, Q_], w=[psQ])
                            P2 = new('P%d' % lev)
                            p.copy(P2[:, :], psP[0:64, :], r=[psP], w=[P2], eng='act')
                            if lev < 4:
                                Q2 = new('Q%d' % lev)
                                p.copy(Q2[:, :], psQ[0:64, :], r=[psQ], w=[Q2], eng='dve')
                            psN = p.psum()
                            p.mms([(psN[0:64, c * 64:(c + 1) * 64], [(P2[:, c * 64:(c + 1) * 64], N_[:, c * 64:(c + 1) * 64])]) for c in range(NCH)],
                                  r=[P2, N_], w=[psN])
                            N2 = new('N%d' % (lev + 1))
                            p.tt(N2[:, :], N_[:, :], psN[0:64, :], ALU.add, r=[N_, psN], w=[N2])
                            P_, N_ = P2, N2
                            if lev < 4:
                                Q_ = Q2
                        X = new('X', [64, 64])
                        U = new('U', [64, 64])
                        for c in range(NCH):
                            cs = slice(c * 64, (c + 1) * 64)
                            psX = p.psum()
                            p.mm(psX[0:64, 0:64], [(at[:, cs], Tst[h][:, :]), (AakT[:, cs], vT[:, cs])], r=[at, Tst[h], AakT, vT], w=[psX])
                            p.copy(X[:, :], psX[0:64, 0:64], r=[psX], w=[X], eng='act')
                            psU = p.psum()
                            p.mm(psU[0:64, 0:64], [(N_[:, cs], X[:, :])], r=[N_, X], w=[psU])
                            p.copy(U[:, :], psU[0:64, 0:64], r=[psU], w=[U], eng='dve')
                            p.mm(psY[0:64, cs], [(Tst[h][:, :], rt[:, cs]), (U[:, :], ArbT[:, cs]), (vT[:, cs], ArkT[:, cs])],
                                 r=[Tst[h], rt, U, ArbT, vT, ArkT], w=[psY])
                            psT2 = p.psum()
                            p.mm(psT2[0:64, 0:64], [(bhT[:, cs], U[:, :]), (khT[:, cs], vT[:, cs])], r=[bhT, U, khT, vT], w=[psT2])
                            p.stt(Tst[h][:, :], Tst[h][:, :], WC[:, c, :], psT2[0:64, 0:64], ALU.mult, ALU.add, r=[Tst[h], WC, psT2], w=[Tst[h]])
                        y = fm_norm(nc, p, st, cst, psY, 64, 64e-5, col('lnx_g', h), col('lnx_b', h), 'rn')
                        p.tt(y[:, :], y[:, :], bonus[:, :], ALU.add, r=[y, bonus], w=[y])
                        orw = new('orw', [64, TT], BF16)
                        p.tt(orw[:, :], y[:, :], gh[:, :], ALU.mult, r=[y, gh], w=[orw])
                        p.dma('sp', SC['orT'][h * 64:(h + 1) * 64, tsl], orw[:, :], r=[orw], w=[('orT', h, tt)])
    p.ps_pool = list(range(8))


def mixers(nc, p, IN, SC, l, stop_after, cstream=None):
    with p.scope() as st:
        cst = make_consts(nc, p, st)
        c128 = p.sb(st, 'c128m', [128, N128])
        c64 = p.sb(st, 'c64m', [64, N64])
        p.dma('sp', c128[:, :], IN['c128'][l], w=[c128])
        p.dma('sp', c64[:, :], IN['c64'][l], w=[c64])
        with p.scope() as stg:
            Lg = p.record(lambda: phase_gla(nc, p, IN, SC, l, cst, c128, c64, stg))
            Ll = p.record(lambda: phase_lru(nc, p, IN, SC, l, cst, c128, c64, stg))
            p.ps_pool = list(range(8))
            p.play([Lg, Ll])
        if stop_after == ('lru', l):
            return True
        phase_rwkv3(nc, p, IN, SC, l, cst, c128, c64, cstream)
        if stop_after == ('rwkv', l):
            return True
    return False


def ln_fm(nc, p, st, onesD, z, out, c128, gname, bname, tag):
    zsq = p.sb(st, tag + 'zsq', [128, 8, TT])
    p.act(zsq[:, :, :], z[:, :, :], AF.Square, r=[z], w=[zsq])
    psM = p.psum()
    p.mm(psM[:, :], [(onesD[:, :], z[:, kc, :]) for kc in range(8)], r=[onesD, z], w=[psM])
    psQ = p.psum()
    p.mm(psQ[:, :], [(onesD[:, :], zsq[:, kc, :]) for kc in range(8)], r=[onesD, zsq], w=[psQ])
    mean = p.sb(st, tag + 'mean', [128, TT])
    rstd = p.sb(st, tag + 'rstd', [128, TT])
    tmp = p.sb(st, tag + 'tmp', [128, TT])
    p.copy(mean[:, :], psM[:, :], r=[psM], w=[mean], eng='act')
    p.act(tmp[:, :], psM[:, :], AF.Square, r=[psM], w=[tmp])
    p.tt(rstd[:, :], psQ[:, :], tmp[:, :], ALU.subtract, r=[psQ, tmp], w=[rstd])
    p.ts(rstd[:, :], rstd[:, :], 0.0, 1e-5, ALU.max, ALU.add, r=[rstd], w=[rstd])
    p.act(tmp[:, :], rstd[:, :], AF.Sqrt, r=[rstd], w=[tmp])
    p.op('dve', lambda: nc.vector.reciprocal(out=rstd[:, :], in_=tmp[:, :]), r=[tmp], w=[rstd])
    tks = [p.sb(st, tag + 'tk', [128, TT]) for _ in range(2)]
    for kc in range(8):
        eng = 'dve' if kc % 2 == 0 else 'pool'
        tk = tks[kc % 2]
        p.tt(tk[:, :], z[:, kc, :], mean[:, :], ALU.subtract, r=[z, mean], w=[tk], eng=eng)
        p.tt(tk[:, :], tk[:, :], rstd[:, :], ALU.mult, r=[tk, rstd], w=[tk], eng=eng)
        p.ts(out[:, kc, :], tk[:, :], c128[:, C128[gname] + kc:C128[gname] + kc + 1], c128[:, C128[bname] + kc:C128[bname] + kc + 1],
             ALU.mult, ALU.add, r=[tk, c128], w=[(out.key, kc)], eng='dve')
    p._mark('dve', p.cnt['dve'], [], [out.key])


def swiglu_bufs(p, st, F, nwd=1):
    nfc = F // 128
    return dict(hmid=p.sb(st, 'hmid', [128, nfc, TT], BF16),
                wgs=[p.sb(st, 'wgs', [128, 8, 512], BF16) for _ in range(2)],
                wus=[p.sb(st, 'wus', [128, 8, 512], BF16) for _ in range(2)],
                sgt=[p.sb(st, 'sgt', [128, TT]) for _ in range(2)],
                wd=[p.sb(st, 'wd', [128, nfc, 512], BF16) for _ in range(nwd)], cnt=[0, 0])


def swiglu_fm(nc, p, st, xb, wg_d, wu_d, wd_d, F, sink, bufs=None):
    nfc = F // 128
    if bufs is None:
        bufs = swiglu_bufs(p, st, F)
    hmid, wgs, wus, sgt = bufs['hmid'], bufs['wgs'], bufs['wus'], bufs['sgt']
    wg3 = wg_d.rearrange("(kc p) f -> p kc f", p=128)
    wu3 = wu_d.rearrange("(kc p) f -> p kc f", p=128)
    for f0 in range(0, F, 512):
        fw = min(512, F - f0)
        gi = bufs['cnt'][0]
        bufs['cnt'][0] += 1
        wg, wu = wgs[gi % 2], wus[gi % 2]
        p.dma('sp', wg[:, :, 0:fw], wg3[:, :, f0:f0 + fw], w=[wg])
        p.dma('sp', wu[:, :, 0:fw], wu3[:, :, f0:f0 + fw], w=[wu])
        for j in range(fw // 128):
            fc = f0 // 128 + j
            psG = p.psum()
            p.mm(psG[:, :], [(wg[:, kc, j * 128:(j + 1) * 128], xb[:, kc, :]) for kc in range(8)], r=[wg, xb], w=[psG])
            psU = p.psum()
            p.mm(psU[:, :], [(wu[:, kc, j * 128:(j + 1) * 128], xb[:, kc, :]) for kc in range(8)], r=[wu, xb], w=[psU])
            sg = sgt[fc % 2]
            p.act(sg[:, :], psG[:, :], AF.Silu, r=[psG], w=[sg])
            p.tt(hmid[:, fc, :], sg[:, :], psU[:, :], ALU.mult, r=[sg, psU], w=[hmid])
    wd3 = wd_d.rearrange("(fc p) m -> p fc m", p=128)
    for half in range(2):
        wi = bufs['cnt'][1]
        bufs['cnt'][1] += 1
        wd = bufs['wd'][wi % len(bufs['wd'])]
        for f0 in range(0, nfc, 7):
            f1 = min(nfc, f0 + 7)
            p.dma('sp', wd[:, f0:f1, :], wd3[:, f0:f1, half * 512:(half + 1) * 512], w=[(wd.key, f0)])
        for mm_ in range(4):
            mo = half * 4 + mm_
            ps = p.psum()
            p.mm(ps[:, :], [(wd[:, fc, mm_ * 128:(mm_ + 1) * 128], hmid[:, fc, :]) for fc in range(nfc)],
                 r=[(wd.key, f0) for f0 in range(0, nfc, 7)] + [hmid], w=[ps])
            sink(mo, ps)


def phase_tail(nc, p, IN, SC, l, xin, outd, outkey):
    with p.scope() as st0:
        c128 = p.sb(st0, 'c128t', [128, N128])
        p.dma('sp', c128[:, :], IN['c128'][l], w=[c128])
        onesD = p.sb(st0, 'onesD', [128, 128])
        p.op('pool', lambda: nc.gpsimd.memset(onesD[:, :], 1.0 / 1024.0), w=[onesD])
        if l == 1:
            wrt = p.sb(st0, 'wrt', [128, 8, NE])
            p.dma('sp', wrt[:, :, :], IN['moe_w_router'].rearrange("(kc p) e -> p kc e", p=128), w=[wrt])
            id128 = p.sb(st0, 'id128', [128, 128])
            ones_ = p.sb(st0, 'ones_', [128, 128])
            p.op('pool', lambda: nc.gpsimd.memset(ones_[:, :], 1.0), w=[ones_])
            p.op('pool', lambda: nc.gpsimd.affine_select(out=id128[:, :], in_=ones_[:, :], pattern=[[1, 128]], compare_op=ALU.is_equal,
                                                         fill=0.0, base=0, channel_multiplier=-1), r=[ones_], w=[id128])
            sel = p.sb(st0, 'sel', [8, NE, 128])
            ones3 = p.sb(st0, 'ones3', [8, NE, 128])
            p.op('pool', lambda: nc.gpsimd.memset(ones3[:, :, :], 1.0), w=[ones3])
            p.op('pool', lambda: nc.gpsimd.affine_select(out=sel[:, :, :], in_=ones3[:, :, :], pattern=[[-1, NE], [0, 128]],
                                                         compare_op=ALU.is_equal, fill=0.0, base=0, channel_multiplier=1), r=[ones3], w=[sel])
        xin3 = xin.rearrange("(kc p) t -> p kc t", p=128)
        out3 = outd.rearrange("(kc p) t -> p kc t", p=128)
        wbp = [SC['wb_pg'][l], SC['wb_pl'][l], SC['wb_pr'][l]]
        obd = [SC['ogT'], SC['olT'], SC['orT']]
        for tt in range(NT):
            tsl = slice(tt * TT, (tt + 1) * TT)
            with p.scope() as stx:
                x1 = p.sb(stx, 'x1', [128, 8, TT])
                x1b = p.sb(stx, 'x1b', [128, 8, TT], BF16)
                with p.scope() as st:
                    xf = p.sb(st, 'xf', [128, 8, TT])
                    xb = p.sb(st, 'xb', [128, 8, TT], BF16)
                    rk = [('x2T', c, tt) for c in range(8)] if l > 0 else []
                    p.dma('sp', xf[:, :, :], xin3[:, :, tsl], r=rk, w=[xf])
                    p.copy(xb[:, :, :], xf[:, :, :], r=[xf], w=[xb], eng='act')
                    acc = p.sb(st, 'acc', [128, 8, TT])
                    mb = p.sb(st, 'mb', [128, 8, TT], BF16)
                    wgh = [p.sb(st, 'wgh', [128, 8, 512], BF16) for _ in range(2)]
                    wph = [p.sb(st, 'wph', [128, 4, 512], BF16) for _ in range(2)]
                    obs = [p.sb(st, 'ob', [128, 4, TT], BF16) for _ in range(2)]
                    sig = [p.sb(st, 'sig', [128, TT]) for _ in range(2)]
                    for b in range(3):
                        for hf in range(2):
                            g0 = NMIX + b * 1024 + hf * 512
                            p.dma('sp', wgh[hf][:, :, :], SC['wb_in'][l].rearrange("(kc p) n -> p kc n", p=128)[:, :, g0:g0 + 512], w=[wgh[hf]])
                            p.dma('sp', wph[hf][:, :, :], wbp[b].rearrange("(fc p) m -> p fc m", p=128)[:, :, hf * 512:(hf + 1) * 512], w=[wph[hf]])
                        ob = obs[b % 2]
                        nrow = 128 if b < 2 else 64
                        okeys = [(['ogT', 'olT', 'orT'][b], i, tt) for i in range(512 // nrow)]
                        p.dma('sp', ob[:, :, :], obd[b].rearrange("(fc p) t -> p fc t", p=128)[:, :, tsl], r=okeys, w=[ob])
                        for mc in range(8):
                            ps1 = p.psum()
                            wgt, wpt, mq = wgh[mc // 4], wph[mc // 4], mc % 4
                            p.mm(ps1[:, :], [(wgt[:, kc, mq * 128:(mq + 1) * 128], xb[:, kc, :]) for kc in range(8)], r=[wgt, xb], w=[ps1])
                            ps2 = p.psum()
                            p.mm(ps2[:, :], [(wpt[:, fc, mq * 128:(mq + 1) * 128], ob[:, fc, :]) for fc in range(4)], r=[wpt, ob], w=[ps2])
                            sg = sig[mc % 2]
                            col = C128['b_gate'] + b * 8 + mc
                            p.act(sg[:, :], ps1[:, :], AF.Sigmoid, bias=c128[:, col:col + 1], r=[ps1, c128], w=[sg])
                            ak = (acc.key, mc)
                            if b == 0:
                                p.tt(acc[:, mc, :], sg[:, :], ps2[:, :], ALU.mult, r=[sg, ps2], w=[ak])
                            else:
                                p.tt(sg[:, :], sg[:, :], ps2[:, :], ALU.mult, r=[sg, ps2], w=[sg])
                                if b == 1:
                                    p.tt(acc[:, mc, :], acc[:, mc, :], sg[:, :], ALU.add, r=[ak, sg], w=[ak], eng='pool')
                                else:
                                    p.tt(mb[:, mc, :], acc[:, mc, :], sg[:, :], ALU.add, r=[ak, sg], w=[(mb.key, mc)], eng='pool')
                    p._mark('pool', p.cnt['pool'], [], [mb.key])
                    for hf in range(2):
                        p.dma('sp', wgh[hf][:, :, :], SC['wb_out'][l].rearrange("(kc p) n -> p kc n", p=128)[:, :, hf * 512:(hf + 1) * 512], w=[wgh[hf]])
                    z = acc
                    for mo in range(8):
                        wo, mq = wgh[mo // 4], mo % 4
                        ps = p.psum()
                        p.mm(ps[:, :], [(wo[:, kc, mq * 128:(mq + 1) * 128], mb[:, kc, :]) for kc in range(8)], r=[wo, mb], w=[ps])
                        p.stt(z[:, mo, :], xf[:, mo, :], ALPHA, ps[:, :], ALU.mult, ALU.add, r=[xf, ps, (acc.key, mo)], w=[(acc.key, mo)])
                    p._mark('dve', p.cnt['dve'], [], [z.key])
                    ln_fm(nc, p, st, onesD, z, x1, c128, 'ln_mix_g', 'ln_mix_b', 'l1')
                    p.copy(x1b[:, :, :], x1[:, :, :], r=[x1], w=[x1b], eng='act')
                with p.scope() as st:
                    z2 = p.sb(st, 'z2', [128, 8, TT])
                    if l == 0:
                        def sink(mo, ps):
                            p.stt(z2[:, mo, :], x1[:, mo, :], ALPHA, ps[:, :], ALU.mult, ALU.add, r=[x1, ps], w=[(z2.key, mo)])
                        swiglu_fm(nc, p, st, x1b, SC['wb_fg'], SC['wb_fu'], SC['wb_fd'], FD, sink, bufs=swiglu_bufs(p, st, FD, nwd=2))
                        p._mark('dve', p.cnt['dve'], [], [z2.key])
                    else:
                        wts = p.sb(st, 'wts', [128, 4, NE])
                        for tb in range(4):
                            psl = p.psum()
                            p.mm(psl[:, 0:NE], [(x1[:, kc, tb * 128:(tb + 1) * 128], wrt[:, kc, :]) for kc in range(8)], r=[x1, wrt], w=[psl])
                            lg = p.sb(st, 'lg', [128, NE])
                            p.copy(lg[:, :], psl[:, 0:NE], r=[psl], w=[lg], eng='act')
                            m1 = p.sb(st, 'm1', [128, 1])
                            m2 = p.sb(st, 'm2', [128, 1])
                            t8 = p.sb(st, 't8', [128, NE])
                            p.op('dve', lambda: nc.vector.tensor_reduce(out=m1[:, :], in_=lg[:, :], axis=mybir.AxisListType.X, op=ALU.max), r=[lg], w=[m1])
                            p.ts(t8[:, :], lg[:, :], m1[:, 0:1], -1e30, ALU.is_equal, ALU.mult, r=[lg, m1], w=[t8])
                            p.tt(t8[:, :], t8[:, :], lg[:, :], ALU.add, r=[t8, lg], w=[t8])
                            p.op('dve', lambda: nc.vector.tensor_reduce(out=m2[:, :], in_=t8[:, :], axis=mybir.AxisListType.X, op=ALU.max), r=[t8], w=[m2])
                            p.ts(t8[:, :], lg[:, :], m2[:, 0:1], None, ALU.is_ge, r=[lg, m2], w=[t8])
                            p.ts(m1[:, :], m1[:, :], -1.0, None, ALU.mult, r=[m1], w=[m1])
                            p.act(lg[:, :], lg[:, :], AF.Exp, bias=m1[:, 0:1], r=[lg, m1], w=[lg])
                            p.tt(lg[:, :], lg[:, :], t8[:, :], ALU.mult, r=[lg, t8], w=[lg])
                            p.op('dve', lambda: nc.vector.tensor_reduce(out=m2[:, :], in_=lg[:, :], axis=mybir.AxisListType.X, op=ALU.add), r=[lg], w=[m2])
                            p.op('dve', lambda: nc.vector.reciprocal(out=m2[:, :], in_=m2[:, :]), r=[m2], w=[m2])
                            p.ts(wts[:, tb, :], lg[:, :], m2[:, 0:1], None, ALU.mult, r=[lg, m2], w=[wts])
                        psw = p.psum()
                        p.transposes([(psw[0:NE, tb * 128:(tb + 1) * 128], wts[:, tb, :]) for tb in range(4)], id128[:, :], r=[wts, id128], w=[psw])
                        wT = p.sb(st, 'wT', [NE, TT])
                        p.copy(wT[:, :], psw[0:NE, :], r=[psw], w=[wT], eng='act')
                        accm = z2
                        mb_ = swiglu_bufs(p, st, FE, nwd=2)
                        wbes = [p.sb(st, 'wbe', [128, TT]) for _ in range(2)]
                        tmpm = [p.sb(st, 'tmpm', [128, TT]) for _ in range(2)]
                        for e in range(NE):
                            psb = p.psum()
                            p.mm(psb[:, :], [(sel[:, e, :], wT[:, :])], r=[sel, wT], w=[psb])
                            wbe = wbes[e % 2]
                            p.copy(wbe[:, :], psb[:, :], r=[psb], w=[wbe], eng='act')

                            def sink(mo, ps, e=e, wbe=wbe, tmpm=tmpm):
                                ak = (accm.key, mo)
                                if e == 0:
                                    p.tt(accm[:, mo, :], ps[:, :], wbe[:, :], ALU.mult, r=[ps, wbe], w=[ak])
                                else:
                                    tm = tmpm[mo % 2]
                                    p.tt(tm[:, :], ps[:, :], wbe[:, :], ALU.mult, r=[ps, wbe], w=[tm])
                                    p.tt(accm[:, mo, :], accm[:, mo, :], tm[:, :], ALU.add, r=[ak, tm], w=[ak], eng='pool')
                            swiglu_fm(nc, p, st, x1b, SC['wb_mg'][e], SC['wb_mu'][e], SC['wb_md'][e], FE, sink, bufs=mb_)
                        p._mark('pool', p.cnt['pool'], [], [accm.key])
                        for mo in range(8):
                            p.stt(z2[:, mo, :], x1[:, mo, :], ALPHA, accm[:, mo, :], ALU.mult, ALU.add, r=[x1, accm], w=[(z2.key, mo)])
                        p._mark('dve', p.cnt['dve'], [], [z2.key])
                    x2 = x1
                    ln_fm(nc, p, st, onesD, z2, x2, c128, 'ln_ffn_g', 'ln_ffn_b', 'l2')
                    p.dma('sp', out3[:, :, tsl], x2[:, :, :], r=[x2], w=[(outkey, c, tt) for c in range(8)])


def phase_rwkv2(nc, p, IN, SC, l, cst, c128, c64, cstream=None):
    YB = 7
    POOL1 = [0, 1, 2, 3]
    POOL2 = [4, 5, 6]
    with p.scope() as st0:
        w2 = p.sb(st0, 'w2', [64, 512])
        a2 = p.sb(st0, 'a2', [64, 512])
        g2a = p.sb(st0, 'g2a', [128, 512])
        g2b = p.sb(st0, 'g2b', [32, 512])
        p.dma('sp', w2[:, :], IN['rwkv_w2'][l], w=[w2])
        p.dma('sp', a2[:, :], IN['rwkv_a2'][l], w=[a2])
        p.dma('sp', g2a[:, :], IN['rwkv_g2'][l, 0:128, :], w=[g2a])
        p.dma('sp', g2b[:, :], IN['rwkv_g2'][l, 128:160, :], w=[g2b])
        omu = p.sb(st0, 'omu', [64, N64])
        p.ts(omu[:, :], c64[:, :], -1.0, 1.0, ALU.mult, ALU.add, r=[c64], w=[omu])
        omug = p.sb(st0, 'omug', [128, 2])
        p.ts(omug[:, :], c128[:, C128['mu_g']:C128['mu_g'] + 2], -1.0, 1.0, ALU.mult, ALU.add, r=[c128], w=[omug])
        Tst = [p.sb(st0, 'Tst%d' % h, [64, 64]) for h in range(8)]
        for h in range(8):
            p.op('pool', lambda: nc.gpsimd.memset(Tst[h][:, :], 0.0), w=[Tst[h]])
        psY = p.ps[YB]

        def T(tag, shape=None, dt=F32):
            return p.sb(st0, tag, shape or [64, TT], dt)
        raw64 = T('raw64', [64, TT + 1])
        raw128 = T('raw128', [128, TT + 1])
        wl, al = T('wl'), T('al')
        gl1 = T('gl1', [128, TT])
        gl2 = T('gl2', [32, TT])
        r, k, v = T('r'), T('k'), T('v')
        sgm, cum, ag, kk, tmp, k2, bv, E = T('sgm'), T('cum'), T('ag'), T('kk'), T('tmp'), T('k2'), T('bv'), T('E')
        kt, bt, kh, bh = T('kt', dt=BF16), T('bt', dt=BF16), T('kh'), T('bh')
        at16, rt16 = T('at16', dt=BF16), T('rt16', dt=BF16)
        D = T('D', [64, NCH, CH])
        Aab = T('Aab', dt=BF16)
        Pp = [T('Pa', dt=BF16), T('Pb', dt=BF16)]
        Qp = [T('Qa', dt=BF16), T('Qb', dt=BF16)]
        Nx = T('Nx', dt=BF16)
        Ny = T('Ny', dt=BF16)
        HB = []
        for i in range(2):
            HB.append(dict(at=T('at'), AakT=T('AakT'), vT=T('vT'), N=T('N'), rt=T('rt'), ArbT=T('ArbT'), ArkT=T('ArkT'),
                           bhT=T('bhT'), khT=T('khT'), WC=T('WC', [64, NCH, 1]), bonus=T('bonus'), gh=T('gh')))
        X = T('X', [64, 64])
        U = T('U', [64, 64])
        no, nosq, nmean, nmsq = T('no'), T('nosq'), T('nmean'), T('nmsq')
        orw = T('orw', [64, TT], BF16)

        def col(name, h=0):
            return c64[:, C64[name] + h:C64[name] + h + 1]

        def ocol(name, h=0):
            return omu[:, C64[name] + h:C64[name] + h + 1]

        def load_shift(out, raw, tt, row0, nrows, mu_ap, omu_ap, rd):
            t0 = tt * TT
            if tt == 0:
                p.op('pool', lambda: nc.gpsimd.memset(raw[0:nrows, 0:1], 0.0), w=[raw])
                p.dma('sp', raw[0:nrows, 1:], SC['hT'][row0:row0 + nrows, 0:TT], r=hk('hT', row0, nrows, tt), w=[raw])
            else:
                p.dma('sp', raw[0:nrows, :], SC['hT'][row0:row0 + nrows, t0 - 1:t0 + TT], r=hk('hT', row0, nrows, tt, True), w=[raw])
            p.ts(out[:, :], raw[0:nrows, 1:TT + 1], omu_ap, None, ALU.mult, r=[raw] + rd, w=[out])
            p.stt(out[:, :], raw[0:nrows, 0:TT], mu_ap, out[:, :], ALU.mult, ALU.add, r=[raw, out] + rd, w=[out])

        def tile_prep(tt):
            load_shift(wl, raw64, tt, RWW, 64, col('mu_w'), ocol('mu_w'), [c64, omu])
            load_shift(al, raw64, tt, RWA, 64, col('mu_a'), ocol('mu_a'), [c64, omu])
            mg = C128['mu_g']
            load_shift(gl1, raw128, tt, RWG, 128, c128[:, mg:mg + 1], omug[:, 0:1], [c128, omug])
            load_shift(gl2, raw128, tt, RWG + 128, 32, c128[0:32, mg + 1:mg + 2], omug[0:32, 1:2], [c128, omug])
            p.act(wl[:, :], wl[:, :], AF.Tanh, r=[wl], w=[wl])
            p.act(gl1[:, :], gl1[:, :], AF.Sigmoid, r=[gl1], w=[gl1])
            p.act(gl2[:, :], gl2[:, :], AF.Sigmoid, r=[gl2], w=[gl2])

        def cslices(t):
            return [t[:, c * 64:(c + 1) * 64] for c in range(NCH)]

        def stage1(tt, h, B):
            p.ps_pool = POOL1
            if h == 0:
                tile_prep(tt)
            hs = slice(h * 64, (h + 1) * 64)
            load_shift(r, raw64, tt, RWR + h * 64, 64, col('mu_r', h), ocol('mu_r', h), [c64, omu])
            load_shift(k, raw64, tt, RWK + h * 64, 64, col('mu_k', h), ocol('mu_k', h), [c64, omu])
            load_shift(v, raw64, tt, RWV + h * 64, 64, col('mu_v', h), ocol('mu_v', h), [c64, omu])
            ps = p.psum()
            p.mm(ps[0:64, :], [(w2[:, hs], wl[:, :])], r=[w2, wl], w=[ps])
            p.act(sgm[:, :], ps[0:64, :], AF.Sigmoid, bias=col('w0', h), r=[ps, c64], w=[sgm])
            p.op('dve', lambda: nc.vector.tensor_tensor_scan(out=cum[:, :], data0=cst.m01[:, :], data1=sgm[:, :], initial=0.0,
                                                             op0=ALU.mult, op1=ALU.add), r=[cst.m01, sgm], w=[cum])
            ps = p.psum()
            p.mm(ps[0:64, :], [(a2[:, hs], al[:, :])], r=[a2, al], w=[ps])
            p.act(ag[:, :], ps[0:64, :], AF.Sigmoid, bias=col('a0', h), r=[ps, c64], w=[ag])
            ps = p.psum()
            p.mm(ps[0:64, :], [(g2a[:, hs], gl1[:, :]), (g2b[:, hs], gl2[:, :])], r=[g2a, g2b, gl1, gl2], w=[ps])
            p.copy(B['gh'][:, :], ps[0:64, :], r=[ps], w=[B['gh']], eng='act')
            p.ts(kk[:, :], k[:, :], col('k_k', h), None, ALU.mult, r=[k, c64], w=[kk])
            p.act(tmp[:, :], kk[:, :], AF.Square, r=[kk], w=[tmp])
            ps = p.psum()
            p.mm(ps[0:64, :], [(cst.ones64[:, :], tmp[:, :])], r=[cst.ones64, tmp], w=[ps])
            p.act(tmp[:, :], ps[0:64, :], AF.Sqrt, r=[ps], w=[tmp])
            p.ts(tmp[:, :], tmp[:, :], 1e-12, None, ALU.max, r=[tmp], w=[tmp])
            p.op('dve', lambda: nc.vector.reciprocal(out=tmp[:, :], in_=tmp[:, :]), r=[tmp], w=[tmp])
            p.tt(kk[:, :], kk[:, :], tmp[:, :], ALU.mult, r=[kk, tmp], w=[kk])
            p.ts(tmp[:, :], ag[:, :], col('k_a', h), ocol('k_a', h), ALU.mult, ALU.add, r=[ag, c64, omu], w=[tmp])
            p.tt(k2[:, :], k[:, :], tmp[:, :], ALU.mult, r=[k, tmp], w=[k2])
            p.tt(bv[:, :], kk[:, :], ag[:, :], ALU.mult, r=[kk, ag], w=[bv])
            rt, at = B['rt'], B['at']
            p.act(E[:, :], cum[:, :], AF.Exp, scale=-C0, r=[cum], w=[E])
            p.tt(rt[:, :], r[:, :], E[:, :], ALU.mult, r=[r, E], w=[rt])
            p.copy(rt16[:, :], rt[:, :], r=[rt], w=[rt16], eng='act')
            p.act(E[:, :], cum[:, :], AF.Exp, scale=C0, r=[cum], w=[E])
            p.tt(kt[:, :], k2[:, :], E[:, :], ALU.mult, r=[k2, E], w=[kt])
            p.tt(bt[:, :], bv[:, :], E[:, :], ALU.mult, r=[bv, E], w=[bt])
            p.tt(tmp[:, :], cum[:, :], sgm[:, :], ALU.subtract, r=[cum, sgm], w=[tmp])
            p.act(E[:, :], tmp[:, :], AF.Exp, scale=-C0, r=[tmp], w=[E])
            p.stt(at[:, :], kk[:, :], -1.0, E[:, :], ALU.mult, ALU.mult, r=[kk, E], w=[at])
            p.copy(at16[:, :], at[:, :], r=[at], w=[at16], eng='act')
            c3 = cview(cum)
            p.tt(D[:, :, :], c3[:, :, CH - 1:CH].to_broadcast([64, NCH, CH]), c3, ALU.subtract, r=[cum], w=[D])
            p.act(D[:, :, :], D[:, :, :], AF.Exp, scale=-C0, r=[D], w=[D])
            Df = D[:, :, :].rearrange("p c t -> p (c t)")
            p.tt(kh[:, :], k2[:, :], Df, ALU.mult, r=[k2, D], w=[kh])
            p.tt(bh[:, :], bv[:, :], Df, ALU.mult, r=[bv, D], w=[bh])
            p.act(B['WC'][:, :, :], c3[:, :, CH - 1:CH], AF.Exp, scale=-C0, r=[cum], w=[B['WC']])
            p.stt(tmp[:, :], r[:, :], col('r_k', h), k2[:, :], ALU.mult, ALU.mult, r=[r, k2, c64], w=[tmp])
            ps = p.psum()
            p.mm(ps[0:64, :], [(cst.ones64[:, :], tmp[:, :])], r=[cst.ones64, tmp], w=[ps])
            p.tt(B['bonus'][:, :], ps[0:64, :], v[:, :], ALU.mult, r=[ps, v], w=[B['bonus']])
            for src, dn in ((v, 'vT'), (kh, 'khT'), (bh, 'bhT')):
                psT = p.psum()
                p.transposes(list(zip([psT[0:64, c * 64:(c + 1) * 64] for c in range(NCH)], cslices(src))),
                             cst.ident[:, :], r=[src, cst.ident], w=[psT])
                p.copy(B[dn][:, :], psT[0:64, :], r=[psT], w=[B[dn]], eng='act')

            def amat(lt, rh, mask, d):
                psA = p.psum()
                p.mms([(psA[0:64, c * 64:(c + 1) * 64], [(lt[:, c * 64:(c + 1) * 64], rh[:, c * 64:(c + 1) * 64])]) for c in range(NCH)],
                      r=[lt, rh], w=[psA])
                p.tt(d[:, :], psA[0:64, :], mask[:, :, :].rearrange("p c t -> p (c t)"), ALU.mult, r=[psA, mask], w=[d])
            amat(bt, at16, cst.mgt, Qp[0])
            amat(bt, rt16, cst.mge, B['ArbT'])
            amat(kt, at16, cst.mgt, B['AakT'])
            amat(kt, rt16, cst.mge, B['ArkT'])
            amat(at16, bt, cst.mlt, Aab)
            P_, Q_ = Aab, Qp[0]
            Ns = [Nx, Ny]
            N_ = Ns[0]
            p.tt(N_[:, :], Q_[:, :], cst.id8[:, :, :].rearrange("p c t -> p (c t)"), ALU.add, r=[Q_, cst.id8], w=[N_])
            for lev in range(5):
                psP = p.psum()
                p.mms([(psP[0:64, c * 64:(c + 1) * 64], [(Q_[:, c * 64:(c + 1) * 64], P_[:, c * 64:(c + 1) * 64])]) for c in range(NCH)],
                      r=[P_, Q_], w=[psP])
                if lev < 4:
                    psQ = p.psum()
                    p.mms([(psQ[0:64, c * 64:(c + 1) * 64], [(P_[:, c * 64:(c + 1) * 64], Q_[:, c * 64:(c + 1) * 64])]) for c in range(NCH)],
                          r=[P_, Q_], w=[psQ])
                P2 = Pp[lev % 2]
                p.copy(P2[:, :], psP[0:64, :], r=[psP], w=[P2], eng='act')
                if lev < 4:
                    Q2 = Qp[(lev + 1) % 2]
                    p.copy(Q2[:, :], psQ[0:64, :], r=[psQ], w=[Q2], eng='dve')
                psN = p.psum()
                p.mms([(psN[0:64, c * 64:(c + 1) * 64], [(P2[:, c * 64:(c + 1) * 64], N_[:, c * 64:(c + 1) * 64])]) for c in range(NCH)],
                      r=[P2, N_], w=[psN])
                N2 = Ns[(lev + 1) % 2] if lev < 4 else B['N']
                p.tt(N2[:, :], N_[:, :], psN[0:64, :], ALU.add, r=[N_, psN], w=[N2])
                P_, N_ = P2, N2
                if lev < 4:
                    Q_ = Q2
            assert N_ is B['N']

        def stage2(tt, h, B):
            p.ps_pool = POOL2
            tsl = slice(tt * TT, (tt + 1) * TT)
            at, AakT, vT, N_, rt, ArbT, ArkT, bhT, khT, WC = (B[x] for x in ('at', 'AakT', 'vT', 'N', 'rt', 'ArbT', 'ArkT', 'bhT', 'khT', 'WC'))
            for c in range(NCH):
                cs = slice(c * 64, (c + 1) * 64)
                psX = p.psum()
                p.mm(psX[0:64, 0:64], [(at[:, cs], Tst[h][:, :]), (AakT[:, cs], vT[:, cs])], r=[at, Tst[h], AakT, vT], w=[psX])
                p.copy(X[:, :], psX[0:64, 0:64], r=[psX], w=[X], eng='act')
                psU = p.psum()
                p.mm(psU[0:64, 0:64], [(N_[:, cs], X[:, :])], r=[N_, X], w=[psU])
                p.copy(U[:, :], psU[0:64, 0:64], r=[psU], w=[U], eng='dve')
                p.mm(psY[0:64, cs], [(Tst[h][:, :], rt[:, cs]), (U[:, :], ArbT[:, cs]), (vT[:, cs], ArkT[:, cs])],
                     r=[Tst[h], rt, U, ArbT, vT, ArkT], w=[psY])
                psT2 = p.psum()
                p.mm(psT2[0:64, 0:64], [(bhT[:, cs], U[:, :]), (khT[:, cs], vT[:, cs])], r=[bhT, U, khT, vT], w=[psT2])
                p.stt(Tst[h][:, :], Tst[h][:, :], WC[:, c, :], psT2[0:64, 0:64], ALU.mult, ALU.add, r=[Tst[h], WC, psT2], w=[Tst[h]])
            ones = cst.onesm64
            p.copy(no[:, :], psY[0:64, :], r=[psY], w=[no], eng='act')
            p.act(nosq[:, :], psY[0:64, :], AF.Square, r=[psY], w=[nosq])
            psM = p.psum()
            p.mm(psM[0:64, :], [(ones[:, :], no[:, :])], r=[ones, no], w=[psM])
            psQ2 = p.psum()
            p.mm(psQ2[0:64, :], [(ones[:, :], nosq[:, :])], r=[ones, nosq], w=[psQ2])
            p.copy(nmean[:, :], psM[0:64, :], r=[psM], w=[nmean], eng='act')
            p.act(nmsq[:, :], psM[0:64, :], AF.Square, r=[psM], w=[nmsq])
            p.tt(nosq[:, :], psQ2[0:64, :], nmsq[:, :], ALU.subtract, r=[psQ2, nmsq], w=[nosq])
            p.ts(nosq[:, :], nosq[:, :], 0.0, 64e-5, ALU.max, ALU.add, r=[nosq], w=[nosq])
            p.act(nmsq[:, :], nosq[:, :], AF.Sqrt, r=[nosq], w=[nmsq])
            p.op('dve', lambda: nc.vector.reciprocal(out=nosq[:, :], in_=nmsq[:, :]), r=[nmsq], w=[nosq])
            p.tt(no[:, :], no[:, :], nmean[:, :], ALU.subtract, r=[no, nmean], w=[no])
            p.tt(no[:, :], no[:, :], nosq[:, :], ALU.mult, r=[no, nosq], w=[no])
            p.ts(no[:, :], no[:, :], col('lnx_g', h), col('lnx_b', h), ALU.mult, ALU.add, r=[no, c64], w=[no])
            p.tt(no[:, :], no[:, :], B['bonus'][:, :], ALU.add, r=[no, B['bonus']], w=[no])
            p.tt(orw[:, :], no[:, :], B['gh'][:, :], ALU.mult, r=[no, B['gh']], w=[orw])
            p.dma('sp', SC['orT'][h * 64:(h + 1) * 64, tsl], orw[:, :], r=[orw], w=[('orT', h, tt)])

        seq = [(tt, h) for tt in range(NT) for h in range(8)]
        L1 = p.record(lambda: stage1(seq[0][0], seq[0][1], HB[0]))
        p.play([L1])
        for n, (tt, h) in enumerate(seq):
            L2 = p.record(lambda: stage2(tt, h, HB[n % 2]))
            lists = [L2]
            if n + 1 < len(seq):
                tn, hn = seq[n + 1]
                lists.append(p.record(lambda: stage1(tn, hn, HB[(n + 1) % 2])))
            if cstream is not None:
                lists.append(cstream.take(len(seq) - n))
            p.play(lists)
    p.ps_pool = list(range(8))


def phase_rwkv3(nc, p, IN, SC, l, cst, c128, c64, cstream=None):
    POOLA = [0, 1]
    POOLB = [2, 3]
    POOLC = [4, 5]
    with p.scope() as st0:
        w2 = p.sb(st0, 'w2', [64, 512])
        a2 = p.sb(st0, 'a2', [64, 512])
        g2a = p.sb(st0, 'g2a', [128, 512])
        g2b = p.sb(st0, 'g2b', [32, 512])
        p.dma('sp', w2[:, :], IN['rwkv_w2'][l], w=[w2])
        p.dma('sp', a2[:, :], IN['rwkv_a2'][l], w=[a2])
        p.dma('sp', g2a[:, :], IN['rwkv_g2'][l, 0:128, :], w=[g2a])
        p.dma('sp', g2b[:, :], IN['rwkv_g2'][l, 128:160, :], w=[g2b])
        omu = p.sb(st0, 'omu', [64, N64])
        p.ts(omu[:, :], c64[:, :], -1.0, 1.0, ALU.mult, ALU.add, r=[c64], w=[omu])
        omug = p.sb(st0, 'omug', [128, 2])
        p.ts(omug[:, :], c128[:, C128['mu_g']:C128['mu_g'] + 2], -1.0, 1.0, ALU.mult, ALU.add, r=[c128], w=[omug])
        Tst = [p.sb(st0, 'Tst%d' % h, [64, 64]) for h in range(8)]
        for h in range(8):
            p.op('pool', lambda: nc.gpsimd.memset(Tst[h][:, :], 0.0), w=[Tst[h]])
        psYs = [p.ps[6], p.ps[7]]

        def T(tag, shape=None, dt=F32):
            return p.sb(st0, tag, shape or [64, TT], dt)
        raw64 = T('raw64', [64, TT + 1])
        raw3 = [T('raw3', [64, TT + 1]) for _ in range(3)]
        raw128 = T('raw128', [128, TT + 1])
        wl, al = T('wl'), T('al')
        gl1 = T('gl1', [128, TT])
        gl2 = T('gl2', [32, TT])
        r, k, v = T('r'), T('k'), T('v')
        sgm, cum, ag, kk, tmp, k2, bv, E = T('sgm'), T('cum'), T('ag'), T('kk'), T('tmp'), T('k2'), T('bv'), T('E')
        kt, bt, kh, bh = T('kt', dt=BF16), T('bt', dt=BF16), T('kh'), T('bh')
        at16, rt16 = T('at16', dt=BF16), T('rt16', dt=BF16)
        D = T('D', [64, NCH, CH])
        AQ = [dict(Aab=T('Aab', dt=BF16), Q0=T('Q0', dt=BF16)) for _ in range(2)]
        Pp = [T('Pa', dt=BF16), T('Pb', dt=BF16)]
        Qp = [T('Qa', dt=BF16), T('Qb', dt=BF16)]
        Nx = T('Nx', dt=BF16)
        Ny = T('Ny', dt=BF16)
        HB = []
        for i in range(3):
            HB.append(dict(at=T('at'), AakT=T('AakT'), vT=T('vT'), N=T('N'), rt=T('rt'), ArbT=T('ArbT'), ArkT=T('ArkT'),
                           bhT=T('bhT'), khT=T('khT'), WC=T('WC', [64, NCH, 1]), bonus=T('bonus'), gh=T('gh')))
        X = T('X', [64, 64])
        U = T('U', [64, 64])
        no, nosq, nmean, nmsq = T('no'), T('nosq'), T('nmean'), T('nmsq')
        orw = T('orw', [64, TT], BF16)

        def col(name, h=0):
            return c64[:, C64[name] + h:C64[name] + h + 1]

        def ocol(name, h=0):
            return omu[:, C64[name] + h:C64[name] + h + 1]

        def load_shift(out, raw, tt, row0, nrows, mu_ap, omu_ap, rd):
            t0 = tt * TT
            if tt == 0:
                p.op('pool', lambda: nc.gpsimd.memset(raw[0:nrows, 0:1], 0.0), w=[raw])
                p.dma('sp', raw[0:nrows, 1:], SC['hT'][row0:row0 + nrows, 0:TT], r=hk('hT', row0, nrows, tt), w=[raw])
            else:
                p.dma('sp', raw[0:nrows, :], SC['hT'][row0:row0 + nrows, t0 - 1:t0 + TT], r=hk('hT', row0, nrows, tt, True), w=[raw])
            p.act(out[:, :], raw[0:nrows, 1:TT + 1], AF.Identity, scale=omu_ap, r=[raw] + rd, w=[out])
            p.stt(out[:, :], raw[0:nrows, 0:TT], mu_ap, out[:, :], ALU.mult, ALU.add, r=[raw, out] + rd, w=[out])

        def tile_prep(tt):
            load_shift(wl, raw64, tt, RWW, 64, col('mu_w'), ocol('mu_w'), [c64, omu])
            load_shift(al, raw64, tt, RWA, 64, col('mu_a'), ocol('mu_a'), [c64, omu])
            mg = C128['mu_g']
            load_shift(gl1, raw128, tt, RWG, 128, c128[:, mg:mg + 1], omug[:, 0:1], [c128, omug])
            load_shift(gl2, raw128, tt, RWG + 128, 32, c128[0:32, mg + 1:mg + 2], omug[0:32, 1:2], [c128, omug])
            p.act(wl[:, :], wl[:, :], AF.Tanh, r=[wl], w=[wl])
            p.act(gl1[:, :], gl1[:, :], AF.Sigmoid, r=[gl1], w=[gl1])
            p.act(gl2[:, :], gl2[:, :], AF.Sigmoid, r=[gl2], w=[gl2])

        def cslices(t):
            return [t[:, c * 64:(c + 1) * 64] for c in range(NCH)]

        def stage1a(tt, h, B, aq):
            p.ps_pool = POOLA
            if h == 0:
                tile_prep(tt)
            hs = slice(h * 64, (h + 1) * 64)
            load_shift(r, raw3[0], tt, RWR + h * 64, 64, col('mu_r', h), ocol('mu_r', h), [c64, omu])
            load_shift(k, raw3[1], tt, RWK + h * 64, 64, col('mu_k', h), ocol('mu_k', h), [c64, omu])
            load_shift(v, raw3[2], tt, RWV + h * 64, 64, col('mu_v', h), ocol('mu_v', h), [c64, omu])
            ps = p.psum()
            p.mm(ps[0:64, :], [(w2[:, hs], wl[:, :])], r=[w2, wl], w=[ps])
            p.act(sgm[:, :], ps[0:64, :], AF.Sigmoid, bias=col('w0', h), r=[ps, c64], w=[sgm])
            p.op('dve', lambda: nc.vector.tensor_tensor_scan(out=cum[:, :], data0=cst.m01[:, :], data1=sgm[:, :], initial=0.0,
                                                             op0=ALU.mult, op1=ALU.add), r=[cst.m01, sgm], w=[cum])
            ps = p.psum()
            p.mm(ps[0:64, :], [(a2[:, hs], al[:, :])], r=[a2, al], w=[ps])
            p.act(ag[:, :], ps[0:64, :], AF.Sigmoid, bias=col('a0', h), r=[ps, c64], w=[ag])
            ps = p.psum()
            p.mm(ps[0:64, :], [(g2a[:, hs], gl1[:, :]), (g2b[:, hs], gl2[:, :])], r=[g2a, g2b, gl1, gl2], w=[ps])
            p.copy(B['gh'][:, :], ps[0:64, :], r=[ps], w=[B['gh']], eng='act')
            p.act(kk[:, :], k[:, :], AF.Identity, scale=col('k_k', h), r=[k, c64], w=[kk])
            p.act(tmp[:, :], kk[:, :], AF.Square, r=[kk], w=[tmp])
            ps = p.psum()
            p.mm(ps[0:64, :], [(cst.ones64[:, :], tmp[:, :])], r=[cst.ones64, tmp], w=[ps])
            p.act(tmp[:, :], ps[0:64, :], AF.Sqrt, r=[ps], w=[tmp])
            p.ts(tmp[:, :], tmp[:, :], 1e-12, None, ALU.max, r=[tmp], w=[tmp])
            p.op('dve', lambda: nc.vector.reciprocal(out=tmp[:, :], in_=tmp[:, :]), r=[tmp], w=[tmp])
            p.tt(kk[:, :], kk[:, :], tmp[:, :], ALU.mult, r=[kk, tmp], w=[kk])
            p.act(tmp[:, :], ag[:, :], AF.Identity, bias=ocol('k_a', h), scale=col('k_a', h), r=[ag, c64, omu], w=[tmp])
            p.tt(k2[:, :], k[:, :], tmp[:, :], ALU.mult, r=[k, tmp], w=[k2])
            p.tt(bv[:, :], kk[:, :], ag[:, :], ALU.mult, r=[kk, ag], w=[bv])
            rt, at = B['rt'], B['at']
            p.act(E[:, :], cum[:, :], AF.Exp, scale=-C0, r=[cum], w=[E])
            p.tt(rt[:, :], r[:, :], E[:, :], ALU.mult, r=[r, E], w=[rt])
            p.copy(rt16[:, :], rt[:, :], r=[rt], w=[rt16], eng='act')
            p.act(E[:, :], cum[:, :], AF.Exp, scale=C0, r=[cum], w=[E])
            p.tt(kt[:, :], k2[:, :], E[:, :], ALU.mult, r=[k2, E], w=[kt])
            p.tt(bt[:, :], bv[:, :], E[:, :], ALU.mult, r=[bv, E], w=[bt])
            p.tt(tmp[:, :], cum[:, :], sgm[:, :], ALU.subtract, r=[cum, sgm], w=[tmp])
            p.act(E[:, :], tmp[:, :], AF.Exp, scale=-C0, r=[tmp], w=[E])
            p.stt(at[:, :], kk[:, :], -1.0, E[:, :], ALU.mult, ALU.mult, r=[kk, E], w=[at])
            p.copy(at16[:, :], at[:, :], r=[at], w=[at16], eng='act')
            c3 = cview(cum)
            p.tt(D[:, :, :], c3[:, :, CH - 1:CH].to_broadcast([64, NCH, CH]), c3, ALU.subtract, r=[cum], w=[D])
            p.act(D[:, :, :], D[:, :, :], AF.Exp, scale=-C0, r=[D], w=[D])
            Df = D[:, :, :].rearrange("p c t -> p (c t)")
            p.tt(kh[:, :], k2[:, :], Df, ALU.mult, r=[k2, D], w=[kh])
            p.tt(bh[:, :], bv[:, :], Df, ALU.mult, r=[bv, D], w=[bh])
            p.act(B['WC'][:, :, :], c3[:, :, CH - 1:CH], AF.Exp, scale=-C0, r=[cum], w=[B['WC']])
            p.stt(tmp[:, :], r[:, :], col('r_k', h), k2[:, :], ALU.mult, ALU.mult, r=[r, k2, c64], w=[tmp])
            ps = p.psum()
            p.mm(ps[0:64, :], [(cst.ones64[:, :], tmp[:, :])], r=[cst.ones64, tmp], w=[ps])
            p.tt(B['bonus'][:, :], ps[0:64, :], v[:, :], ALU.mult, r=[ps, v], w=[B['bonus']])
            for src, dn in ((v, 'vT'), (kh, 'khT'), (bh, 'bhT')):
                psT = p.psum()
                p.transposes(list(zip([psT[0:64, c * 64:(c + 1) * 64] for c in range(NCH)], cslices(src))),
                             cst.ident[:, :], r=[src, cst.ident], w=[psT])
                p.copy(B[dn][:, :], psT[0:64, :], r=[psT], w=[B[dn]], eng='act')

            def amat(lt, rh, mask, d):
                psA = p.psum()
                p.mms([(psA[0:64, c * 64:(c + 1) * 64], [(lt[:, c * 64:(c + 1) * 64], rh[:, c * 64:(c + 1) * 64])]) for c in range(NCH)],
                      r=[lt, rh], w=[psA])
                p.tt(d[:, :], psA[0:64, :], mask[:, :, :].rearrange("p c t -> p (c t)"), ALU.mult, r=[psA, mask], w=[d])
            amat(bt, at16, cst.mgt, aq['Q0'])
            amat(bt, rt16, cst.mge, B['ArbT'])
            amat(kt, at16, cst.mgt, B['AakT'])
            amat(kt, rt16, cst.mge, B['ArkT'])
            amat(at16, bt, cst.mlt, aq['Aab'])

        def stage1b(B, aq):
            p.ps_pool = POOLB
            P_, Q_ = aq['Aab'], aq['Q0']
            Ns = [Nx, Ny]
            N_ = Ns[0]
            p.tt(N_[:, :], Q_[:, :], cst.id8[:, :, :].rearrange("p c t -> p (c t)"), ALU.add, r=[Q_, cst.id8], w=[N_])
            for lev in range(5):
                psP = p.psum()
                p.mms([(psP[0:64, c * 64:(c + 1) * 64], [(Q_[:, c * 64:(c + 1) * 64], P_[:, c * 64:(c + 1) * 64])]) for c in range(NCH)],
                      r=[P_, Q_], w=[psP])
                if lev < 4:
                    psQ = p.psum()
                    p.mms([(psQ[0:64, c * 64:(c + 1) * 64], [(P_[:, c * 64:(c + 1) * 64], Q_[:, c * 64:(c + 1) * 64])]) for c in range(NCH)],
                          r=[P_, Q_], w=[psQ])
                P2 = Pp[lev % 2]
                p.copy(P2[:, :], psP[0:64, :], r=[psP], w=[P2], eng='act')
                if lev < 4:
                    Q2 = Qp[lev % 2]
                    p.copy(Q2[:, :], psQ[0:64, :], r=[psQ], w=[Q2], eng='act')
                psN = p.psum()
                p.mms([(psN[0:64, c * 64:(c + 1) * 64], [(P2[:, c * 64:(c + 1) * 64], N_[:, c * 64:(c + 1) * 64])]) for c in range(NCH)],
                      r=[P2, N_], w=[psN])
                N2 = Ns[(lev + 1) % 2] if lev < 4 else B['N']
                p.tt(N2[:, :], N_[:, :], psN[0:64, :], ALU.add, r=[N_, psN], w=[N2])
                P_, N_ = P2, N2
                if lev < 4:
                    Q_ = Q2
            assert N_ is B['N']

        def chain(tt, h, B, psY):
            p.ps_pool = POOLC
            at, AakT, vT, N_, rt, ArbT, ArkT, bhT, khT, WC = (B[x] for x in ('at', 'AakT', 'vT', 'N', 'rt', 'ArbT', 'ArkT', 'bhT', 'khT', 'WC'))
            for c in range(NCH):
                cs = slice(c * 64, (c + 1) * 64)
                psX = p.psum()
                p.mm(psX[0:64, 0:64], [(at[:, cs], Tst[h][:, :]), (AakT[:, cs], vT[:, cs])], r=[at, Tst[h], AakT, vT], w=[psX])
                p.copy(X[:, :], psX[0:64, 0:64], r=[psX], w=[X], eng='act')
                psU = p.psum()
                p.mm(psU[0:64, 0:64], [(N_[:, cs], X[:, :])], r=[N_, X], w=[psU])
                p.copy(U[:, :], psU[0:64, 0:64], r=[psU], w=[U], eng='act')
                p.mm(psY[0:64, cs], [(Tst[h][:, :], rt[:, cs]), (U[:, :], ArbT[:, cs]), (vT[:, cs], ArkT[:, cs])],
                     r=[Tst[h], rt, U, ArbT, vT, ArkT], w=[psY])
                psT2 = p.psum()
                p.mm(psT2[0:64, 0:64], [(bhT[:, cs], U[:, :]), (khT[:, cs], vT[:, cs])], r=[bhT, U, khT, vT], w=[psT2])
                p.stt(Tst[h][:, :], Tst[h][:, :], WC[:, c, :], psT2[0:64, 0:64], ALU.mult, ALU.add, r=[Tst[h], WC, psT2], w=[Tst[h]])

        def norm(tt, h, B, psY):
            p.ps_pool = POOLA
            tsl = slice(tt * TT, (tt + 1) * TT)
            ones = cst.onesm64
            p.copy(no[:, :], psY[0:64, :], r=[psY], w=[no], eng='act')
            p.act(nosq[:, :], psY[0:64, :], AF.Square, r=[psY], w=[nosq])
            psM = p.psum()
            p.mm(psM[0:64, :], [(ones[:, :], no[:, :])], r=[ones, no], w=[psM])
            psQ2 = p.psum()
            p.mm(psQ2[0:64, :], [(ones[:, :], nosq[:, :])], r=[ones, nosq], w=[psQ2])
            p.copy(nmean[:, :], psM[0:64, :], r=[psM], w=[nmean], eng='act')
            p.act(nmsq[:, :], psM[0:64, :], AF.Square, r=[psM], w=[nmsq])
            p.tt(nosq[:, :], psQ2[0:64, :], nmsq[:, :], ALU.subtract, r=[psQ2, nmsq], w=[nosq])
            p.ts(nosq[:, :], nosq[:, :], 0.0, 64e-5, ALU.max, ALU.add, r=[nosq], w=[nosq])
            p.act(nmsq[:, :], nosq[:, :], AF.Sqrt, r=[nosq], w=[nmsq])
            p.op('dve', lambda: nc.vector.reciprocal(out=nosq[:, :], in_=nmsq[:, :]), r=[nmsq], w=[nosq])
            p.tt(no[:, :], no[:, :], nmean[:, :], ALU.subtract, r=[no, nmean], w=[no])
            p.tt(no[:, :], no[:, :], nosq[:, :], ALU.mult, r=[no, nosq], w=[no])
            p.ts(no[:, :], no[:, :], col('lnx_g', h), col('lnx_b', h), ALU.mult, ALU.add, r=[no, c64], w=[no])
            p.tt(no[:, :], no[:, :], B['bonus'][:, :], ALU.add, r=[no, B['bonus']], w=[no])
            p.tt(orw[:, :], no[:, :], B['gh'][:, :], ALU.mult, r=[no, B['gh']], w=[orw])
            p.dma('sp', SC['orT'][h * 64:(h + 1) * 64, tsl], orw[:, :], r=[orw], w=[('orT', h, tt)])

        seq = [(tt, h) for tt in range(NT) for h in range(8)]
        NS = len(seq)

        def A_stream(n):
            def f():
                if 0 <= n - 1 < NS:
                    t_, h_ = seq[n - 1]
                    norm(t_, h_, HB[(n - 1) % 3], psYs[(n - 1) % 2])
                if n + 2 < NS:
                    t_, h_ = seq[n + 2]
                    stage1a(t_, h_, HB[(n + 2) % 3], AQ[(n + 2) % 2])
            return p.record(f)

        def B_stream(n):
            def f():
                if n + 1 < NS:
                    stage1b(HB[(n + 1) % 3], AQ[(n + 1) % 2])
            return p.record(f)

        def C_stream(n):
            def f():
                if 0 <= n < NS:
                    t_, h_ = seq[n]
                    chain(t_, h_, HB[n % 3], psYs[n % 2])
            return p.record(f)
        for n in range(-2, NS + 1):
            lists = [C_stream(n), B_stream(n), A_stream(n)]
            if cstream is not None and 0 <= n < NS:
                lists.append(cstream.take(NS - n))
            p.play(lists)
    p.ps_pool = list(range(8))
```
